# Optimizing a Trainium2 kernel written in Bass

```python
import math
import jax
import jax.numpy as jnp
from jax import lax
import numpy as np

D_MODEL = 1024
BATCH = 16
SEQ = 256
DEPTH = 4
DEC_BATCH = 2
DEC_SEQ = 4096
PAST_LEN = 256

GRID_W = 64
N_MIXERS = 3
N_A_LAYERS = (DEPTH + 2) // 3
N_B_LAYERS = (DEPTH + 1) // 3
N_C_LAYERS = DEPTH // 3
NORM_EPS = 1e-6

S5_GROUP_CH = 16
S5_GROUPS = D_MODEL // S5_GROUP_CH
S5_STATE = 64
S5_DT_MIN = 1e-3
S5_DT_MAX = 1e-1

HG_DK = 128
HG_HEADS = D_MODEL // HG_DK
HG_DV = D_MODEL // HG_HEADS
HG_CHUNK = 64

SSD_INNER = 2 * D_MODEL
SSD_HEADDIM = 64
SSD_HEADS = SSD_INNER // SSD_HEADDIM
SSD_GROUPS = 4
SSD_HPG = SSD_HEADS // SSD_GROUPS
SSD_STATE = 128
SSD_CONV = 5
SSD_CHUNK = 64
SSD_XBC = SSD_INNER + 2 * SSD_GROUPS * SSD_STATE
SSD_IN = SSD_INNER + SSD_XBC + 2 * SSD_HEADS
SSD_DT_MIN = 1e-3
SSD_DT_MAX = 1e-1

N_EXPERTS = 16
EXPERT_FF = 2 * D_MODEL
EC_CAPACITY = 2

kernel_name = 'hybrid_s5_hgrn2_ssd_ecmoe_diffusion_step'


def rmsnorm(x, g):
    xf = x.astype(jnp.float32)
    xf = xf * lax.rsqrt(jnp.mean(xf * xf, axis=-1, keepdims=True) + NORM_EPS)
    return (xf * g.astype(jnp.float32)).astype(x.dtype)


def grid_pos_embed(n_tokens):
    rows = n_tokens // GRID_W
    quarter = D_MODEL // 4
    omega = 1.0 / (10000.0 ** (jnp.arange(quarter, dtype=jnp.float32) / quarter))
    r = jnp.arange(rows, dtype=jnp.float32)[:, None] * omega
    cl = jnp.arange(GRID_W, dtype=jnp.float32)[:, None] * omega
    emb_r = jnp.concatenate([jnp.sin(r), jnp.cos(r)], axis=-1)
    emb_c = jnp.concatenate([jnp.sin(cl), jnp.cos(cl)], axis=-1)
    emb = jnp.concatenate([jnp.broadcast_to(emb_r[:, None], (rows, GRID_W, D_MODEL // 2)),
                           jnp.broadcast_to(emb_c[None], (rows, GRID_W, D_MODEL // 2))], axis=-1)
    return emb.reshape(rows * GRID_W, D_MODEL)


def _linear_combine(left, right):
    a1, b1 = left
    a2, b2 = right
    return a1 * a2, a2 * b1 + b2


def s5_mixer(h, h0_re, h0_im, lam_re, lam_im, log_dt, b_re, b_im, c_re, c_im, d_skip, w_glu, b_glu):
    bsz, t, _ = h.shape
    hf = h.astype(jnp.float32)
    u = hf.reshape(bsz, t, S5_GROUPS, S5_GROUP_CH).astype(jnp.complex64)
    h0 = lax.complex(h0_re.astype(jnp.float32), h0_im.astype(jnp.float32))
    y = d_skip.astype(jnp.float32) * hf
    fin = []
    for d in range(2):
        lam = lax.complex(lam_re[d].astype(jnp.float32), lam_im[d].astype(jnp.float32))
        dt = jnp.exp(log_dt[d].astype(jnp.float32))[:, None]
        lam_bar = jnp.exp(lam * dt)
        b_bar = ((lam_bar - 1.0) / lam)[..., None] * lax.complex(b_re[d].astype(jnp.float32), b_im[d].astype(jnp.float32))
        cmat = lax.complex(c_re[d].astype(jnp.float32), c_im[d].astype(jnp.float32))
        bu = jnp.einsum('gph,btgh->btgp', b_bar, u)
        acum, hs = lax.associative_scan(_linear_combine, (jnp.broadcast_to(lam_bar, bu.shape), bu),
                                        reverse=(d == 1), axis=1)
        hs = hs + acum * h0[:, d][:, None]
        y = y + jnp.einsum('ghp,btgp->btgh', cmat, hs).real.reshape(bsz, t, D_MODEL)
        fin.append(hs[:, 0] if d == 1 else hs[:, -1])
    fin = jnp.stack(fin, axis=1)
    yg = jax.nn.gelu(y).astype(h.dtype)
    ab = yg @ w_glu + b_glu
    out = ab[..., :D_MODEL] * jax.nn.sigmoid(ab[..., D_MODEL:])
    return out, jnp.real(fin), jnp.imag(fin)


def gla_chunked(q, k, v, logf, s0):
    bsz, t, nh, _ = q.shape
    n = t // HG_CHUNK

    def to_chunks(z):
        return jnp.moveaxis(z.reshape(bsz, n, HG_CHUNK, *z.shape[2:]), 1, 0)

    mask = jnp.tril(jnp.ones((HG_CHUNK, HG_CHUNK), dtype=bool))[:, :, None, None]

    def step(s, inp):
        qi, ki, vi, gi = inp
        cum = jnp.cumsum(gi, axis=1)
        seg = jnp.where(mask, cum[:, :, None] - cum[:, None], -jnp.inf)
        scores = jnp.einsum('bthk,bshk,btshk->bhts', qi, ki, jnp.exp(seg))
        o = (jnp.einsum('bhts,bshv->bthv', scores, vi)
             + jnp.einsum('bthk,bhkv->bthv', qi * jnp.exp(cum), s))
        last = cum[:, -1]
        s_new = (jnp.exp(last)[..., None] * s
                 + jnp.einsum('bshk,bshv->bhkv', ki * jnp.exp(last[:, None] - cum), vi))
        return s_new, o

    s_fin, oc = lax.scan(step, s0, (to_chunks(q), to_chunks(k), to_chunks(v), to_chunks(logf)))
    o = jnp.moveaxis(oc, 0, 1).reshape(bsz, t, nh, v.shape[-1])
    return o, s_fin


def hgrn2_mixer(h, s0, lb, w_qig, w_f, b_f, g_norm, w_o):
    bsz, t, _ = h.shape
    q, v, gate = jnp.split((h @ w_qig).astype(jnp.float32), 3, axis=-1)
    q = q.reshape(bsz, t, HG_HEADS, HG_DK)
    v = v.reshape(bsz, t, HG_HEADS, HG_DV)
    outs, fin = [], []
    for d in range(2):
        lbd = lb[d].reshape(HG_HEADS, HG_DK)
        f = lbd + (1.0 - lbd) * jax.nn.sigmoid((h @ w_f[d] + b_f[d]).astype(jnp.float32).reshape(bsz, t, HG_HEADS, HG_DK))
        args = (q, 1.0 - f, v, jnp.log(f))
        if d == 1:
            args = tuple(jnp.flip(a, axis=1) for a in args)
        od, sd = gla_chunked(*args, s0[:, d].astype(jnp.float32))
        outs.append(jnp.flip(od, axis=1) if d == 1 else od)
        fin.append(sd)
    o = rmsnorm(outs[0] + outs[1], g_norm) * jax.nn.silu(gate.reshape(bsz, t, HG_HEADS, HG_DV))
    out = o.reshape(bsz, t, D_MODEL).astype(h.dtype) @ w_o
    return out, jnp.stack(fin, axis=1)


def depthwise_conv_centred(x, w, b):
    k = w.shape[0]
    y = lax.conv_general_dilated(x, w[:, None, :], window_strides=(1,), padding=[(k // 2, k // 2)],
                                 dimension_numbers=('NWC', 'WIO', 'NWC'), feature_group_count=x.shape[-1])
    return y + b


def ssd_chunked(x, dt, bm, cm, a, h0):
    bsz, t = x.shape[:2]
    n = t // SSD_CHUNK

    def chunk(z):
        return z.reshape(bsz, n, SSD_CHUNK, *z.shape[2:])

    x, dt, bm, cm = chunk(x), chunk(dt), chunk(bm), chunk(cm)
    cum = jnp.cumsum(dt * a, axis=2)
    mask = jnp.tril(jnp.ones((SSD_CHUNK, SSD_CHUNK), dtype=bool))[:, :, None, None]
    lmat = jnp.exp(jnp.where(mask, cum[:, :, :, None] - cum[:, :, None], -jnp.inf))
    cb = jnp.einsum('bnlgd,bnmgd->bnlmg', cm, bm)
    xdt = x * dt[..., None]
    y = jnp.einsum('bnlmgj,bnmgjp->bnlgjp', cb[..., None] * lmat, xdt)
    dec_end = jnp.exp(cum[:, :, -1:] - cum)
    chunk_states = jnp.einsum('bnmgd,bnmgjp->bngjpd', bm, xdt * dec_end[..., None])
    chunk_decay = jnp.exp(cum[:, :, -1])

    def step(hc, inp):
        dec, st = inp
        return dec[..., None, None] * hc + st, hc

    h_fin, h_start = lax.scan(step, h0, (jnp.moveaxis(chunk_decay, 1, 0), jnp.moveaxis(chunk_states, 1, 0)))
    h_start = jnp.moveaxis(h_start, 0, 1)
    y = y + jnp.einsum('bnlgd,bngjpd->bnlgjp', cm, h_start) * jnp.exp(cum)[..., None]
    return y.reshape(bsz, t, *y.shape[3:]), h_fin


def ssd_mixer(h, h0, w_in, conv_w, conv_b, dt_bias, a_log, d_skip, norm_g, w_out):
    bsz, t, _ = h.shape
    zxbcdt = h @ w_in
    z = zxbcdt[..., :SSD_INNER].astype(jnp.float32)
    xbc = jax.nn.silu(depthwise_conv_centred(zxbcdt[..., SSD_INNER:SSD_INNER + SSD_XBC], conv_w, conv_b)).astype(jnp.float32)
    dt_raw = zxbcdt[..., SSD_INNER + SSD_XBC:].astype(jnp.float32).reshape(bsz, t, 2, SSD_GROUPS, SSD_HPG)
    x = xbc[..., :SSD_INNER].reshape(bsz, t, SSD_GROUPS, SSD_HPG, SSD_HEADDIM)
    bm = xbc[..., SSD_INNER:SSD_INNER + SSD_GROUPS * SSD_STATE].reshape(bsz, t, SSD_GROUPS, SSD_STATE)
    cm = xbc[..., SSD_INNER + SSD_GROUPS * SSD_STATE:].reshape(bsz, t, SSD_GROUPS, SSD_STATE)
    dt = jax.nn.softplus(dt_raw + dt_bias.astype(jnp.float32).reshape(2, SSD_GROUPS, SSD_HPG))
    a = -jnp.exp(a_log.astype(jnp.float32)).reshape(2, SSD_GROUPS, SSD_HPG)
    y = d_skip.astype(jnp.float32).reshape(SSD_GROUPS, SSD_HPG)[..., None] * x
    fin = []
    for d in range(2):
        args = (x, dt[:, :, d], bm, cm)
        if d == 1:
            args = tuple(jnp.flip(v, axis=1) for v in args)
        hd0 = h0[:, d].astype(jnp.float32).reshape(bsz, SSD_GROUPS, SSD_HPG, SSD_HEADDIM, SSD_STATE)
        yd, hd = ssd_chunked(*args, a[d], hd0)
        y = y + (jnp.flip(yd, axis=1) if d == 1 else yd)
        fin.append(hd.reshape(bsz, SSD_HEADS, SSD_HEADDIM, SSD_STATE))
    y = rmsnorm(y.reshape(bsz, t, SSD_INNER) * jax.nn.silu(z), norm_g)
    out = y.astype(h.dtype) @ w_out
    return out, jnp.stack(fin, axis=1)


def expert_choice_moe(h, w_router, w_gate, w_up, w_down):
    bsz, t, _ = h.shape
    cap = EC_CAPACITY * t // N_EXPERTS
    aff = jax.nn.softmax((h @ w_router).astype(jnp.float32), axis=-1)
    g, idx = lax.top_k(jnp.swapaxes(aff, 1, 2), cap)
    xs = jax.vmap(lambda hb, ib: hb[ib])(h, idx)
    hid = jax.nn.silu(jnp.einsum('becd,edf->becf', xs, w_gate)) * jnp.einsum('becd,edf->becf', xs, w_up)
    ys = jnp.einsum('becf,efd->becd', hid, w_down) * g[..., None].astype(h.dtype)
    return jax.vmap(lambda yb, ib: jnp.zeros((t, D_MODEL), yb.dtype).at[ib.reshape(-1)].add(yb.reshape(-1, D_MODEL)))(ys, idx)


def trunk(x, cond, st_s5_re, st_s5_im, st_hg, st_ssd, W):
    lb_table = jnp.cumsum(jax.nn.softmax(W['hg_lb_logits'].astype(jnp.float32), axis=1), axis=1)
    lb_table = lb_table - lb_table[:, :1]
    silu_c = jax.nn.silu(cond)
    fr, fi, fh, fs = [], [], [], []
    for i in range(DEPTH):
        mod = (silu_c @ W['w_ada'][i] + W['b_ada'][i])[:, None, :]
        sh1, sc1, g1, sh2, sc2, g2 = jnp.split(mod, 6, axis=-1)
        h = rmsnorm(x, W['norm_mix'][i]) * (1.0 + sc1) + sh1
        kind, j = i % N_MIXERS, i // N_MIXERS
        if kind == 0:
            y, r, im = s5_mixer(h, st_s5_re[:, j], st_s5_im[:, j], W['s5_lam_re'][j], W['s5_lam_im'][j],
                                W['s5_log_dt'][j], W['s5_b_re'][j], W['s5_b_im'][j], W['s5_c_re'][j],
                                W['s5_c_im'][j], W['s5_d'][j], W['s5_w_glu'][j], W['s5_b_glu'][j])
            fr.append(r)
            fi.append(im)
        elif kind == 1:
            y, s = hgrn2_mixer(h, st_hg[:, j], lb_table[:, i], W['hg_w_qig'][j], W['hg_w_f'][j],
                               W['hg_b_f'][j], W['hg_norm'][j], W['hg_w_o'][j])
            fh.append(s)
        else:
            y, s = ssd_mixer(h, st_ssd[:, j], W['ssd_w_in'][j], W['ssd_conv_w'][j], W['ssd_conv_b'][j],
                             W['ssd_dt_bias'][j], W['ssd_a_log'][j], W['ssd_d'][j], W['ssd_norm'][j],
                             W['ssd_w_out'][j])
            fs.append(s)
        x = x + g1 * y
        h = rmsnorm(x, W['norm_ffn'][i]) * (1.0 + sc2) + sh2
        x = x + g2 * expert_choice_moe(h, W['moe_router'][i], W['moe_w_gate'][i], W['moe_w_up'][i], W['moe_w_down'][i])
    return (rmsnorm(x, W['norm_final']), jnp.stack(fr, axis=1), jnp.stack(fi, axis=1),
            jnp.stack(fh, axis=1), jnp.stack(fs, axis=1))


def setup_inputs(seed: int = 0) -> dict:
    key = jax.random.key(seed)
    ks = iter(jax.random.split(key, 48))

    def nrm(shape, scale):
        return scale * jax.random.normal(next(ks), shape, jnp.float32)

    def unif(shape, lo, hi):
        return jax.random.uniform(next(ks), shape, jnp.float32, lo, hi)

    D = D_MODEL
    s5_n = jnp.arange(S5_STATE, dtype=jnp.float32)
    ssd_dt = jnp.exp(unif((N_C_LAYERS, 2, SSD_HEADS), math.log(SSD_DT_MIN), math.log(SSD_DT_MAX)))
    return {
        'x_prompt': nrm((BATCH, SEQ, D), 1.0),
        'x_sample': nrm((DEC_BATCH, DEC_SEQ, D), 1.0),
        'state_s5_re': nrm((DEC_BATCH, N_A_LAYERS, 2, S5_GROUPS, S5_STATE), 0.1),
        'state_s5_im': nrm((DEC_BATCH, N_A_LAYERS, 2, S5_GROUPS, S5_STATE), 0.1),
        'state_hgrn': nrm((DEC_BATCH, N_B_LAYERS, 2, HG_HEADS, HG_DK, HG_DV), 0.5),
        'state_ssd': nrm((DEC_BATCH, N_C_LAYERS, 2, SSD_HEADS, SSD_HEADDIM, SSD_STATE), 0.1),
        'c': nrm((DEC_BATCH, D), 1.0),
        'c_ctx': nrm((D,), 1.0),
        'w_ada': nrm((DEPTH, D, 6 * D), 0.5 * D ** -0.5),
        'b_ada': nrm((DEPTH, 6 * D), 0.02),
        'norm_mix': 1.0 + nrm((DEPTH, D), 0.02),
        'norm_ffn': 1.0 + nrm((DEPTH, D), 0.02),
        'norm_final': 1.0 + nrm((D,), 0.02),
        's5_lam_re': -0.5 + nrm((N_A_LAYERS, 2, S5_GROUPS, S5_STATE), 0.01),
        's5_lam_im': math.pi * s5_n + nrm((N_A_LAYERS, 2, S5_GROUPS, S5_STATE), 0.01),
        's5_log_dt': unif((N_A_LAYERS, 2, S5_GROUPS), math.log(S5_DT_MIN), math.log(S5_DT_MAX)),
        's5_b_re': nrm((N_A_LAYERS, 2, S5_GROUPS, S5_STATE, S5_GROUP_CH), (2 * S5_GROUP_CH) ** -0.5),
        's5_b_im': nrm((N_A_LAYERS, 2, S5_GROUPS, S5_STATE, S5_GROUP_CH), (2 * S5_GROUP_CH) ** -0.5),
        's5_c_re': nrm((N_A_LAYERS, 2, S5_GROUPS, S5_GROUP_CH, S5_STATE), (2 * S5_STATE) ** -0.5),
        's5_c_im': nrm((N_A_LAYERS, 2, S5_GROUPS, S5_GROUP_CH, S5_STATE), (2 * S5_STATE) ** -0.5),
        's5_d': nrm((N_A_LAYERS, D), 1.0),
        's5_w_glu': nrm((N_A_LAYERS, D, 2 * D), D ** -0.5),
        's5_b_glu': nrm((N_A_LAYERS, 2 * D), 0.02),
        'hg_w_qig': nrm((N_B_LAYERS, D, 3 * D), D ** -0.5),
        'hg_w_f': nrm((N_B_LAYERS, 2, D, HG_HEADS * HG_DK), D ** -0.5),
        'hg_b_f': nrm((N_B_LAYERS, 2, HG_HEADS * HG_DK), 0.1),
        'hg_lb_logits': nrm((2, DEPTH, HG_HEADS * HG_DK), 0.5),
        'hg_norm': 1.0 + nrm((N_B_LAYERS, HG_DV), 0.02),
        'hg_w_o': nrm((N_B_LAYERS, D, D), D ** -0.5),
        'ssd_w_in': nrm((N_C_LAYERS, D, SSD_IN), D ** -0.5),
        'ssd_conv_w': nrm((N_C_LAYERS, SSD_CONV, SSD_XBC), SSD_CONV ** -0.5),
        'ssd_conv_b': nrm((N_C_LAYERS, SSD_XBC), 0.02),
        'ssd_dt_bias': ssd_dt + jnp.log(-jnp.expm1(-ssd_dt)),
        'ssd_a_log': jnp.log(unif((N_C_LAYERS, 2, SSD_HEADS), 1.0, 16.0)),
        'ssd_d': 1.0 + nrm((N_C_LAYERS, SSD_HEADS), 0.1),
        'ssd_norm': 1.0 + nrm((N_C_LAYERS, SSD_INNER), 0.02),
        'ssd_w_out': nrm((N_C_LAYERS, SSD_INNER, D), SSD_INNER ** -0.5),
        'moe_router': nrm((DEPTH, D, N_EXPERTS), D ** -0.5),
        'moe_w_gate': nrm((DEPTH, N_EXPERTS, D, EXPERT_FF), D ** -0.5),
        'moe_w_up': nrm((DEPTH, N_EXPERTS, D, EXPERT_FF), D ** -0.5),
        'moe_w_down': nrm((DEPTH, N_EXPERTS, EXPERT_FF, D), EXPERT_FF ** -0.5),
    }


def reference(x_prompt, x_sample, state_s5_re, state_s5_im, state_hgrn, state_ssd, c, c_ctx,
              w_ada, b_ada, norm_mix, norm_ffn, norm_final,
              s5_lam_re, s5_lam_im, s5_log_dt, s5_b_re, s5_b_im, s5_c_re, s5_c_im, s5_d, s5_w_glu, s5_b_glu,
              hg_w_qig, hg_w_f, hg_b_f, hg_lb_logits, hg_norm, hg_w_o,
              ssd_w_in, ssd_conv_w, ssd_conv_b, ssd_dt_bias, ssd_a_log, ssd_d, ssd_norm, ssd_w_out,
              moe_router, moe_w_gate, moe_w_up, moe_w_down):
    W = dict(w_ada=w_ada, b_ada=b_ada, norm_mix=norm_mix, norm_ffn=norm_ffn, norm_final=norm_final,
             s5_lam_re=s5_lam_re, s5_lam_im=s5_lam_im, s5_log_dt=s5_log_dt, s5_b_re=s5_b_re, s5_b_im=s5_b_im,
             s5_c_re=s5_c_re, s5_c_im=s5_c_im, s5_d=s5_d, s5_w_glu=s5_w_glu, s5_b_glu=s5_b_glu,
             hg_w_qig=hg_w_qig, hg_w_f=hg_w_f, hg_b_f=hg_b_f, hg_lb_logits=hg_lb_logits, hg_norm=hg_norm,
             hg_w_o=hg_w_o, ssd_w_in=ssd_w_in, ssd_conv_w=ssd_conv_w, ssd_conv_b=ssd_conv_b,
             ssd_dt_bias=ssd_dt_bias, ssd_a_log=ssd_a_log, ssd_d=ssd_d, ssd_norm=ssd_norm, ssd_w_out=ssd_w_out,
             moe_router=moe_router, moe_w_gate=moe_w_gate, moe_w_up=moe_w_up, moe_w_down=moe_w_down)
    bp = x_prompt.shape[0]
    z_s5 = jnp.zeros((bp, N_A_LAYERS, 2, S5_GROUPS, S5_STATE), jnp.float32)
    z_hg = jnp.zeros((bp, N_B_LAYERS, 2, HG_HEADS, HG_DK, HG_DV), jnp.float32)
    z_ssd = jnp.zeros((bp, N_C_LAYERS, 2, SSD_HEADS, SSD_HEADDIM, SSD_STATE), jnp.float32)
    y_prompt, new_s5_re, new_s5_im, new_hgrn, new_ssd = trunk(x_prompt, c_ctx[None, :], z_s5, z_s5, z_hg, z_ssd, W)
    x_lat = x_sample + grid_pos_embed(x_sample.shape[1]).astype(x_sample.dtype)[None]
    y_sample, _, _, _, _ = trunk(x_lat, c, state_s5_re, state_s5_im, state_hgrn, state_ssd, W)
    return (y_prompt, y_sample, new_s5_re, new_s5_im, new_hgrn, new_ssd)
```

```python
import numpy as np
from contextlib import ExitStack
import concourse.bass as bass
import concourse.mybir as mybir
from concourse.bass_utils import run_bass_kernel_spmd

F32 = mybir.dt.float32
BF16 = mybir.dt.bfloat16
I32 = mybir.dt.int32
AF = mybir.ActivationFunctionType
ALU = mybir.AluOpType
AX = mybir.AxisListType

SEM_LIMIT = 30000
T = 4096
D = 1024
NT = 32
NSEG = 16
SEG = 256
DEPTH = 4
NE = 16
FF = 2048
EPS = 1e-6


class Tok:
    __slots__ = ("w", "r")

    def __init__(self):
        self.w = None
        self.r = []


class Buf:
    def __init__(self, t):
        self.t = t
        self.k = Tok()

    def __getitem__(self, key):
        return self.t[key]


def _tok(x):
    return x.k if isinstance(x, Buf) else x


class Prog:
    ENG = ("pe", "act", "dve", "pool", "sp")

    def __init__(self, nc, es):
        self.nc = nc
        self.es = es
        self.streams = {e: [] for e in self.ENG}
        self.nsem = 0
        self.csem = {}
        self.ccnt = {}
        for e in ("pe", "act", "dve", "pool"):
            self.csem[e] = self.new_sem(e)
            self.ccnt[e] = 0
        self.dslots = {}
        for q, n in (("sp", 12), ("pool", 6), ("act", 4)):
            self.dslots[q] = [[self.new_sem("d" + q), 0] for _ in range(n)]
        self.dnext = {"sp": 0, "pool": 0, "act": 0}
        self.seen = {e: {} for e in self.ENG}
        self.ninst = 0
        self.pending = {}
        self.cprev = {}

    def new_sem(self, name):
        self.nsem += 1
        return self.es.enter_context(self.nc.semaphore(f"{name}_{self.nsem}"))

    def _collect(self, eng, reads, writes):
        evs = []
        for t in reads:
            t = _tok(t)
            if t.w is not None:
                evs.append(t.w)
        for t in writes:
            t = _tok(t)
            if t.w is not None:
                evs.append(t.w)
            evs.extend(t.r)
        seen = self.seen[eng]
        best = {}
        for (src, sem, val) in evs:
            if eng == "pe" and src == "pe":
                continue
            if src == "pe" and sem is self.csem["pe"] and val > self.ccnt["pe"]:
                raise RuntimeError("wait on a pending (unsignaled) PE event")
            k = id(sem)
            if seen.get(k, -1) >= val:
                continue
            if k not in best or best[k][1] < val:
                best[k] = (sem, val)
        for k, (sem, val) in best.items():
            seen[k] = val
        return list(best.values())

    def _commit(self, ev, reads, writes):
        for t in reads:
            _tok(t).r.append(ev)
        for t in writes:
            t = _tok(t)
            t.w = ev
            t.r = []

    def op(self, eng, fn, reads=(), writes=(), signal=True):
        waits = self._collect(eng, reads, writes)
        if self.ccnt[eng] + 1 > SEM_LIMIT and signal and not self.pending.get(eng, False):
            self.cprev[eng] = (self.csem[eng], self.ccnt[eng])
            self.csem[eng] = self.new_sem(eng)
            self.ccnt[eng] = 0
        self.pending[eng] = not signal
        if signal:
            self.ccnt[eng] += 1
            sem = self.csem[eng]
            ev = (eng, sem, self.ccnt[eng])
            self.streams[eng].append((waits, fn, (sem, 1)))
        else:
            assert eng == "pe"
            ev = (eng, self.csem[eng], self.ccnt[eng] + 1)
            self.streams[eng].append((waits, fn, None))
        self._commit(ev, reads, writes)
        self.ninst += 1
        return ev

    def _dma_common(self, q, fn, reads, writes):
        slots = self.dslots[q]
        i = self.dnext[q]
        self.dnext[q] = (i + 1) % len(slots)
        slot = slots[i]
        waits = self._collect(q, reads, writes)
        sem, cnt = slot
        if cnt > 0:
            k = id(sem)
            if self.seen[q].get(k, -1) < cnt:
                self.seen[q][k] = cnt
                waits.append((sem, cnt))
        if cnt + 16 > SEM_LIMIT:
            sem = self.new_sem("d" + q)
            cnt = 0
        cnt += 16
        slot[0], slot[1] = sem, cnt
        ev = ("dma", sem, cnt)
        self.streams[q].append((waits, fn, (sem, 16)))
        self._commit(ev, reads, writes)
        self.ninst += 1
        return ev

    def dma(self, q, out, in_, reads=(), writes=(), **kw):
        return self._dma_common(q, lambda e: e.dma_start(out=out, in_=in_, **kw), reads, writes)

    def idma(self, out, out_off, in_, in_off, reads=(), writes=(), **kw):
        return self._dma_common(
            "pool",
            lambda e: e.indirect_dma_start(out=out, out_offset=out_off, in_=in_, in_offset=in_off, **kw),
            reads, writes)

    def barrier(self):
        evs = []
        for e in ("pe", "act", "dve", "pool"):
            if self.ccnt[e] > 0:
                evs.append((self.csem[e], self.ccnt[e]))
            elif e in self.cprev:
                evs.append(self.cprev[e])
        for q in self.dslots:
            for sem, cnt in self.dslots[q]:
                if cnt > 0:
                    evs.append((sem, cnt))
        for eng in self.ENG:
            waits = []
            for sem, val in evs:
                k = id(sem)
                if self.seen[eng].get(k, -1) >= val:
                    continue
                if eng == "pe" and sem is self.csem["pe"]:
                    continue
                self.seen[eng][k] = val
                waits.append((sem, val))
            if waits:
                self.streams[eng].append((waits, None, None))

    def final_wait(self, eng, toks):
        waits = self._collect(eng, toks, ())
        self.streams[eng].append((waits, None, None))

    def mm(self, out, lhsT, rhs, start=True, stop=True, reads=(), writes=(), signal=True, **kw):
        return self.op("pe", lambda e: e.matmul(out, lhsT=lhsT, rhs=rhs, start=start, stop=stop, **kw),
                       reads, writes, signal)

    def tr(self, out, in_, ident, reads=(), writes=(), signal=True):
        return self.op("pe", lambda e: e.transpose(out, in_, ident), reads, writes, signal)

    def act(self, out, in_, func, reads=(), writes=(), **kw):
        return self.op("act", lambda e: e.activation(out=out, in_=in_, func=func, **kw), reads, writes)

    def tt(self, eng, out, in0, in1, op, reads=(), writes=()):
        return self.op(eng, lambda e: e.tensor_tensor(out=out, in0=in0, in1=in1, op=op), reads, writes)

    def ts(self, eng, out, in0, s1, s2, op0, op1=None, reads=(), writes=(), accum_out=None):
        if op1 is None:
            op1 = ALU.bypass
        if accum_out is None:
            return self.op(eng, lambda e: e.tensor_scalar(out=out, in0=in0, scalar1=s1, scalar2=s2, op0=op0, op1=op1),
                           reads, writes)
        return self.op(eng, lambda e: e.tensor_scalar(out=out, in0=in0, scalar1=s1, scalar2=s2, op0=op0, op1=op1,
                                                       accum_out=accum_out), reads, writes)

    def stt(self, out, in0, scalar, in1, op0, op1, reads=(), writes=()):
        return self.op("dve", lambda e: e.scalar_tensor_tensor(out=out, in0=in0, scalar=scalar, in1=in1, op0=op0, op1=op1),
                       reads, writes)

    def cp(self, eng, out, in_, reads=(), writes=()):
        if eng == "act":
            return self.op("act", lambda e: e.copy(out=out, in_=in_), reads, writes)
        return self.op(eng, lambda e: e.tensor_copy(out=out, in_=in_), reads, writes)

    def memset(self, eng, ap, val, writes=()):
        return self.op(eng, lambda e: e.memset(ap, val), (), writes)

    def scan(self, out, d0, d1, init, op0, op1, reads=(), writes=()):
        return self.op("dve", lambda e: e.tensor_tensor_scan(out=out, data0=d0, data1=d1, initial=init, op0=op0, op1=op1),
                       reads, writes)

    def emit(self):
        nc = self.nc
        streams = self.streams
        with nc.Block() as block:
            def run(e, lst):
                for waits, fn, inc in lst:
                    for sem, val in waits:
                        e.wait_ge(sem, val)
                    if fn is None:
                        continue
                    ins = fn(e)
                    if inc is not None:
                        ins.then_inc(inc[0], inc[1])

            @block.sync
            def _(e):
                run(e, streams["sp"])

            @block.tensor
            def _(e):
                run(e, streams["pe"])

            @block.scalar
            def _(e):
                run(e, streams["act"])

            @block.vector
            def _(e):
                run(e, streams["dve"])

            @block.gpsimd
            def _(e):
                run(e, streams["pool"])


class Ring:
    def __init__(self, bufs):
        self.bufs = bufs
        self.i = 0

    def next(self):
        b = self.bufs[self.i]
        self.i = (self.i + 1) % len(self.bufs)
        return b


class Builder:
    def __init__(self, cfg):
        self.cfg = cfg
        self.nc = bass.Bass("TRN2", target_bir_lowering=False)
        self.din = {}
        self.dout = {}

    def inp(self, name, shape, dt=F32):
        self.din[name] = self.nc.dram_tensor(name, list(shape), dt, kind="ExternalInput")
        return self.din[name]

    def outp(self, name, shape, dt=F32):
        self.dout[name] = self.nc.dram_tensor(name, list(shape), dt, kind="ExternalOutput")
        return self.dout[name]

    def scratch(self, name, shape, dt=F32):
        return self.nc.dram_tensor(name, list(shape), dt, kind="Internal")

    def _nm(self, name):
        self._cnt = getattr(self, "_cnt", 0) + 1
        return f"s{self._cnt}_{name}"

    def sb(self, st, name, shape, dt=F32):
        return Buf(st.enter_context(self.nc.sbuf_tensor(self._nm(name), list(shape), dt)))

    def ps(self, st, name, shape, dt=F32):
        return Buf(st.enter_context(self.nc.psum_tensor(self._nm(name), list(shape), dt)))

    def ring(self, st, name, n, shape, dt=F32, psum=False):
        f = self.ps if psum else self.sb
        return Ring([f(st, f"{name}{i}", shape, dt) for i in range(n)])

    def build(self):
        nc = self.nc
        cfg = self.cfg
        layers = cfg.get("layers", list(range(DEPTH)))
        x0 = self.inp("x0", [T, D])
        pos = self.inp("pos", [T, D])
        condT = self.inp("condT", [128, 8])
        flag = self.inp("flag", [128, 1])
        capv = self.inp("capv", [16, 16])
        ident_d = self.inp("ident", [128, 128])
        iota_d = self.inp("iota512", [128, 512])
        tokid_d = self.inp("tokid", [128, NT])
        w_ada = self.inp("w_ada", [DEPTH, D, 6 * D])
        b_ada = self.inp("b_ada", [DEPTH, 6 * D])
        norm_mix = self.inp("norm_mix", [DEPTH, D])
        norm_ffn = self.inp("norm_ffn", [DEPTH, D])
        norm_final = self.inp("norm_final", [1, D])
        moe_router = self.inp("moe_router", [DEPTH, D, NE])
        moe_w_gate = self.inp("moe_w_gate", [DEPTH, NE, D, FF])
        moe_w_up = self.inp("moe_w_up", [DEPTH, NE, D, FF])
        moe_w_down = self.inp("moe_w_down", [DEPTH, NE, FF, D])
        y_out = self.outp("y", [T, D])

        self.X = self.scratch("Xres", [T, D])
        self.Xk = [Tok() for _ in range(NT)]
        self.H2 = self.scratch("H2", [T, D], BF16)
        self.H2k = Tok()
        self.modrow = self.scratch("modrow", [1, 6 * D])
        self.HT = self.scratch("HT", [D, T], BF16)
        self.HTk = Tok()
        self.outk = []
        self.inp("s5_lre", [2, 128, 2, 32]); self.inp("s5_lim", [2, 128, 2, 32]); self.inp("s5_ldt", [2, 128, 2, 32])
        self.inp("s5_h0r", [2, 128, 2, 32]); self.inp("s5_h0i", [2, 128, 2, 32]); self.inp("s5_dsk", [2, 128, 8])
        self.inp("s5_bre", [2, 128, 2, 8, 128]); self.inp("s5_bim", [2, 128, 2, 8, 128])
        self.inp("s5_cre", [2, 128, 2, 32, 128]); self.inp("s5_cim", [2, 128, 2, 32, 128])
        self.inp("s5_w_glu", [2, D, 2 * D]); self.inp("s5_b_glu", [2, 2 * D])
        self.inp("tau256", [128, 256])
        self.outp("o_s5re", [NSEG, 2, 2, 64, 64]); self.outp("o_s5im", [NSEG, 2, 2, 64, 64])
        self.inp("hg_w_qig", [1, D, 3 * D]); self.inp("hg_w_f", [1, 2, D, D]); self.inp("hg_w_o", [1, D, D])
        self.inp("hg_bfT", [1, 128, 2, 8]); self.inp("hg_gn", [1, 128, 1]); self.inp("hg_lbl", [128, 2, 4, 8])
        self.inp("hg_s0", [2, 8, 128, 128])
        self.inp("hg_maskF", [128, 128]); self.inp("hg_maskB", [128, 128]); self.inp("hg_rmask", [128, 512])
        self.outp("o_hg", [NSEG, 1, 2, 8, 128, 128])
        self.OF = self.scratch("OF", [8, 128, T])
        self.OFk = Tok()
        self.inp("ssd_w_in", [1, D, 5184]); self.inp("ssd_w_out", [1, 2 * D, D])
        self.inp("ssd_cwT", [1, 128, 24, 5]); self.inp("ssd_cbT", [1, 128, 24])
        self.inp("ssd_dtb", [1, 64]); self.inp("ssd_alog", [1, 64]); self.inp("ssd_d", [1, 32]); self.inp("ssd_norm", [1, 2 * D])
        self.inp("ssd_h0", [128, 2, 4, 512])
        for nm in ("TriF", "UF", "TriB", "UB", "negF", "negB", "Ones0", "Ones1"):
            self.inp("ssd_" + nm, [128, 128])
        self.outp("o_ssd", [NSEG, 1, 2, 32, 64, 128])
        self.XBC = self.scratch("XBC", [3 * D, T]); self.XBCk = Tok()
        self.XC = self.scratch("XC", [3 * D, T], BF16); self.XCk = Tok()
        self.YF = self.scratch("YF", [T, 2 * D]); self.YFk = Tok()
        self.modk = Tok()

        with ExitStack() as es:
            P = Prog(nc, es)
            self.P = P
            g = ExitStack()
            es.enter_context(g)
            self.ident = self.sb(g, "ident", [128, 128])
            self.identb = self.sb(g, "identb", [128, 128], BF16)
            self.flag = self.sb(g, "flag", [128, 1])
            self.mod = self.sb(g, "mod", [128, 6 * D])
            self.psum = self.ring(g, "psb", 8, [128, 512], F32, psum=True)
            self.epsb = self.sb(g, "epsb", [128, 1])
            P.memset("dve", self.epsb[:], EPS, writes=[self.epsb])
            P.dma("sp", self.ident[:], ident_d[:, :], writes=[self.ident])
            P.dma("sp", self.flag[:], flag[:, :], writes=[self.flag])
            P.cp("dve", self.identb[:], self.ident[:], reads=[self.ident], writes=[self.identb])

            if cfg.get("init", True):
                self.phase_init(x0, pos)
            else:
                self.phase_init(x0, None)
            for i in layers:
                self.phase_mod(i, condT, w_ada, b_ada, norm_mix, norm_ffn)
                if cfg.get("mixer", True):
                    self.phase_mixer(i)
                if cfg.get("moe", True):
                    self.phase_moe(i, moe_router, moe_w_gate, moe_w_up, moe_w_down, capv, iota_d, tokid_d)
            self.phase_final(norm_final, y_out, final_norm=cfg.get("final_norm", True))
            P.emit()
        return nc

    def phase_init(self, x0, pos):
        P = self.P
        with ExitStack() as st:
            ra = self.ring(st, "ia", 3, [128, D])
            rb = self.ring(st, "ib", 3, [128, D])
            for tt in range(NT):
                a = ra.next()
                P.dma("sp", a[:], x0[tt * 128:(tt + 1) * 128, :], writes=[a])
                if pos is not None:
                    b = rb.next()
                    P.dma("sp", b[:], pos[tt * 128:(tt + 1) * 128, :], writes=[b])
                    P.tt("dve", a[:], a[:], b[:], ALU.add, reads=[a, b], writes=[a])
                P.dma("sp", self.X[tt * 128:(tt + 1) * 128, :], a[:], reads=[a], writes=[self.Xk[tt]])
            P.barrier()

    def phase_mod(self, i, condT, w_ada, b_ada, norm_mix, norm_ffn):
        P = self.P
        with ExitStack() as st:
            sc = self.sb(st, "m_sc", [128, 8])
            sg = self.sb(st, "m_sg", [128, 8])
            brow = self.sb(st, "m_brow", [1, 6 * D])
            mrow = self.sb(st, "m_mrow", [1, 6 * D])
            wr = self.ring(st, "m_w", 3, [128, 3 * D])
            nrm = self.sb(st, "m_nrm", [128, 2 * D])
            P.dma("sp", sc[:], condT[:, :], writes=[sc])
            P.dma("sp", brow[:], b_ada[i:i + 1, :], writes=[brow])
            P.act(sg[:], sc[:], AF.Sigmoid, reads=[sc], writes=[sg])
            P.tt("dve", sc[:], sc[:], sg[:], ALU.mult, reads=[sc, sg], writes=[sc])
            P.dma("sp", nrm[:, 0:D], norm_mix[i:i + 1, :].broadcast_to([128, D]), writes=[nrm])
            P.dma("sp", nrm[:, D:2 * D], norm_ffn[i:i + 1, :].broadcast_to([128, D]), writes=[nrm])
            for half in range(2):
                banks = [self.psum.next() for _ in range(6)]
                for kc in range(8):
                    w = wr.next()
                    P.dma("sp", w[:], w_ada[i, kc * 128:(kc + 1) * 128, half * 3 * D:(half + 1) * 3 * D], writes=[w])
                    for cb in range(6):
                        P.mm(banks[cb][0:1, :], lhsT=sc[:, kc:kc + 1], rhs=w[:, cb * 512:(cb + 1) * 512],
                             start=(kc == 0), stop=(kc == 7), reads=[sc, w], writes=[banks[cb]], signal=(kc == 7 or cb == 5))
                for cb in range(6):
                    c0 = half * 3 * D + cb * 512
                    P.tt("dve", mrow[0:1, c0:c0 + 512], banks[cb][0:1, :], brow[0:1, c0:c0 + 512], ALU.add,
                         reads=[banks[cb], brow], writes=[mrow])
            P.dma("sp", self.modrow[:, :], mrow[:], reads=[mrow], writes=[self.modk])
            P.dma("sp", self.mod[:], self.modrow[0:1, :].broadcast_to([128, 6 * D]), reads=[self.modk], writes=[self.mod])
            m = self.mod
            P.stt(m[:, D:2 * D], m[:, D:2 * D], 1.0, nrm[:, 0:D], ALU.add, ALU.mult, reads=[m, nrm], writes=[m])
            P.stt(m[:, 4 * D:5 * D], m[:, 4 * D:5 * D], 1.0, nrm[:, D:2 * D], ALU.add, ALU.mult, reads=[m, nrm], writes=[m])
            P.barrier()

    def modA(self, sub):
        return self.mod[:, (3 * sub + 1) * D:(3 * sub + 2) * D]

    def modB(self, sub):
        return self.mod[:, (3 * sub) * D:(3 * sub + 1) * D]

    def modG(self, sub):
        return self.mod[:, (3 * sub + 2) * D:(3 * sub + 3) * D]

    def norm_tile(self, tt, sub, xt, h, stat):
        P = self.P
        P.dma("sp", xt[:], self.X[tt * 128:(tt + 1) * 128, :], reads=[self.Xk[tt]], writes=[xt])
        P.act(h[:], xt[:], AF.Square, reads=[xt], writes=[h, stat], accum_out=stat[:, 0:1])
        P.act(stat[:, 1:2], stat[:, 0:1], AF.Ln, reads=[stat], writes=[stat], scale=1.0 / D, bias=self.epsb[:, 0:1])
        P.act(stat[:, 2:3], stat[:, 1:2], AF.Exp, reads=[stat], writes=[stat], scale=-0.5)
        P.stt(h[:], xt[:], stat[:, 2:3], self.modA(sub), ALU.mult, ALU.mult, reads=[xt, stat, self.mod], writes=[h])
        P.tt("pool", h[:], h[:], self.modB(sub), ALU.add, reads=[h, self.mod], writes=[h])

    def phase_mixer(self, i):
        self.build_hT()
        kind, j = i % 3, i // 3
        if kind == 0:
            self.phase_s5(i, j)
        elif kind == 1:
            self.phase_hgrn(i, j)
        else:
            self.phase_ssd(i, j)

    def build_hT(self):
        P = self.P
        HTv = self.HT.ap().rearrange("(c p) t -> p c t", p=128)
        with ExitStack() as st:
            rx = self.ring(st, "p_x", 2, [128, D])
            rh = self.ring(st, "p_h", 2, [128, D])
            rhb = self.ring(st, "p_hb", 2, [128, D], BF16)
            rhT = self.ring(st, "p_hT", 2, [128, 8, 512], BF16)
            rstat = self.ring(st, "p_st", 3, [128, 8])
            for tt in range(NT):
                xt, h, hb, stat = rx.next(), rh.next(), rhb.next(), rstat.next()
                if tt % 4 == 0:
                    hT = rhT.next()
                self.norm_tile(tt, 0, xt, h, stat)
                P.cp("act", hb[:], h[:], reads=[h], writes=[hb])
                pb = self.psum.next()
                for c in range(8):
                    P.tr(self.psbf(pb)[:, c * 128:(c + 1) * 128], hb[:, c * 128:(c + 1) * 128], self.identb[:],
                         reads=[hb, self.identb], writes=[pb], signal=(c == 7))
                t4 = tt % 4
                P.cp("dve", hT[:, :, t4 * 128:(t4 + 1) * 128],
                     self.psbf(pb)[:, 0:1024].rearrange("p (c t) -> p c t", t=128), reads=[pb], writes=[hT])
                if self.cfg.get("s5_dbg") and tt == 0:
                    self.dbg("d_h", h[:], [128, D], h); self.dbg("d_hb", hb[:], [128, D], hb, BF16)
                    self.dbg("d_stat", stat[:], [128, 8], stat); self.dbg("d_xt", xt[:], [128, D], xt)
                    self.dbg("d_modA", self.modA(0), [128, D], self.mod); self.dbg("d_modB", self.modB(0), [128, D], self.mod)
                if t4 == 3:
                    t0 = (tt - 3) * 128
                    P.dma("sp", HTv[:, :, t0:t0 + 512], hT[:], reads=[hT], writes=[self.HTk])
                    if self.cfg.get("s5_dbg") and tt == 3:
                        self.dbg("d_hT", hT[:], [128, 8, 512], hT, BF16)
            P.barrier()

    def phase_s5(self, i, j):
        P = self.P
        di = self.din
        TWO_PI = 2.0 * np.pi
        self._sc = {}
        with ExitStack() as st:
            yg_all = self.sb(st, "yg_all", [128, 8, T], BF16)
            so_re = self.sb(st, "so_re", [128, NSEG, 2, 32])
            so_im = self.sb(st, "so_im", [128, NSEG, 2, 32])
            with ExitStack() as s1:
                lre = self.sb(s1, "lre", [128, 2, 32]); lim = self.sb(s1, "lim", [128, 2, 32])
                ldt = self.sb(s1, "ldt", [128, 2, 32])
                h0r = self.sb(s1, "h0r", [128, 2, 32]); h0i = self.sb(s1, "h0i", [128, 2, 32])
                dsk = self.sb(s1, "dsk", [128, 8])
                rr = self.sb(s1, "rr", [128, 2, 32]); th = self.sb(s1, "th", [128, 2, 32])
                cr = self.sb(s1, "cr", [128, 2, 32]); ci = self.sb(s1, "ci", [128, 2, 32])
                tmp = [self.sb(s1, f"tmp{k}", [128, 2, 32]) for k in range(6)]
                P.dma("sp", lre[:], di["s5_lre"][j], writes=[lre])
                P.dma("sp", lim[:], di["s5_lim"][j], writes=[lim])
                P.dma("sp", ldt[:], di["s5_ldt"][j], writes=[ldt])
                P.dma("sp", h0r[:], di["s5_h0r"][j], writes=[h0r])
                P.dma("sp", h0i[:], di["s5_h0i"][j], writes=[h0i])
                P.dma("sp", dsk[:], di["s5_dsk"][j], writes=[dsk])
                dt_ = tmp[0]
                P.act(dt_[:], ldt[:], AF.Exp, reads=[ldt], writes=[dt_])
                P.tt("dve", rr[:], lre[:], dt_[:], ALU.mult, reads=[lre, dt_], writes=[rr])
                P.act(rr[:], rr[:], AF.Exp, reads=[rr], writes=[rr])
                P.tt("dve", th[:], lim[:], dt_[:], ALU.mult, reads=[lim, dt_], writes=[th])
                P.ts("dve", th[:], th[:], 1.0 / TWO_PI, None, ALU.mult, reads=[th], writes=[th])
                cs, sn = tmp[1], tmp[2]
                self.sincos_turns(s1, th, [128, 2, 32], cs, sn, "pp")
                nr, ni, den = tmp[3], tmp[4], tmp[5]
                P.tt("dve", nr[:], rr[:], cs[:], ALU.mult, reads=[rr, cs], writes=[nr])
                P.ts("dve", nr[:], nr[:], -1.0, None, ALU.add, reads=[nr], writes=[nr])
                P.tt("dve", ni[:], rr[:], sn[:], ALU.mult, reads=[rr, sn], writes=[ni])
                P.tt("dve", den[:], lre[:], lre[:], ALU.mult, reads=[lre], writes=[den])
                P.tt("dve", cs[:], lim[:], lim[:], ALU.mult, reads=[lim], writes=[cs])
                P.tt("dve", den[:], den[:], cs[:], ALU.add, reads=[den, cs], writes=[den])
                P.op("dve", lambda e: e.reciprocal(out=den[:], in_=den[:]), reads=[den], writes=[den])
                P.tt("dve", cr[:], nr[:], lre[:], ALU.mult, reads=[nr, lre], writes=[cr])
                P.tt("dve", cs[:], ni[:], lim[:], ALU.mult, reads=[ni, lim], writes=[cs])
                P.tt("dve", cr[:], cr[:], cs[:], ALU.add, reads=[cr, cs], writes=[cr])
                P.tt("dve", cr[:], cr[:], den[:], ALU.mult, reads=[cr, den], writes=[cr])
                P.tt("dve", ci[:], ni[:], lre[:], ALU.mult, reads=[ni, lre], writes=[ci])
                P.tt("dve", cs[:], nr[:], lim[:], ALU.mult, reads=[nr, lim], writes=[cs])
                P.tt("dve", ci[:], ci[:], cs[:], ALU.subtract, reads=[ci, cs], writes=[ci])
                P.tt("dve", ci[:], ci[:], den[:], ALU.mult, reads=[ci, den], writes=[ci])
                BT = self.sb(s1, "BT", [128, 2, 2, 8, 128], BF16)
                with ExitStack() as s2:
                    bre = self.sb(s2, "bre", [128, 8, 128]); bim = self.sb(s2, "bim", [128, 8, 128])
                    o1 = self.sb(s2, "o1", [128, 8, 128]); o2 = self.sb(s2, "o2", [128, 8, 128])
                    ob = self.sb(s2, "ob", [128, 8, 128], BF16)
                    for d in range(2):
                        P.dma("sp", bre[:], di["s5_bre"][j, :, d], writes=[bre])
                        P.dma("sp", bim[:], di["s5_bim"][j, :, d], writes=[bim])
                        v4 = lambda b: b[:].rearrange("p c (q x) -> p c q x", x=32)
                        bc = lambda t: t[:, d, :].rearrange("p (c q) -> p c q", q=4).unsqueeze(3).to_broadcast([128, 8, 4, 32])
                        for reim in range(2):
                            a, b_ = (bre, bim) if reim == 0 else (bim, bre)
                            P.tt("dve", v4(o1), v4(a), bc(cr), ALU.mult, reads=[a, cr], writes=[o1])
                            P.tt("dve", v4(o2), v4(b_), bc(ci), ALU.mult, reads=[b_, ci], writes=[o2])
                            P.tt("dve", ob[:], o1[:], o2[:], ALU.subtract if reim == 0 else ALU.add,
                                 reads=[o1, o2], writes=[ob])
                            for c in range(8):
                                pb = self.psum.next()
                                P.tr(self.psbf(pb)[:, 0:128], ob[:, c, :], self.identb[:], reads=[ob, self.identb], writes=[pb])
                                P.cp("act", BT[:, d, reim, c, :], self.psbf(pb)[:, 0:128], reads=[pb], writes=[BT])
                    P.barrier()
                ycs = self.ring(s1, "y_c", 1, [128, T])
                hTc_r = self.ring(s1, "hTc", 2, [128, T], BF16)
                Cre_r = self.ring(s1, "Cre", 2, [128, 2, 4, 128]); Cim_r = self.ring(s1, "Cim", 2, [128, 2, 4, 128])
                CC = self.sb(s1, "CC", [128, 512]); SSp = self.sb(s1, "SSp", [128, 512]); SSn = self.sb(s1, "SSn", [128, 512])
                tau = self.sb(s1, "tau", [128, 256]); ang = self.sb(s1, "ang", [128, 256])
                rt = self.sb(s1, "rt", [128, 256])
                P.dma("sp", tau[:], di["tau256"][:, :], writes=[tau])
                rbu = self.ring(s1, "bu", 2, [128, 512])
                rA = self.ring(s1, "rA", 2, [128, 512]); rB = self.ring(s1, "rB", 2, [128, 512])
                rv = self.ring(s1, "rv", 2, [128, 512]); rg = self.ring(s1, "rg", 2, [128, 512])
                rh32 = self.ring(s1, "h32", 2, [128, 512])
                rcar = self.ring(s1, "car", 3, [128, 2])
                HTv = self.HT.ap().rearrange("(c p) t -> p c t", p=128)
                swp = lambda b: b[:].rearrange("p (two x) -> p two x", two=2)[:, ::-1, :]
                v3 = lambda b: b[:].rearrange("p (two x) -> p two x", two=2)
                for c in range(8):
                    y_c, hTc, Cre, Cim = ycs.next(), hTc_r.next(), Cre_r.next(), Cim_r.next()
                    P.dma("sp", hTc[:], HTv[:, c, :], reads=[self.HTk], writes=[hTc])
                    P.dma("sp", Cre[:], di["s5_cre"][j, :, :, 4 * c:4 * c + 4, :], writes=[Cre])
                    P.dma("sp", Cim[:], di["s5_cim"][j, :, :, 4 * c:4 * c + 4, :], writes=[Cim])
                    P.ts("pool", Cim[:], Cim[:], -1.0, None, ALU.mult, reads=[Cim], writes=[Cim])
                    P.act(y_c[:], hTc[:], AF.Identity, reads=[hTc, dsk], writes=[y_c], scale=dsk[:, c:c + 1])
                    for d in range(2):
                        for q in range(4):
                            stx = 4 * c + q
                            P.ts("dve", ang[:], tau[:], th[:, d, stx:stx + 1], None, ALU.mult, reads=[tau, th], writes=[ang])
                            self.sincos_turns(s1, ang, [128, 256], CC[:, 0:256], SSp[:, 0:256], f"t{c}{d}{q}",
                                              outk=[CC, SSp])
                            P.cp("pool", CC[:, 256:512], CC[:, 0:256], reads=[CC], writes=[CC])
                            P.ts("pool", SSp[:, 256:512], SSp[:, 0:256], -1.0, None, ALU.mult, reads=[SSp], writes=[SSp])
                            P.ts("pool", SSn[:], SSp[:], -1.0, None, ALU.mult, reads=[SSp], writes=[SSn])
                            P.ts("dve", rt[:], tau[:], 0.0, rr[:, d, stx:stx + 1], ALU.mult, ALU.add, reads=[tau, rr], writes=[rt])
                            car = rcar.next()
                            P.cp("dve", car[:, 0:1], h0r[:, d, stx:stx + 1], reads=[h0r], writes=[car])
                            P.cp("dve", car[:, 1:2], h0i[:, d, stx:stx + 1], reads=[h0i], writes=[car])
                            segs = range(NSEG) if d == 0 else range(NSEG - 1, -1, -1)
                            for sgi in segs:
                                t0 = sgi * SEG
                                u = hTc[32 * q:32 * q + 32, t0:t0 + SEG]
                                ysl = y_c[:, t0:t0 + SEG]
                                if d == 1:
                                    u = u[:, ::-1]
                                    ysl = ysl[:, ::-1]
                                pb = self.psum.next()
                                P.mm(pb[:, 0:256], lhsT=BT[32 * q:32 * q + 32, d, 0, c, :], rhs=u, reads=[BT, hTc],
                                     writes=[pb], signal=False, tile_position=(32 * q, 0))
                                P.mm(pb[:, 256:512], lhsT=BT[32 * q:32 * q + 32, d, 1, c, :], rhs=u, reads=[BT, hTc],
                                     writes=[pb], tile_position=(32 * q, 0))
                                bu, A_, B_, v, g_, h32 = rbu.next(), rA.next(), rB.next(), rv.next(), rg.next(), rh32.next()
                                P.cp("act", bu[:], pb[:, :], reads=[pb], writes=[bu])
                                P.tt("dve", A_[:], bu[:], CC[:], ALU.mult, reads=[bu, CC], writes=[A_])
                                P.tt("pool", v3(B_), swp(bu), v3(SSp), ALU.mult, reads=[bu, SSp], writes=[B_])
                                P.tt("pool", v[:], A_[:], B_[:], ALU.add, reads=[A_, B_], writes=[v])
                                P.scan(g_[:, 0:256], rt[:], v[:, 0:256], car[:, 0:1], ALU.mult, ALU.add,
                                       reads=[rt, v, car], writes=[g_])
                                P.scan(g_[:, 256:512], rt[:], v[:, 256:512], car[:, 1:2], ALU.mult, ALU.add,
                                       reads=[rt, v, car], writes=[g_])
                                P.tt("dve", A_[:], g_[:], CC[:], ALU.mult, reads=[g_, CC], writes=[A_])
                                P.tt("pool", v3(B_), swp(g_), v3(SSn), ALU.mult, reads=[g_, SSn], writes=[B_])
                                P.tt("pool", h32[:], A_[:], B_[:], ALU.add, reads=[A_, B_], writes=[h32])
                                car = rcar.next()
                                P.cp("act", so_re[:, sgi, d, stx:stx + 1], h32[:, 255:256], reads=[h32], writes=[so_re])
                                P.cp("act", so_im[:, sgi, d, stx:stx + 1], h32[:, 511:512], reads=[h32], writes=[so_im])
                                P.ts("dve", car[:, 0:1], h32[:, 255:256], self.flag[:, 0:1], None, ALU.mult,
                                     reads=[h32, self.flag], writes=[car])
                                P.ts("dve", car[:, 1:2], h32[:, 511:512], self.flag[:, 0:1], None, ALU.mult,
                                     reads=[h32, self.flag], writes=[car])
                                py = self.psum.next()
                                P.mm(py[:, 0:256], lhsT=Cre[:, d, q, :], rhs=h32[:, 0:256], start=True, stop=False,
                                     reads=[Cre, h32], writes=[py], signal=False)
                                P.mm(py[:, 0:256], lhsT=Cim[:, d, q, :], rhs=h32[:, 256:512], start=False, stop=True,
                                     reads=[Cim, h32], writes=[py])
                                P.tt("dve", ysl, py[:, 0:256], ysl, ALU.add, reads=[py, y_c], writes=[y_c])
                                if self.cfg.get("s5_dbg"):
                                    self.dbg("d_cr", cr[:], [128, 2, 32], cr); self.dbg("d_ci", ci[:], [128, 2, 32], ci)
                                    self.dbg("d_th", th[:], [128, 2, 32], th); self.dbg("d_rr", rr[:], [128, 2, 32], rr)
                                    self.dbg("d_BT", BT[:, 0, 0, 0, :], [128, 128], BT, BF16)
                                    self.dbg("d_CC", CC[:], [128, 512], CC); self.dbg("d_SSp", SSp[:], [128, 512], SSp)
                                    self.dbg("d_rt", rt[:], [128, 256], rt); self.dbg("d_bu", bu[:], [128, 512], bu)
                                    self.dbg("d_v", v[:], [128, 512], v); self.dbg("d_g", g_[:], [128, 512], g_)
                                    self.dbg("d_h32", h32[:], [128, 512], h32); self.dbg("d_yc", y_c[:, 0:256], [128, 256], y_c)
                                    self.dbg("d_hTc", hTc[:, 0:256], [128, 256], hTc, BF16)
                                    self.dbg("d_hTc2", hTc[:, :], [128, T], hTc, BF16)
                                    self.dbg("d_HT", self.HT.ap()[0:256, :], [256, T], self.HTk, BF16)
                                    P.barrier()
                                    return
                    P.act(yg_all[:, c, :], y_c[:], AF.Gelu_apprx_tanh, reads=[y_c], writes=[yg_all])
                if self.cfg.get("state_out", True):
                    rso = self.ring(s1, "sot", 3, [32, 128])
                    for (so, name) in ((so_re, "o_s5re"), (so_im, "o_s5im")):
                        for sgi in range(NSEG):
                            for d in range(2):
                                pb = self.psum.next()
                                P.tr(pb[0:32, 0:128], so[:, sgi, d, :], self.ident[:], reads=[so, self.ident], writes=[pb])
                                sot = rso.next()
                                P.cp("act", sot[:], pb[0:32, 0:128], reads=[pb], writes=[sot])
                                k = Tok()
                                P.dma("sp", self.dout[name][sgi, j, d].rearrange("(st gl) p -> st (gl p)", gl=2), sot[:],
                                      reads=[sot], writes=[k])
                                self.outk.append(k)
                P.barrier()
            with ExitStack() as s3:
                wglu = self.sb(s3, "wglu", [128, 8, 2 * D], BF16)
                bglu = self.sb(s3, "bglu", [128, 2 * D])
                for c in range(8):
                    P.dma("pool", wglu[:, c, :], di["s5_w_glu"][j, c * 128:(c + 1) * 128, :], writes=[wglu])
                P.dma("sp", bglu[:], di["s5_b_glu"][j:j + 1, :].broadcast_to([128, 2 * D]), writes=[bglu])
                rx = self.ring(s3, "g_x", 2, [128, D])
                ra = self.ring(s3, "g_a", 2, [128, D]); rb = self.ring(s3, "g_b", 2, [128, D])
                for tt in range(NT):
                    xt, ta, tb = rx.next(), ra.next(), rb.next()
                    P.dma("sp", xt[:], self.X[tt * 128:(tt + 1) * 128, :], reads=[self.Xk[tt]], writes=[xt])
                    banks = [self.psum.next() for _ in range(4)]
                    for cb in range(4):
                        for c in range(8):
                            P.mm(banks[cb][:, :], lhsT=yg_all[:, c, tt * 128:(tt + 1) * 128], rhs=wglu[:, c, cb * 512:(cb + 1) * 512],
                                 start=(c == 0), stop=(c == 7), reads=[yg_all, wglu], writes=[banks[cb]], signal=(c == 7))
                    for hf in range(2):
                        sl = slice(hf * 512, (hf + 1) * 512)
                        sl2 = slice(D + hf * 512, D + (hf + 1) * 512)
                        P.tt("dve", tb[:, sl], banks[2 + hf][:, :], bglu[:, sl2], ALU.add, reads=[banks[2 + hf], bglu], writes=[tb])
                        P.act(tb[:, sl], tb[:, sl], AF.Sigmoid, reads=[tb], writes=[tb])
                        P.tt("dve", ta[:, sl], banks[hf][:, :], bglu[:, sl], ALU.add, reads=[banks[hf], bglu], writes=[ta])
                        P.tt("pool", ta[:, sl], ta[:, sl], tb[:, sl], ALU.mult, reads=[ta, tb], writes=[ta])
                    P.tt("pool", ta[:], ta[:], self.modG(0), ALU.mult, reads=[ta, self.mod], writes=[ta])
                    P.tt("dve", xt[:], xt[:], ta[:], ALU.add, reads=[xt, ta], writes=[xt])
                    P.dma("sp", self.X[tt * 128:(tt + 1) * 128, :], xt[:], reads=[xt], writes=[self.Xk[tt]])
                P.barrier()

    def phase_hgrn(self, i, j):
        P = self.P
        di = self.din
        HTv = self.HT.ap().rearrange("(c p) t -> p c t", p=128)
        with ExitStack() as st:
            lbt = self.sb(st, "hg_lb", [128, 2, 8]); oml = self.sb(st, "hg_oml", [128, 2, 8])
            bf = self.sb(st, "hg_bf", [128, 2, 8]); gn = self.sb(st, "hg_gn", [128, 1])
            maskF = self.sb(st, "hg_mF", [128, 128]); maskB = self.sb(st, "hg_mB", [128, 128])
            rmask = self.sb(st, "hg_rm", [128, 512]); ones = self.sb(st, "hg_ones", [128, 128])
            P.dma("sp", bf[:], di["hg_bfT"][j], writes=[bf])
            P.dma("sp", gn[:], di["hg_gn"][j], writes=[gn])
            P.dma("sp", maskF[:], di["hg_maskF"][:, :], writes=[maskF])
            P.dma("sp", maskB[:], di["hg_maskB"][:, :], writes=[maskB])
            P.dma("sp", rmask[:], di["hg_rmask"][:, :], writes=[rmask])
            P.memset("pool", ones[:], 1.0, writes=[ones])
            with ExitStack() as s0:
                lg = self.sb(s0, "hg_lg", [128, 2, 4, 8]); tot = self.sb(s0, "hg_tot", [128, 2, 8])
                P.dma("sp", lg[:], di["hg_lbl"][:, :, :, :], writes=[lg])
                P.act(lg[:], lg[:], AF.Exp, reads=[lg], writes=[lg])
                P.op("dve", lambda e: e.reduce_sum(out=tot[:], in_=lg[:].rearrange("p d l c -> p d c l"), axis=AX.X),
                     reads=[lg], writes=[tot])
                P.op("dve", lambda e: e.reciprocal(out=tot[:], in_=tot[:]), reads=[tot], writes=[tot])
                if i >= 1:
                    P.op("dve", lambda e: e.reduce_sum(out=lbt[:], in_=lg[:, :, 1:i + 1, :].rearrange("p d l c -> p d c l"),
                                                       axis=AX.X), reads=[lg], writes=[lbt])
                    P.tt("dve", lbt[:], lbt[:], tot[:], ALU.mult, reads=[lbt, tot], writes=[lbt])
                else:
                    P.memset("dve", lbt[:], 0.0, writes=[lbt])
                P.ts("dve", oml[:], lbt[:], -1.0, 1.0, ALU.mult, ALU.add, reads=[lbt], writes=[oml])
                P.barrier()
            wq = self.sb(st, "hg_wq", [128, 8, D], BF16); wv = self.sb(st, "hg_wv", [128, 8, D], BF16)
            for c in range(8):
                P.dma("pool", wq[:, c, :], di["hg_w_qig"][j, c * 128:(c + 1) * 128, 0:D], writes=[wq])
                P.dma("pool", wv[:, c, :], di["hg_w_qig"][j, c * 128:(c + 1) * 128, D:2 * D], writes=[wv])
            S32 = self.sb(st, "hg_S", [128, 8, 128]); Sbf = self.sb(st, "hg_Sbf", [128, 8, 128], BF16)
            rhT = self.ring(st, "hg_hT", 2, [128, 8, 512], BF16)
            rvt = self.ring(st, "hg_vt", 2, [128, 4, 8, 128], BF16)
            r32 = self.ring(st, "hg_a", 8, [128, 512])
            r16 = self.ring(st, "hg_b", 6, [128, 512], BF16)
            rklT = self.ring(st, "hg_klT", 2, [128, 4, 128], BF16)
            rsc = self.ring(st, "hg_sc", 3, [128, 128], BF16)
            rel = self.ring(st, "hg_el", 2, [128, 16])
            rso = self.ring(st, "hg_so", 2, [128, 128])
            for d in range(2):
                with ExitStack() as s1:
                    wf = self.sb(s1, "hg_wf", [128, 8, D], BF16)
                    for c in range(8):
                        P.dma("pool", wf[:, c, :], di["hg_w_f"][j, d, c * 128:(c + 1) * 128, :], writes=[wf])
                    if d == 1:
                        wg = self.sb(s1, "hg_wg", [128, 8, D], BF16); wo = self.sb(s1, "hg_wo", [128, 8, D], BF16)
                        for c in range(8):
                            P.dma("pool", wg[:, c, :], di["hg_w_qig"][j, c * 128:(c + 1) * 128, 2 * D:3 * D], writes=[wg])
                            P.dma("pool", wo[:, c, :], di["hg_w_o"][j, c * 128:(c + 1) * 128, :], writes=[wo])
                        og = self.sb(s1, "hg_og", [128, 8, 512], BF16)
                        rx = self.ring(s1, "hg_x", 2, [128, D]); rta = self.ring(s1, "hg_ta", 2, [128, D])
                    for hd in range(8):
                        P.dma("sp", S32[:, hd, :], di["hg_s0"][d, hd], writes=[S32])
                    P.cp("act", Sbf[:], S32[:], reads=[S32], writes=[Sbf])
                    mask = maskF if d == 0 else maskB
                    tbs = range(8) if d == 0 else range(7, -1, -1)
                    for tb in tbs:
                        hT, vt = rhT.next(), rvt.next()
                        P.dma("sp", hT[:], HTv[:, :, tb * 512:(tb + 1) * 512], reads=[self.HTk], writes=[hT])
                        for g4 in range(4):
                            for hf in range(2):
                                pb = self.psum.next()
                                for c in range(8):
                                    P.mm(pb[:, :], lhsT=hT[:, c, g4 * 128:(g4 + 1) * 128], rhs=wv[:, c, hf * 512:(hf + 1) * 512],
                                         start=(c == 0), stop=(c == 7), reads=[hT, wv], writes=[pb], signal=(c == 7))
                                P.cp("act", vt[:, g4, hf * 4:(hf + 1) * 4, :].rearrange("p a b -> p (a b)"), pb[:, :],
                                     reads=[pb], writes=[vt])
                        for hd in range(8):
                            hs = slice(hd * 128, (hd + 1) * 128)
                            pq, pf = self.psum.next(), self.psum.next()
                            for c in range(8):
                                P.mm(pq[:, :], lhsT=wq[:, c, hs], rhs=hT[:, c, :], start=(c == 0), stop=(c == 7),
                                     reads=[wq, hT], writes=[pq], signal=(c == 7))
                            for c in range(8):
                                P.mm(pf[:, :], lhsT=wf[:, c, hs], rhs=hT[:, c, :], start=(c == 0), stop=(c == 7),
                                     reads=[wf, hT], writes=[pf], signal=(c == 7))
                            f_, lf, km, cum, ex = r32.next(), r32.next(), r32.next(), r32.next(), r32.next()
                            qe, ke, kl = r16.next(), r16.next(), r16.next()
                            el = rel.next()
                            P.act(f_[:], pf[:, :], AF.Sigmoid, reads=[pf, bf], writes=[f_], bias=bf[:, d, hd:hd + 1])
                            P.ts("dve", f_[:], f_[:], oml[:, d, hd:hd + 1], lbt[:, d, hd:hd + 1], ALU.mult, ALU.add,
                                 reads=[f_, oml, lbt], writes=[f_])
                            P.act(lf[:], f_[:], AF.Ln, reads=[f_], writes=[lf])
                            P.ts("pool", km[:], f_[:], -1.0, 1.0, ALU.mult, ALU.add, reads=[f_], writes=[km])
                            P.scan(cum[:], rmask[:], lf[:], 0.0, ALU.mult, ALU.add, reads=[rmask, lf], writes=[cum])
                            last = cum[:].rearrange("p (n c) -> p n c", c=32)[:, :, 31:32]
                            P.act(el[:].unsqueeze(2), last, AF.Exp, reads=[cum], writes=[el])
                            if d == 1:
                                P.tt("dve", lf[:], lf[:], cum[:], ALU.subtract, reads=[lf, cum], writes=[lf])
                                P.tt("pool", cum[:].rearrange("p (n c) -> p n c", c=32),
                                     lf[:].rearrange("p (n c) -> p n c", c=32), last.to_broadcast([128, 16, 32]), ALU.add,
                                     reads=[lf, cum], writes=[cum])
                            P.act(ex[:], cum[:], AF.Exp, reads=[cum], writes=[ex])
                            P.tt("dve", qe[:], pq[:, :], ex[:], ALU.mult, reads=[pq, ex], writes=[qe])
                            P.act(ex[:], cum[:], AF.Exp, reads=[cum], writes=[ex], scale=-1.0)
                            P.tt("pool", ke[:], km[:], ex[:], ALU.mult, reads=[km, ex], writes=[ke])
                            P.tt("pool", kl[:].rearrange("p (n c) -> p n c", c=32), ke[:].rearrange("p (n c) -> p n c", c=32),
                                 el[:].unsqueeze(2).to_broadcast([128, 16, 32]), ALU.mult, reads=[ke, el], writes=[kl])
                            klT = rklT.next()
                            pt = self.psum.next()
                            for g4 in range(4):
                                P.tr(self.psbf(pt)[:, g4 * 128:(g4 + 1) * 128], kl[:, g4 * 128:(g4 + 1) * 128], self.identb[:],
                                     reads=[kl, self.identb], writes=[pt], signal=(g4 == 3))
                            P.cp("act", klT[:].rearrange("p a b -> p (a b)"), self.psbf(pt)[:, 0:512], reads=[pt], writes=[klT])
                            if d == 1:
                                pg = self.psum.next()
                                for c in range(8):
                                    P.mm(pg[:, :], lhsT=wg[:, c, hs], rhs=hT[:, c, :], start=(c == 0), stop=(c == 7),
                                         reads=[wg, hT], writes=[pg], signal=(c == 7))
                                sgt = r32.next()
                                P.act(sgt[:], pg[:, :], AF.Silu, reads=[pg], writes=[sgt])
                                ofw = r32.next()
                                P.dma("sp", ofw[:], self.OF[hd, :, tb * 512:(tb + 1) * 512], reads=[self.OFk], writes=[ofw])
                            else:
                                ofw = r32.next()
                            g4s = range(4) if d == 0 else range(3, -1, -1)
                            for g4 in g4s:
                                gs = slice(g4 * 128, (g4 + 1) * 128)
                                psc = self.psum.next()
                                P.mm(psc[:, 0:128], lhsT=ke[:, gs], rhs=qe[:, gs], reads=[ke, qe], writes=[psc])
                                scT = rsc.next()
                                P.tt("dve", scT[:], psc[:, 0:128], mask[:], ALU.mult, reads=[psc, mask], writes=[scT])
                                po = self.psum.next()
                                P.mm(po[:, 0:128], lhsT=vt[:, g4, hd, :], rhs=scT[:], start=True, stop=False,
                                     reads=[vt, scT], writes=[po], signal=False)
                                js = range(4) if d == 0 else range(3, -1, -1)
                                for jn, jc in enumerate(js):
                                    cs = slice(g4 * 128 + jc * 32, g4 * 128 + jc * 32 + 32)
                                    ci = tb * 16 + g4 * 4 + jc
                                    P.mm(po[:, jc * 32:jc * 32 + 32], lhsT=Sbf[:, hd, :], rhs=qe[:, cs], start=False, stop=(jn == 3),
                                         reads=[Sbf, qe], writes=[po], signal=(jn == 3), skip_group_check=True)
                                    pS = self.psum.next()
                                    P.mm(pS[:, 0:128], lhsT=klT[32 * jc:32 * jc + 32, g4, :], rhs=vt[32 * jc:32 * jc + 32, g4, hd, :],
                                         reads=[klT, vt], writes=[pS], tile_position=(32 * jc, 0))
                                    P.stt(S32[:, hd, :], S32[:, hd, :], el[:, g4 * 4 + jc:g4 * 4 + jc + 1], pS[:, 0:128],
                                          ALU.mult, ALU.add, reads=[S32, el, pS], writes=[S32])
                                    seg_done = ((ci + 1) % 8 == 0) if d == 0 else (ci % 8 == 0)
                                    if seg_done:
                                        sgi = ci // 8
                                        if self.cfg.get("state_out", True):
                                            so = rso.next()
                                            P.cp("act", so[:], S32[:, hd, :], reads=[S32], writes=[so])
                                            k = Tok()
                                            P.dma("sp", self.dout["o_hg"][sgi, j, d, hd], so[:], reads=[so], writes=[k])
                                            self.outk.append(k)
                                        P.ts("pool", S32[:, hd, :], S32[:, hd, :], self.flag[:, 0:1], None, ALU.mult,
                                             reads=[S32, self.flag], writes=[S32])
                                    P.cp("act", Sbf[:, hd, :], S32[:, hd, :], reads=[S32], writes=[Sbf])
                                if d == 0:
                                    P.cp("act", ofw[:, gs], po[:, 0:128], reads=[po], writes=[ofw])
                                else:
                                    P.tt("dve", ofw[:, gs], po[:, 0:128], ofw[:, gs], ALU.add, reads=[po, ofw], writes=[ofw])
                            if d == 0:
                                P.dma("sp", self.OF[hd, :, tb * 512:(tb + 1) * 512], ofw[:], reads=[ofw], writes=[self.OFk])
                            else:
                                sq = r32.next()
                                P.act(sq[:], ofw[:], AF.Square, reads=[ofw], writes=[sq])
                                pn = self.psum.next()
                                P.mm(pn[:, :], lhsT=ones[:], rhs=sq[:], reads=[ones, sq], writes=[pn])
                                P.act(sq[:], pn[:, :], AF.Ln, reads=[pn], writes=[sq], scale=1.0 / 128, bias=self.epsb[:, 0:1])
                                P.act(sq[:], sq[:], AF.Exp, reads=[sq], writes=[sq], scale=-0.5)
                                P.tt("dve", ofw[:], ofw[:], sq[:], ALU.mult, reads=[ofw, sq], writes=[ofw])
                                P.stt(og[:, hd, :], ofw[:], gn[:, 0:1], sgt[:], ALU.mult, ALU.mult, reads=[ofw, gn, sgt], writes=[og])
                        if d == 1:
                            for g4 in range(4):
                                tt = tb * 4 + g4
                                xt, ta = rx.next(), rta.next()
                                P.dma("sp", xt[:], self.X[tt * 128:(tt + 1) * 128, :], reads=[self.Xk[tt]], writes=[xt])
                                for hf in range(2):
                                    pb = self.psum.next()
                                    for hd in range(8):
                                        P.mm(pb[:, :], lhsT=og[:, hd, g4 * 128:(g4 + 1) * 128], rhs=wo[:, hd, hf * 512:(hf + 1) * 512],
                                             start=(hd == 0), stop=(hd == 7), reads=[og, wo], writes=[pb], signal=(hd == 7))
                                    P.tt("dve", ta[:, hf * 512:(hf + 1) * 512], pb[:, :], self.modG(0)[:, hf * 512:(hf + 1) * 512],
                                         ALU.mult, reads=[pb, self.mod], writes=[ta])
                                P.tt("pool", xt[:], xt[:], ta[:], ALU.add, reads=[xt, ta], writes=[xt])
                                P.dma("sp", self.X[tt * 128:(tt + 1) * 128, :], xt[:], reads=[xt], writes=[self.Xk[tt]])
                    P.barrier()

    def phase_ssd(self, i, j):
        P = self.P
        di = self.din
        HTv = self.HT.ap().rearrange("(c p) t -> p c t", p=128)
        XI = 2 * D
        NXBC = 3 * D
        with ExitStack() as st:
            wx = self.sb(st, "sd_wx", [128, 8, NXBC], BF16)
            for c in range(8):
                P.dma("pool", wx[:, c, :], di["ssd_w_in"][j, c * 128:(c + 1) * 128, XI:XI + NXBC], writes=[wx])
            rhT = self.ring(st, "sd_hT", 2, [128, 8, 512], BF16)
            ro = self.ring(st, "sd_o", 4, [128, 512])
            for tb in range(8):
                hT = rhT.next()
                P.dma("sp", hT[:], HTv[:, :, tb * 512:(tb + 1) * 512], reads=[self.HTk], writes=[hT])
                for ct in range(24):
                    pb = self.psum.next()
                    for c in range(8):
                        P.mm(pb[:, :], lhsT=wx[:, c, ct * 128:(ct + 1) * 128], rhs=hT[:, c, :], start=(c == 0), stop=(c == 7),
                             reads=[wx, hT], writes=[pb], signal=(c == 7))
                    o = ro.next()
                    P.cp("act" if ct % 2 == 0 else "dve", o[:], pb[:, :], reads=[pb], writes=[o])
                    P.dma("sp", self.XBC[ct * 128:(ct + 1) * 128, tb * 512:(tb + 1) * 512], o[:], reads=[o], writes=[self.XBCk])
            P.barrier()
        with ExitStack() as st:
            cw = self.sb(st, "sd_cw", [128, 24, 5]); cb = self.sb(st, "sd_cb", [128, 24])
            P.dma("sp", cw[:], di["ssd_cwT"][j], writes=[cw])
            P.dma("sp", cb[:], di["ssd_cbT"][j], writes=[cb])
            rxp = self.ring(st, "sd_xp", 2, [128, NSEG, SEG + 4])
            racc = self.ring(st, "sd_acc", 2, [128, NSEG, SEG])
            rob = self.ring(st, "sd_ob", 2, [128, NSEG, SEG], BF16)
            for ct in range(24):
                xp, acc, ob = rxp.next(), racc.next(), rob.next()
                P.dma("sp", xp[:, :, 2:2 + SEG], self.XBC[ct * 128:(ct + 1) * 128, :].rearrange("p (s t) -> p s t", t=SEG),
                      reads=[self.XBCk], writes=[xp])
                P.memset("pool", xp[:, 0:1, 0:2], 0.0, writes=[xp])
                P.memset("pool", xp[:, NSEG - 1:NSEG, SEG + 2:SEG + 4], 0.0, writes=[xp])
                P.ts("pool", xp[:, 1:NSEG, 0:2], xp[:, 0:NSEG - 1, SEG:SEG + 2], self.flag[:, 0:1], None, ALU.mult,
                     reads=[xp, self.flag], writes=[xp])
                P.ts("pool", xp[:, 0:NSEG - 1, SEG + 2:SEG + 4], xp[:, 1:NSEG, 2:4], self.flag[:, 0:1], None, ALU.mult,
                     reads=[xp, self.flag], writes=[xp])
                P.ts("dve", acc[:], xp[:, :, 0:SEG], cw[:, ct, 0:1], cb[:, ct:ct + 1], ALU.mult, ALU.add,
                     reads=[xp, cw, cb], writes=[acc])
                for k in range(1, 5):
                    P.stt(acc[:], xp[:, :, k:k + SEG], cw[:, ct, k:k + 1], acc[:], ALU.mult, ALU.add,
                          reads=[xp, cw, acc], writes=[acc])
                P.act(ob[:], acc[:], AF.Silu, reads=[acc], writes=[ob])
                P.dma("sp", self.XC[ct * 128:(ct + 1) * 128, :], ob[:].rearrange("p s t -> p (s t)"), reads=[ob], writes=[self.XCk])
            P.barrier()
        with ExitStack() as st:
            XCv = self.XC.ap().rearrange("(k p) t -> p k t", p=128)
            wdt = self.sb(st, "sd_wdt", [128, 8, 64], BF16)
            for c in range(8):
                P.dma("pool", wdt[:, c, :], di["ssd_w_in"][j, c * 128:(c + 1) * 128, XI + NXBC:XI + NXBC + 64], writes=[wdt])
            dtb = self.sb(st, "sd_dtb", [128, 64]); arow = self.sb(st, "sd_a", [128, 64]); dsk = self.sb(st, "sd_dsk", [128, 32])
            P.dma("sp", dtb[:], di["ssd_dtb"][j:j + 1, :].broadcast_to([128, 64]), writes=[dtb])
            P.dma("sp", arow[:], di["ssd_alog"][j:j + 1, :].broadcast_to([128, 64]), writes=[arow])
            P.dma("sp", dsk[:], di["ssd_d"][j:j + 1, :].broadcast_to([128, 32]), writes=[dsk])
            P.act(arow[:], arow[:], AF.Exp, reads=[arow], writes=[arow])
            P.ts("dve", arow[:], arow[:], -1.0, None, ALU.mult, reads=[arow], writes=[arow])
            cm = {}
            for nm in ("TriF", "UF", "TriB", "UB", "negF", "negB", "Ones0", "Ones1"):
                cm[nm] = self.sb(st, "sd_" + nm, [128, 128])
                P.dma("sp", cm[nm][:], di["ssd_" + nm][:, :], writes=[cm[nm]])
            H32 = self.sb(st, "sd_H32", [128, 4, 512]); Hbf = self.sb(st, "sd_Hbf", [128, 4, 512], BF16)
            rxf = self.ring(st, "sd_xf", 2, [128, 24, 128], BF16)
            rhg = self.ring(st, "sd_hg", 2, [128, 8, 128], BF16)
            rxt = self.ring(st, "sd_xt", 2, [128, 2048], BF16)
            rbt = self.ring(st, "sd_bt", 2, [128, 4, 128], BF16)
            rxd = self.ring(st, "sd_xd", 1, [128, 2048], BF16); rxe = self.ring(st, "sd_xe", 1, [128, 2048], BF16)
            rsm = self.ring(st, "sd_sm", 12, [128, 64])
            rl = self.ring(st, "sd_l", 2, [128, 8, 128])
            rcbt = self.ring(st, "sd_cbt", 2, [128, 128])
            rtm = self.ring(st, "sd_tm", 2, [128, 512]); rlm = self.ring(st, "sd_lm", 2, [128, 512])
            rmt = self.ring(st, "sd_mt", 2, [128, 4, 128], BF16)
            rya = self.ring(st, "sd_ya", 1, [128, 2048])
            rso = self.ring(st, "sd_so", 2, [64, 512])
            for d in range(2):
                with ExitStack() as s1:
                    if d == 1:
                        wz = self.sb(s1, "sd_wz", [128, 8, XI], BF16); wout = self.sb(s1, "sd_wo", [128, 16, D], BF16)
                        for c in range(8):
                            P.dma("pool", wz[:, c, :], di["ssd_w_in"][j, c * 128:(c + 1) * 128, 0:XI], writes=[wz])
                        for k in range(16):
                            P.dma("pool", wout[:, k, :], di["ssd_w_out"][j, k * 128:(k + 1) * 128, :], writes=[wout])
                        ng = self.sb(s1, "sd_ng", [128, XI])
                        P.dma("sp", ng[:], di["ssd_norm"][j:j + 1, :].broadcast_to([128, XI]), writes=[ng])
                        rsz = self.ring(s1, "sd_sz", 1, [128, XI]); ryn = self.ring(s1, "sd_yn", 1, [128, XI], BF16)
                        ryT = self.ring(s1, "sd_yT", 1, [128, 16, 128], BF16)
                        rx = self.ring(s1, "sd_x", 1, [128, D]); rta = self.ring(s1, "sd_ta", 1, [128, D])
                        rst = self.ring(s1, "sd_st", 2, [128, 8])
                    P.dma("sp", H32[:], di["ssd_h0"][:, d], writes=[H32])
                    P.cp("act", Hbf[:], H32[:], reads=[H32], writes=[Hbf])
                    Tri, U, neg = (cm["TriF"], cm["UF"], cm["negF"]) if d == 0 else (cm["TriB"], cm["UB"], cm["negB"])
                    gis = range(NT) if d == 0 else range(NT - 1, -1, -1)
                    for gi in gis:
                        ts_ = slice(gi * 128, (gi + 1) * 128)
                        xf, hg, xt, bt = rxf.next(), rhg.next(), rxt.next(), rbt.next()
                        P.dma("sp", xf[:], XCv[:, :, ts_], reads=[self.XCk], writes=[xf])
                        P.dma("sp", hg[:], HTv[:, :, ts_], reads=[self.HTk], writes=[hg])
                        for hb in range(2):
                            pb = self.psum.next()
                            for k in range(8):
                                P.tr(self.psbf(pb)[:, k * 128:(k + 1) * 128], xf[:, hb * 8 + k, :], self.identb[:],
                                     reads=[xf, self.identb], writes=[pb], signal=(k == 7))
                            P.cp("act" if hb == 0 else "dve", xt[:, hb * 1024:(hb + 1) * 1024], self.psbf(pb)[:, 0:1024],
                                 reads=[pb], writes=[xt])
                        pb = self.psum.next()
                        for g in range(4):
                            P.tr(self.psbf(pb)[:, g * 128:(g + 1) * 128], xf[:, 16 + g, :], self.identb[:],
                                 reads=[xf, self.identb], writes=[pb], signal=(g == 3))
                        P.cp("act", bt[:].rearrange("p a b -> p (a b)"), self.psbf(pb)[:, 0:512], reads=[pb], writes=[bt])
                        pdt = self.psum.next()
                        for c in range(8):
                            P.mm(pdt[:, 0:32], lhsT=hg[:, c, :], rhs=wdt[:, c, d * 32:(d + 1) * 32], start=(c == 0), stop=(c == 7),
                                 reads=[hg, wdt], writes=[pdt], signal=(c == 7))
                        dt_, dA, ecum, edec = rsm.next(), rsm.next(), rsm.next(), rsm.next()
                        cdec = rsm.next()
                        P.tt("dve", dt_[:, 0:32], pdt[:, 0:32], dtb[:, d * 32:(d + 1) * 32], ALU.add, reads=[pdt, dtb], writes=[dt_])
                        P.act(dt_[:, 0:32], dt_[:, 0:32], AF.Exp, reads=[dt_], writes=[dt_])
                        P.act(dt_[:, 0:32], dt_[:, 0:32], AF.Ln, reads=[dt_], writes=[dt_], bias=1.0)
                        P.tt("dve", dA[:, 0:32], dt_[:, 0:32], arow[:, d * 32:(d + 1) * 32], ALU.mult, reads=[dt_, arow], writes=[dA])
                        pc = self.psum.next()
                        P.mm(pc[:, 0:32], lhsT=Tri[:], rhs=dA[:, 0:32], reads=[Tri, dA], writes=[pc], signal=False)
                        P.mm(pc[:, 32:64], lhsT=U[:], rhs=dA[:, 0:32], reads=[U, dA], writes=[pc], signal=False)
                        P.mm(pc[:, 64:96], lhsT=cm["Ones0"][:], rhs=dA[:, 0:32], reads=[cm["Ones0"], dA], writes=[pc], signal=False)
                        P.mm(pc[:, 96:128], lhsT=cm["Ones1"][:], rhs=dA[:, 0:32], reads=[cm["Ones1"], dA], writes=[pc])
                        P.act(ecum[:, 0:32], pc[:, 0:32], AF.Exp, reads=[pc], writes=[ecum])
                        P.act(edec[:, 0:32], pc[:, 32:64], AF.Exp, reads=[pc], writes=[edec])
                        P.act(cdec[:, 0:64], pc[:, 64:128], AF.Exp, reads=[pc], writes=[cdec])
                        xd, xe = rxd.next(), rxe.next()
                        x3 = lambda b: b[:].rearrange("p (h q) -> p h q", q=64)
                        bc32 = lambda ap: ap.unsqueeze(2).to_broadcast([128, 32, 64])
                        P.tt("pool", x3(xd), x3(xt), bc32(dt_[:, 0:32]), ALU.mult, reads=[xt, dt_], writes=[xd])
                        P.tt("dve", x3(xe), x3(xd), bc32(edec[:, 0:32]), ALU.mult, reads=[xd, edec], writes=[xe])
                        ya = rya.next()
                        if d == 1:
                            P.dma("sp", ya[:], self.YF[ts_, :], reads=[self.YFk], writes=[ya])
                        else:
                            P.tt("pool", x3(ya), x3(xt), bc32(dsk[:, 0:32]), ALU.mult, reads=[xt, dsk], writes=[ya])
                        for g in range(4):
                            hsl = slice(g * 8, g * 8 + 8)
                            lall = rl.next()
                            P.tt("pool", lall[:], U[:].unsqueeze(1).to_broadcast([128, 8, 128]),
                                 dA[:, hsl].unsqueeze(2).to_broadcast([128, 8, 128]), ALU.mult, reads=[U, dA], writes=[lall])
                            pcb = self.psum.next()
                            P.mm(pcb[:, 0:128], lhsT=xf[:, 16 + g, :], rhs=xf[:, 20 + g, :], reads=[xf], writes=[pcb])
                            cbt = rcbt.next()
                            P.cp("act", cbt[:], pcb[:, 0:128], reads=[pcb], writes=[cbt])
                            pyi = self.psum.next()
                            for hf in range(2):
                                pd = self.psum.next()
                                for h4 in range(4):
                                    P.mm(pd[:, h4 * 128:(h4 + 1) * 128], lhsT=lall[:, hf * 4 + h4, :], rhs=Tri[:],
                                         reads=[lall, Tri], writes=[pd], signal=(h4 == 3))
                                tm, lm, mt = rtm.next(), rlm.next(), rmt.next()
                                P.tt("dve", tm[:].rearrange("p (a b) -> p a b", b=128), pd[:, :].rearrange("p (a b) -> p a b", b=128),
                                     neg[:].unsqueeze(1).to_broadcast([128, 4, 128]), ALU.add, reads=[pd, neg], writes=[tm])
                                P.act(lm[:], tm[:], AF.Exp, reads=[tm], writes=[lm])
                                P.tt("pool", mt[:], lm[:].rearrange("p (a b) -> p a b", b=128),
                                     cbt[:].unsqueeze(1).to_broadcast([128, 4, 128]), ALU.mult, reads=[lm, cbt], writes=[mt])
                                for h4 in range(4):
                                    hl = hf * 4 + h4
                                    hh = g * 8 + hl
                                    P.mm(pyi[:, hl * 64:(hl + 1) * 64], lhsT=mt[:, h4, :], rhs=xd[:, hh * 64:(hh + 1) * 64],
                                         reads=[mt, xd], writes=[pyi], signal=(h4 == 3))
                            pyo = self.psum.next()
                            js = range(2) if d == 0 else range(1, -1, -1)
                            for jn, jc in enumerate(js):
                                rs = slice(64 * jc, 64 * jc + 64)
                                ci = gi * 2 + jc
                                P.mm(pyo[rs, :], lhsT=xf[:, 20 + g, rs], rhs=Hbf[:, g, :], reads=[xf, Hbf], writes=[pyo],
                                     signal=(jn == 1), tile_position=(0, 64 * jc))
                                pst = self.psum.next()
                                P.mm(pst[:, :], lhsT=bt[rs, g, :], rhs=xe[rs, g * 512:(g + 1) * 512], reads=[bt, xe], writes=[pst],
                                     tile_position=(64 * jc, 0))
                                P.tt("dve", H32[:, g, :].rearrange("p (h q) -> p h q", q=64),
                                     H32[:, g, :].rearrange("p (h q) -> p h q", q=64),
                                     cdec[:, jc * 32 + g * 8:jc * 32 + g * 8 + 8].unsqueeze(2).to_broadcast([128, 8, 64]), ALU.mult,
                                     reads=[H32, cdec], writes=[H32])
                                P.tt("dve", H32[:, g, :], H32[:, g, :], pst[:, :], ALU.add, reads=[H32, pst], writes=[H32])
                                seg_done = ((ci + 1) % 4 == 0) if d == 0 else (ci % 4 == 0)
                                if seg_done:
                                    sgi = ci // 4
                                    if self.cfg.get("state_out", True):
                                        so = rso.next()
                                        for q2 in range(2):
                                            pt = self.psum.next()
                                            for h4 in range(4):
                                                hl = q2 * 4 + h4
                                                P.tr(pt[0:64, h4 * 128:(h4 + 1) * 128], H32[:, g, hl * 64:(hl + 1) * 64], self.ident[:],
                                                     reads=[H32, self.ident], writes=[pt], signal=(h4 == 3))
                                            if q2 == 1:
                                                so = rso.next()
                                            P.cp("act", so[:], pt[0:64, :], reads=[pt], writes=[so])
                                            k = Tok()
                                            P.dma("sp", self.dout["o_ssd"][sgi, j, d, g * 8 + q2 * 4:g * 8 + q2 * 4 + 4].rearrange("h p n -> p h n"),
                                                  so[:].rearrange("p (h n) -> p h n", n=128), reads=[so], writes=[k])
                                            self.outk.append(k)
                                    P.ts("pool", H32[:, g, :], H32[:, g, :], self.flag[:, 0:1], None, ALU.mult,
                                         reads=[H32, self.flag], writes=[H32])
                                P.cp("act", Hbf[:, g, :], H32[:, g, :], reads=[H32], writes=[Hbf])
                            tm = rtm.next()
                            P.tt("dve", tm[:].rearrange("p (h q) -> p h q", q=64), pyo[:, :].rearrange("p (h q) -> p h q", q=64),
                                 ecum[:, hsl].unsqueeze(2).to_broadcast([128, 8, 64]), ALU.mult, reads=[pyo, ecum], writes=[tm])
                            P.tt("pool", ya[:, g * 512:(g + 1) * 512], ya[:, g * 512:(g + 1) * 512], tm[:], ALU.add,
                                 reads=[ya, tm], writes=[ya])
                            P.tt("dve", ya[:, g * 512:(g + 1) * 512], pyi[:, :], ya[:, g * 512:(g + 1) * 512], ALU.add,
                                 reads=[pyi, ya], writes=[ya])
                        if d == 0:
                            P.dma("sp", self.YF[ts_, :], ya[:], reads=[ya], writes=[self.YFk])
                        else:
                            sz, yn, yT, xtile, ta, stat = rsz.next(), ryn.next(), ryT.next(), rx.next(), rta.next(), rst.next()
                            P.dma("sp", xtile[:], self.X[ts_, :], reads=[self.Xk[gi]], writes=[xtile])
                            for zb in range(4):
                                pz = self.psum.next()
                                for c in range(8):
                                    P.mm(pz[:, :], lhsT=hg[:, c, :], rhs=wz[:, c, zb * 512:(zb + 1) * 512], start=(c == 0), stop=(c == 7),
                                         reads=[hg, wz], writes=[pz], signal=(c == 7))
                                P.act(sz[:, zb * 512:(zb + 1) * 512], pz[:, :], AF.Silu, reads=[pz], writes=[sz])
                            P.tt("dve", ya[:], ya[:], sz[:], ALU.mult, reads=[ya, sz], writes=[ya])
                            P.act(sz[:], ya[:], AF.Square, reads=[ya], writes=[sz, stat], accum_out=stat[:, 0:1])
                            P.act(stat[:, 1:2], stat[:, 0:1], AF.Ln, reads=[stat], writes=[stat], scale=1.0 / XI, bias=self.epsb[:, 0:1])
                            P.act(stat[:, 2:3], stat[:, 1:2], AF.Exp, reads=[stat], writes=[stat], scale=-0.5)
                            P.stt(yn[:], ya[:], stat[:, 2:3], ng[:], ALU.mult, ALU.mult, reads=[ya, stat, ng], writes=[yn])
                            for hb in range(2):
                                pb = self.psum.next()
                                for k in range(8):
                                    P.tr(self.psbf(pb)[:, k * 128:(k + 1) * 128], yn[:, (hb * 8 + k) * 128:(hb * 8 + k + 1) * 128],
                                         self.identb[:], reads=[yn, self.identb], writes=[pb], signal=(k == 7))
                                P.cp("act" if hb == 0 else "dve", yT[:, hb * 8:(hb + 1) * 8, :].rearrange("p a b -> p (a b)"),
                                     self.psbf(pb)[:, 0:1024], reads=[pb], writes=[yT])
                            for hf in range(2):
                                pb = self.psum.next()
                                for k in range(16):
                                    P.mm(pb[:, :], lhsT=yT[:, k, :], rhs=wout[:, k, hf * 512:(hf + 1) * 512], start=(k == 0), stop=(k == 15),
                                         reads=[yT, wout], writes=[pb], signal=(k == 15))
                                P.tt("dve", ta[:, hf * 512:(hf + 1) * 512], pb[:, :], self.modG(0)[:, hf * 512:(hf + 1) * 512], ALU.mult,
                                     reads=[pb, self.mod], writes=[ta])
                            P.tt("pool", xtile[:], xtile[:], ta[:], ALU.add, reads=[xtile, ta], writes=[xtile])
                            P.dma("sp", self.X[ts_, :], xtile[:], reads=[xtile], writes=[self.Xk[gi]])
                    P.barrier()

    def dbg(self, name, ap, shape, buf, dt=F32):
        o = self.outp(name, shape, dt)
        k = Tok()
        self.P.dma("sp", o.ap(), ap, reads=[buf], writes=[k])
        self.outk.append(k)

    def sincos_turns(self, st, x, shape, cs_out, sn_out, tag, outk=None):
        P = self.P
        key = ("sc", tuple(shape))
        if not hasattr(self, "_sc"):
            self._sc = {}
        if key not in self._sc:
            self._sc[key] = (self.sb(st, "sc_i", shape, I32), self.sb(st, "sc_f", shape), self.sb(st, "sc_y", shape),
                             self.sb(st, "sc_m", shape))
        ki, kf, y, m = self._sc[key]
        TWO_PI = 2.0 * np.pi
        outs = [(sn_out, 0.0), (cs_out, 0.25)]
        for (o, off) in outs:
            oap = o[:] if isinstance(o, Buf) else o
            ow = [o] if isinstance(o, Buf) else list(outk)
            if off != 0.0:
                P.ts("dve", y[:], x[:], off, None, ALU.add, reads=[x], writes=[y])
                src = y
            else:
                src = x
            P.cp("dve", ki[:], src[:], reads=[src], writes=[ki])
            P.cp("dve", kf[:], ki[:], reads=[ki], writes=[kf])
            P.tt("dve", y[:], src[:], kf[:], ALU.subtract, reads=[src, kf], writes=[y])
            P.ts("dve", y[:], y[:], 0.5, -0.5, ALU.min, ALU.max, reads=[y], writes=[y])
            P.act(oap, y[:], AF.Sin, reads=[y], writes=ow, scale=TWO_PI)

    def phase_moe(self, i, moe_router, w_gate, w_up, w_down, capv_d, iota_d, tokid_d):
        P = self.P
        nc = self.nc
        n_exp = self.cfg.get("n_exp", NE)
        with ExitStack() as st:
            aff_all = self.sb(st, "aff_all", [128, NT, NE])
            pos_tok = self.sb(st, "pos_tok", [128, NT, NE])
            gm_tok = self.sb(st, "gm_tok", [128, NT, NE])
            tokid = self.sb(st, "tokid", [128, NT])
            tg_all = self.sb(st, "tg_all", [128, NT, NE, 2])
            iota = self.sb(st, "iota", [128, 512])
            P.dma("sp", tokid[:], tokid_d[:, :], writes=[tokid])
            P.dma("sp", iota[:], iota_d[:, :], writes=[iota])
            with ExitStack() as s2:
                affT = self.sb(s2, "affT", [16, T])
                wrt = self.sb(s2, "wrt", [128, 8, NE])
                P.dma("sp", wrt[:], moe_router[i].rearrange("(c p) e -> p c e", p=128), writes=[wrt])
                with ExitStack() as s3:
                    rx = self.ring(s3, "e_x", 2, [128, D])
                    rh = self.ring(s3, "e_h", 2, [128, D])
                    rhT = self.ring(s3, "e_hT", 2, [128, 8, 128])
                    rstat = self.ring(s3, "e_st", 3, [128, 8])
                    rlg = self.ring(s3, "e_lg", 3, [128, NE])
                    for tt in range(NT):
                        xt, h, hT, stat, lg = rx.next(), rh.next(), rhT.next(), rstat.next(), rlg.next()
                        self.norm_tile(tt, 1, xt, h, stat)
                        P.dma("pool", self.H2[tt * 128:(tt + 1) * 128, :], h[:], reads=[h], writes=[self.H2k])
                        for hb in range(2):
                            pb = self.psum.next()
                            for c4 in range(4):
                                c = hb * 4 + c4
                                P.tr(pb[:, c4 * 128:(c4 + 1) * 128], h[:, c * 128:(c + 1) * 128], self.ident[:],
                                     reads=[h, self.ident], writes=[pb], signal=(c4 == 3))
                            P.cp("act" if hb == 0 else "dve", hT[:, hb * 4:(hb + 1) * 4, :].rearrange("p c t -> p (c t)"),
                                 pb[:, :], reads=[pb], writes=[hT])
                        pl = self.psum.next()
                        for c in range(8):
                            P.mm(pl[:, 0:NE], lhsT=hT[:, c, :], rhs=wrt[:, c, :], start=(c == 0), stop=(c == 7),
                                 reads=[hT, wrt], writes=[pl], signal=(c == 7))
                        P.op("dve", lambda e, o=stat[:, 3:4], a=pl[:, 0:NE]: e.reduce_max(out=o, in_=a, axis=AX.X),
                             reads=[pl], writes=[stat])
                        P.ts("dve", stat[:, 4:5], stat[:, 3:4], -1.0, None, ALU.mult, reads=[stat], writes=[stat])
                        P.act(lg[:], pl[:, 0:NE], AF.Exp, reads=[pl, stat], writes=[lg, stat], bias=stat[:, 4:5],
                              accum_out=stat[:, 5:6])
                        P.op("dve", lambda e, o=stat[:, 6:7], a=stat[:, 5:6]: e.reciprocal(out=o, in_=a),
                             reads=[stat], writes=[stat])
                        P.ts("dve", aff_all[:, tt, :], lg[:], stat[:, 6:7], None, ALU.mult, reads=[lg, stat],
                             writes=[aff_all])
                        pt = self.psum.next()
                        P.tr(pt[0:16, 0:128], aff_all[:, tt, :], self.ident[:], reads=[aff_all, self.ident], writes=[pt])
                        P.cp("act", affT[:, tt * 128:(tt + 1) * 128], pt[0:16, 0:128], reads=[pt], writes=[affT])
                    P.barrier()
                if self.cfg.get("moe_stage", "full") == "a":
                    P.barrier()
                    return
                cmpb = self.sb(s2, "cmpb", [16, T])
                incl = self.sb(s2, "incl", [16, T])
                lo = self.sb(s2, "lo", [16, NSEG])
                hi = self.sb(s2, "hi", [16, NSEG])
                mid = self.sb(s2, "mid", [16, NSEG])
                cnt = self.sb(s2, "cnt", [16, NSEG])
                tot = self.sb(s2, "tot", [16, 1])
                ge = self.sb(s2, "ge", [16, NSEG])
                capv = self.sb(s2, "capv", [16, NSEG])
                ones16 = self.sb(s2, "ones16", [16, T])
                P.dma("sp", capv[:], capv_d[:, :], writes=[capv])
                P.memset("dve", lo[:], 0.0, writes=[lo])
                P.memset("dve", hi[:], 1.0, writes=[hi])
                P.memset("pool", ones16[:], 1.0, writes=[ones16])
                a3 = affT[:].rearrange("e (s t) -> e s t", t=SEG)
                c3 = cmpb[:].rearrange("e (s t) -> e s t", t=SEG)
                fl = self.flag[0:16, 0:1]
                for it in range(self.cfg.get("bisect", 30)):
                    P.tt("dve", mid[:], lo[:], hi[:], ALU.add, reads=[lo, hi], writes=[mid])
                    P.ts("dve", mid[:], mid[:], 0.5, None, ALU.mult, reads=[mid], writes=[mid])
                    P.tt("dve", c3, a3, mid[:].unsqueeze(2).to_broadcast([16, NSEG, SEG]), ALU.is_gt,
                         reads=[affT, mid], writes=[cmpb])
                    P.op("dve", lambda e: e.reduce_sum(out=cnt[:], in_=c3, axis=AX.X), reads=[cmpb], writes=[cnt])
                    P.op("dve", lambda e: e.reduce_sum(out=tot[:], in_=cnt[:], axis=AX.X), reads=[cnt], writes=[tot])
                    P.ts("dve", ge[:], cnt[:], -1.0, tot[:, 0:1], ALU.mult, ALU.add, reads=[cnt, tot], writes=[ge])
                    P.stt(cnt[:], ge[:], fl, cnt[:], ALU.mult, ALU.add, reads=[ge, cnt, self.flag], writes=[cnt])
                    P.tt("dve", ge[:], cnt[:], capv[:], ALU.is_ge, reads=[cnt, capv], writes=[ge])
                    P.tt("dve", cnt[:], mid[:], lo[:], ALU.subtract, reads=[mid, lo], writes=[cnt])
                    P.tt("dve", cnt[:], cnt[:], ge[:], ALU.mult, reads=[cnt, ge], writes=[cnt])
                    P.tt("dve", lo[:], lo[:], cnt[:], ALU.add, reads=[lo, cnt], writes=[lo])
                    P.tt("dve", cnt[:], hi[:], mid[:], ALU.subtract, reads=[hi, mid], writes=[cnt])
                    P.tt("dve", cnt[:], cnt[:], ge[:], ALU.mult, reads=[cnt, ge], writes=[cnt])
                    P.tt("dve", hi[:], mid[:], cnt[:], ALU.add, reads=[mid, cnt], writes=[hi])
                P.tt("dve", c3, a3, lo[:].unsqueeze(2).to_broadcast([16, NSEG, SEG]), ALU.is_gt,
                     reads=[affT, lo], writes=[cmpb])
                P.scan(incl[:], ones16[:], cmpb[:], 0.0, ALU.mult, ALU.add, reads=[ones16, cmpb], writes=[incl])
                P.tt("dve", incl[:], incl[:], cmpb[:], ALU.mult, reads=[incl, cmpb], writes=[incl])
                P.ts("dve", incl[:], incl[:], -1.0, None, ALU.add, reads=[incl], writes=[incl])
                P.tt("dve", cmpb[:], cmpb[:], affT[:], ALU.mult, reads=[cmpb, affT], writes=[cmpb])
                for tt in range(NT):
                    pt = self.psum.next()
                    P.tr(pt[:, 0:16], incl[:, tt * 128:(tt + 1) * 128], self.ident[0:16, 0:16],
                         reads=[incl, self.ident], writes=[pt], signal=False)
                    P.tr(pt[:, 16:32], cmpb[:, tt * 128:(tt + 1) * 128], self.ident[0:16, 0:16],
                         reads=[cmpb, self.ident], writes=[pt])
                    P.cp("act", pos_tok[:, tt, :], pt[:, 0:16], reads=[pt], writes=[pos_tok])
                    P.cp("dve", gm_tok[:, tt, :], pt[:, 16:32], reads=[pt], writes=[gm_tok])
                P.cp("dve", tg_all[:, :, :, 0], tokid[:].unsqueeze(2).to_broadcast([128, NT, NE]), reads=[tokid], writes=[tg_all])
                P.cp("dve", tg_all[:, :, :, 1], gm_tok[:], reads=[gm_tok], writes=[tg_all])
                P.barrier()
            if self.cfg.get("moe_stage", "full") == "b":
                return
            with ExitStack() as s4:
                rwg = self.ring(s4, "wg", 2, [128, 8, 512], BF16)
                rwu = self.ring(s4, "wu", 2, [128, 8, 512], BF16)
                rwd = self.ring(s4, "wd", 2, [128, 16, D], BF16)
                roh = self.ring(s4, "oh", 3, [128, 512])
                rrow = self.ring(s4, "row", 2, [2, 512])
                ridx = self.ring(s4, "idx", 2, [128, 4], I32)
                rgt = self.ring(s4, "gt", 2, [128, 4])
                rxs = self.ring(s4, "xs", 4, [128, D], BF16)
                rxsT = self.ring(s4, "xsT", 1, [128, 8, 512], BF16)
                rhid = self.ring(s4, "hid", 1, [128, 16, 512], BF16)
                rsg = self.ring(s4, "sg", 2, [128, 512], BF16)
                rys = self.ring(s4, "ys", 2, [128, D])
                for ex in range(n_exp):
                    idx, gt = ridx.next(), rgt.next()
                    pi = self.psum.next()
                    for tt in range(NT):
                        oh = roh.next()
                        P.ts("dve", oh[:], iota[:], pos_tok[:, tt, ex:ex + 1], None, ALU.is_equal,
                             reads=[iota, pos_tok], writes=[oh])
                        P.mm(pi[0:2, :], lhsT=tg_all[:, tt, ex, :], rhs=oh[:, :], start=(tt == 0), stop=(tt == NT - 1),
                             reads=[oh, tg_all], writes=[pi], signal=True)
                    row = rrow.next()
                    P.cp("act", row[:], pi[0:2, :], reads=[pi], writes=[row])
                    pj = self.psum.next()
                    for jb in range(4):
                        P.tr(pj[:, 2 * jb:2 * jb + 2], row[0:2, jb * 128:(jb + 1) * 128], self.ident[0:2, 0:2],
                             reads=[row, self.ident], writes=[pj], signal=(jb == 3))
                    pj3 = pj[:, 0:8].rearrange("p (j two) -> p j two", two=2)
                    P.cp("dve", idx[:].unsqueeze(2), pj3[:, :, 0:1], reads=[pj], writes=[idx])
                    P.cp("act", gt[:].unsqueeze(2), pj3[:, :, 1:2], reads=[pj], writes=[gt])
                    if self.cfg.get("moe_stage", "full") == "c1":
                        continue
                    xsT = rxsT.next()
                    for jb in range(4):
                        xs = rxs.next()
                        P.idma(xs[:], None, self.H2[:, :], bass.IndirectOffsetOnAxis(ap=idx[:, jb:jb + 1], axis=0),
                               reads=[idx, self.H2k], writes=[xs])
                        pb = self.psum.next()
                        for c in range(8):
                            P.tr(self.psbf(pb)[:, c * 128:(c + 1) * 128], xs[:, c * 128:(c + 1) * 128], self.identb[:],
                                 reads=[xs, self.identb], writes=[pb], signal=(c == 7))
                        P.cp("act" if jb % 2 == 0 else "dve", xsT[:, :, jb * 128:(jb + 1) * 128],
                             self.psbf(pb)[:, 0:1024].rearrange("p (c t) -> p c t", t=128), reads=[pb], writes=[xsT])
                    if self.cfg.get("moe_stage", "full") == "c2":
                        continue
                    hid = rhid.next()
                    wd = rwd.next()
                    P.dma("pool", wd[:], w_down[i, ex].rearrange("(fc p) d -> p fc d", p=128), writes=[wd])
                    for fg in range(4):
                        wg, wu = rwg.next(), rwu.next()
                        P.dma("pool", wg[:], w_gate[i, ex, :, fg * 512:(fg + 1) * 512].rearrange("(c p) f -> p c f", p=128),
                              writes=[wg])
                        P.dma("pool", wu[:], w_up[i, ex, :, fg * 512:(fg + 1) * 512].rearrange("(c p) f -> p c f", p=128),
                              writes=[wu])
                        for f4 in range(4):
                            fc = fg * 4 + f4
                            pg, pu = self.psum.next(), self.psum.next()
                            for c in range(8):
                                P.mm(pg[:, :], lhsT=wg[:, c, f4 * 128:(f4 + 1) * 128], rhs=xsT[:, c, :],
                                     start=(c == 0), stop=(c == 7), reads=[wg, xsT], writes=[pg], signal=(c == 7))
                            for c in range(8):
                                P.mm(pu[:, :], lhsT=wu[:, c, f4 * 128:(f4 + 1) * 128], rhs=xsT[:, c, :],
                                     start=(c == 0), stop=(c == 7), reads=[wu, xsT], writes=[pu], signal=(c == 7))
                            sg = rsg.next()
                            P.act(sg[:], pg[:, :], AF.Silu, reads=[pg], writes=[sg])
                            P.tt("dve", hid[:, fc, :], pu[:, :], sg[:], ALU.mult, reads=[pu, sg], writes=[hid])
                    if self.cfg.get("moe_stage", "full") == "c3":
                        continue
                    for jb in range(4):
                        ys = rys.next()
                        for hf in range(2):
                            py = self.psum.next()
                            for fc in range(16):
                                P.mm(py[:, :], lhsT=hid[:, fc, jb * 128:(jb + 1) * 128], rhs=wd[:, fc, hf * 512:(hf + 1) * 512],
                                     start=(fc == 0), stop=(fc == 15), reads=[hid, wd], writes=[py], signal=(fc == 15))
                            P.stt(ys[:, hf * 512:(hf + 1) * 512], py[:, :], gt[:, jb:jb + 1],
                                  self.modG(1)[:, hf * 512:(hf + 1) * 512], ALU.mult, ALU.mult,
                                  reads=[py, gt, self.mod], writes=[ys])
                        P.idma(self.X[:, :], bass.IndirectOffsetOnAxis(ap=idx[:, jb:jb + 1], axis=0), ys[:], None,
                               reads=[ys, idx], writes=self.Xk, compute_op=ALU.add)
            P.barrier()

    def psbf(self, pb):
        return pb.t.bitcast(BF16)

    def phase_final(self, norm_final, y_out, final_norm=True):
        P = self.P
        with ExitStack() as st:
            rx = self.ring(st, "f_x", 3, [128, D])
            rh = self.ring(st, "f_h", 2, [128, D])
            rstat = self.ring(st, "f_st", 3, [128, 8])
            nf = self.sb(st, "f_nf", [128, D])
            epsb = self.sb(st, "f_eps", [128, 1])
            P.memset("dve", epsb[:], EPS, writes=[epsb])
            P.dma("sp", nf[:], norm_final[0:1, :].broadcast_to([128, D]), writes=[nf])
            outk = []
            for tt in range(NT):
                xt = rx.next()
                P.dma("sp", xt[:], self.X[tt * 128:(tt + 1) * 128, :], reads=[self.Xk[tt]], writes=[xt])
                if final_norm:
                    h, stat = rh.next(), rstat.next()
                    P.act(h[:], xt[:], AF.Square, reads=[xt], writes=[h, stat], accum_out=stat[:, 0:1])
                    P.act(stat[:, 1:2], stat[:, 0:1], AF.Ln, reads=[stat], writes=[stat], scale=1.0 / D, bias=epsb[:, 0:1])
                    P.act(stat[:, 2:3], stat[:, 1:2], AF.Exp, reads=[stat], writes=[stat], scale=-0.5)
                    P.stt(xt[:], xt[:], stat[:, 2:3], nf[:], ALU.mult, ALU.mult, reads=[xt, stat, nf], writes=[xt])
                k = Tok()
                P.dma("sp", y_out[tt * 128:(tt + 1) * 128, :], xt[:], reads=[xt], writes=[k])
                outk.append(k)
            P.final_wait("sp", outk + self.outk)


def host_consts():
    c = {}
    c["ident"] = np.eye(128, dtype=np.float32)
    c["iota512"] = np.ascontiguousarray(np.broadcast_to(np.arange(512, dtype=np.float32)[None, :], (128, 512)))
    c["tokid"] = np.ascontiguousarray((np.arange(NT)[None, :] * 128 + np.arange(128)[:, None]).astype(np.float32))
    return c


def unit_inputs(inp, unit, pos_table):
    f32 = np.float32
    d = {}
    if unit == 0:
        d["x0"] = np.ascontiguousarray(np.asarray(inp["x_prompt"], f32).reshape(T, D))
        d["pos"] = np.zeros((T, D), f32)
        cond = np.asarray(inp["c_ctx"], f32)
        d["flag"] = np.zeros((128, 1), f32)
        d["capv"] = np.full((16, 16), 32.0, f32)
    else:
        b = unit - 1
        d["x0"] = np.ascontiguousarray(np.asarray(inp["x_sample"], f32)[b])
        d["pos"] = pos_table
        cond = np.asarray(inp["c"], f32)[b]
        d["flag"] = np.ones((128, 1), f32)
        d["capv"] = np.full((16, 16), 512.0, f32)
    d["condT"] = np.ascontiguousarray(cond.reshape(8, 128).T)
    return d


def s5_host(inp, unit):
    f32 = np.float32
    out = {}

    def gp(a):
        a = np.asarray(a, f32).reshape(2, 2, 32, 2, 64)
        return np.ascontiguousarray(a.transpose(0, 3, 4, 1, 2).reshape(2, 128, 2, 32))

    out["s5_lre"] = gp(inp["s5_lam_re"])
    out["s5_lim"] = gp(inp["s5_lam_im"])
    ldt = np.asarray(inp["s5_log_dt"], f32)
    out["s5_ldt"] = gp(np.broadcast_to(ldt[..., None], (2, 2, 64, 64)))
    if unit == 0:
        out["s5_h0r"] = np.zeros((2, 128, 2, 32), f32)
        out["s5_h0i"] = np.zeros((2, 128, 2, 32), f32)
    else:
        out["s5_h0r"] = gp(np.asarray(inp["state_s5_re"], f32)[unit - 1])
        out["s5_h0i"] = gp(np.asarray(inp["state_s5_im"], f32)[unit - 1])
    out["s5_dsk"] = np.ascontiguousarray(np.asarray(inp["s5_d"], f32).reshape(2, 8, 128).transpose(0, 2, 1))
    for nm, key in (("s5_bre", "s5_b_re"), ("s5_bim", "s5_b_im")):
        b = np.asarray(inp[key], f32).reshape(2, 2, 8, 4, 2, 64, 16)
        o = np.zeros((2, 2, 64, 2, 8, 4, 2, 16), f32)
        for gl in range(2):
            o[:, gl, :, :, :, :, gl, :] = b[:, :, :, :, gl].transpose(0, 4, 1, 2, 3, 5)
        out[nm] = np.ascontiguousarray(o.reshape(2, 128, 2, 8, 128))
    for nm, key in (("s5_cre", "s5_c_re"), ("s5_cim", "s5_c_im")):
        cc = np.asarray(inp[key], f32).reshape(2, 2, 8, 4, 2, 16, 64)
        o = np.zeros((2, 2, 64, 2, 8, 4, 4, 2, 16), f32)
        for gl in range(2):
            for q in range(4):
                o[:, gl, :, :, :, q, q, gl, :] = cc[:, :, :, q, gl].transpose(0, 4, 1, 2, 3)
        out[nm] = np.ascontiguousarray(o.reshape(2, 128, 2, 32, 128))
    out["s5_w_glu"] = np.asarray(inp["s5_w_glu"], f32)
    out["s5_b_glu"] = np.asarray(inp["s5_b_glu"], f32)
    out["tau256"] = np.ascontiguousarray(np.broadcast_to(np.arange(1, 257, dtype=f32)[None, :], (128, 256)))
    return out


def mixer_host(inp, unit):
    d = {}
    d.update(s5_host(inp, unit))
    d.update(hg_host(inp, unit))
    d.update(ssd_host(inp, unit))
    return d


def hg_host(inp, unit):
    f32 = np.float32
    d = {}
    d["hg_w_qig"] = np.asarray(inp["hg_w_qig"], f32)
    d["hg_w_f"] = np.asarray(inp["hg_w_f"], f32)
    d["hg_w_o"] = np.asarray(inp["hg_w_o"], f32)
    d["hg_bfT"] = np.ascontiguousarray(np.asarray(inp["hg_b_f"], f32).reshape(1, 2, 8, 128).transpose(0, 3, 1, 2))
    d["hg_gn"] = np.ascontiguousarray(np.asarray(inp["hg_norm"], f32).reshape(1, 128, 1))
    d["hg_lbl"] = np.ascontiguousarray(np.asarray(inp["hg_lb_logits"], f32).reshape(2, 4, 8, 128).transpose(3, 0, 1, 2))
    if unit == 0:
        d["hg_s0"] = np.zeros((2, 8, 128, 128), f32)
    else:
        d["hg_s0"] = np.ascontiguousarray(np.asarray(inp["state_hgrn"], f32)[unit - 1, 0])
    p = np.arange(128)
    same = (p[:, None] // 32) == (p[None, :] // 32)
    d["hg_maskF"] = (same & ((p[None, :] % 32) >= (p[:, None] % 32))).astype(f32)
    d["hg_maskB"] = (same & ((p[None, :] % 32) <= (p[:, None] % 32))).astype(f32)
    rm = np.ones((128, 512), f32); rm[:, ::32] = 0.0
    d["hg_rmask"] = rm
    return d


def ssd_host(inp, unit):
    f32 = np.float32
    d = {}
    d["ssd_w_in"] = np.asarray(inp["ssd_w_in"], f32)
    d["ssd_w_out"] = np.asarray(inp["ssd_w_out"], f32)
    d["ssd_cwT"] = np.ascontiguousarray(np.asarray(inp["ssd_conv_w"], f32).reshape(1, 5, 24, 128).transpose(0, 3, 2, 1))
    d["ssd_cbT"] = np.ascontiguousarray(np.asarray(inp["ssd_conv_b"], f32).reshape(1, 24, 128).transpose(0, 2, 1))
    d["ssd_dtb"] = np.ascontiguousarray(np.asarray(inp["ssd_dt_bias"], f32).reshape(1, 64))
    d["ssd_alog"] = np.ascontiguousarray(np.asarray(inp["ssd_a_log"], f32).reshape(1, 64))
    d["ssd_d"] = np.asarray(inp["ssd_d"], f32).reshape(1, 32)
    d["ssd_norm"] = np.asarray(inp["ssd_norm"], f32).reshape(1, 2048)
    if unit == 0:
        d["ssd_h0"] = np.zeros((128, 2, 4, 512), f32)
    else:
        h0 = np.asarray(inp["state_ssd"], f32)[unit - 1, 0]
        d["ssd_h0"] = np.ascontiguousarray(h0.reshape(2, 4, 8, 64, 128).transpose(4, 0, 1, 2, 3).reshape(128, 2, 4, 512))
    p = np.arange(128)
    same = (p[:, None] // 64) == (p[None, :] // 64)
    r, t = p[:, None], p[None, :]
    d["ssd_TriF"] = (same & (r <= t)).astype(f32)
    d["ssd_UF"] = (same & (r > t)).astype(f32)
    d["ssd_TriB"] = (same & (r >= t)).astype(f32)
    d["ssd_UB"] = (same & (r < t)).astype(f32)
    d["ssd_negF"] = np.where(same & (t >= r), 0.0, -30000.0).astype(f32)
    d["ssd_negB"] = np.where(same & (t <= r), 0.0, -30000.0).astype(f32)
    d["ssd_Ones0"] = np.ascontiguousarray(np.broadcast_to((p[:, None] < 64), (128, 128))).astype(f32)
    d["ssd_Ones1"] = np.ascontiguousarray(np.broadcast_to((p[:, None] >= 64), (128, 128))).astype(f32)
    return d


def pos_table():
    quarter = D // 4
    omega = (1.0 / (10000.0 ** (np.arange(quarter, dtype=np.float32) / np.float32(quarter)))).astype(np.float32)
    r = np.arange(64, dtype=np.float32)[:, None] * omega
    emb_r = np.concatenate([np.sin(r), np.cos(r)], axis=-1).astype(np.float32)
    emb = np.concatenate([np.broadcast_to(emb_r[:, None], (64, 64, D // 2)),
                          np.broadcast_to(emb_r[None], (64, 64, D // 2))], axis=-1)
    return np.ascontiguousarray(emb.reshape(T, D).astype(np.float32))


_CACHE = {}
UNIT_OF_CORE = [0, 3, 1, 3, 2, 3, 3, 3]


def kernel(**inputs):
    f32 = np.float32
    if "nc" not in _CACHE:
        b = Builder(dict())
        _CACHE["nc"] = b.build()
        _CACHE["b"] = b
    nc = _CACHE["nc"]
    pt = pos_table()
    shared = host_consts()
    for k in ["w_ada", "b_ada", "norm_mix", "norm_ffn", "moe_router", "moe_w_gate", "moe_w_up", "moe_w_down"]:
        shared[k] = np.asarray(inputs[k], f32)
    shared["norm_final"] = np.asarray(inputs["norm_final"], f32).reshape(1, -1)
    units = {}
    for u in range(3):
        d = dict(shared)
        d.update(unit_inputs(inputs, u, pt))
        d.update(mixer_host(inputs, u))
        units[u] = d
    units[3] = units[0]
    in_maps = [units[u] for u in UNIT_OF_CORE]
    res = run_bass_kernel_spmd(nc, in_maps, core_ids=list(range(8)))
    R = res.results
    c0, c1, c2 = UNIT_OF_CORE.index(0), UNIT_OF_CORE.index(1), UNIT_OF_CORE.index(2)
    y_prompt = np.asarray(R[c0]["y"], f32).reshape(16, 256, D)
    y_sample = np.stack([np.asarray(R[c1]["y"], f32), np.asarray(R[c2]["y"], f32)], axis=0)
    new_s5_re = np.asarray(R[c0]["o_s5re"], f32)
    new_s5_im = np.asarray(R[c0]["o_s5im"], f32)
    new_hgrn = np.asarray(R[c0]["o_hg"], f32)
    new_ssd = np.asarray(R[c0]["o_ssd"], f32)
    return (y_prompt, y_sample, new_s5_re, new_s5_im, new_hgrn, new_ssd)
```

```python
import numpy as np
from contextlib import ExitStack
import concourse.bass as bass
import concourse.mybir as mybir
from concourse.bass_utils import run_bass_kernel_spmd

F32 = mybir.dt.float32
BF16 = mybir.dt.bfloat16
I32 = mybir.dt.int32
AF = mybir.ActivationFunctionType
ALU = mybir.AluOpType
AX = mybir.AxisListType

SEM_LIMIT = 30000
T = 4096
D = 1024
NT = 32
NSEG = 16
SEG = 256
DEPTH = 4
NE = 16
FF = 2048
EPS = 1e-6


class Tok:
    __slots__ = ("w", "r")

    def __init__(self):
        self.w = None
        self.r = []


class Buf:
    def __init__(self, t):
        self.t = t
        self.k = Tok()

    def __getitem__(self, key):
        return self.t[key]


def _tok(x):
    return x.k if isinstance(x, Buf) else x


class Prog:
    ENG = ("pe", "act", "dve", "pool", "sp")

    def __init__(self, nc, es):
        self.nc = nc
        self.es = es
        self.streams = {e: [] for e in self.ENG}
        self.nsem = 0
        self.csem = {}
        self.ccnt = {}
        for e in ("pe", "act", "dve", "pool"):
            self.csem[e] = self.new_sem(e)
            self.ccnt[e] = 0
        self.dslots = {}
        for q, n in (("sp", 12), ("pool", 6), ("act", 4)):
            self.dslots[q] = [[self.new_sem("d" + q), 0] for _ in range(n)]
        self.dnext = {"sp": 0, "pool": 0, "act": 0}
        self.seen = {e: {} for e in self.ENG}
        self.ninst = 0
        self.pending = {}
        self.cprev = {}

    def new_sem(self, name):
        self.nsem += 1
        return self.es.enter_context(self.nc.semaphore(f"{name}_{self.nsem}"))

    def _collect(self, eng, reads, writes):
        evs = []
        for t in reads:
            t = _tok(t)
            if t.w is not None:
                evs.append(t.w)
        for t in writes:
            t = _tok(t)
            if t.w is not None:
                evs.append(t.w)
            evs.extend(t.r)
        seen = self.seen[eng]
        best = {}
        for (src, sem, val) in evs:
            if eng == "pe" and src == "pe":
                continue
            if src == "pe" and sem is self.csem["pe"] and val > self.ccnt["pe"]:
                raise RuntimeError("wait on a pending (unsignaled) PE event")
            k = id(sem)
            if seen.get(k, -1) >= val:
                continue
            if k not in best or best[k][1] < val:
                best[k] = (sem, val)
        for k, (sem, val) in best.items():
            seen[k] = val
        return list(best.values())

    def _commit(self, ev, reads, writes):
        for t in reads:
            _tok(t).r.append(ev)
        for t in writes:
            t = _tok(t)
            t.w = ev
            t.r = []

    def op(self, eng, fn, reads=(), writes=(), signal=True):
        waits = self._collect(eng, reads, writes)
        if self.ccnt[eng] + 1 > SEM_LIMIT and signal and not self.pending.get(eng, False):
            self.cprev[eng] = (self.csem[eng], self.ccnt[eng])
            self.csem[eng] = self.new_sem(eng)
            self.ccnt[eng] = 0
        self.pending[eng] = not signal
        if signal:
            self.ccnt[eng] += 1
            sem = self.csem[eng]
            ev = (eng, sem, self.ccnt[eng])
            self.streams[eng].append((waits, fn, (sem, 1)))
        else:
            assert eng == "pe"
            ev = (eng, self.csem[eng], self.ccnt[eng] + 1)
            self.streams[eng].append((waits, fn, None))
        self._commit(ev, reads, writes)
        self.ninst += 1
        return ev

    def _dma_common(self, q, fn, reads, writes):
        slots = self.dslots[q]
        i = self.dnext[q]
        self.dnext[q] = (i + 1) % len(slots)
        slot = slots[i]
        waits = self._collect(q, reads, writes)
        sem, cnt = slot
        if cnt > 0:
            k = id(sem)
            if self.seen[q].get(k, -1) < cnt:
                self.seen[q][k] = cnt
                waits.append((sem, cnt))
        if cnt + 16 > SEM_LIMIT:
            sem = self.new_sem("d" + q)
            cnt = 0
        cnt += 16
        slot[0], slot[1] = sem, cnt
        ev = ("dma", sem, cnt)
        self.streams[q].append((waits, fn, (sem, 16)))
        self._commit(ev, reads, writes)
        self.ninst += 1
        return ev

    def dma(self, q, out, in_, reads=(), writes=(), **kw):
        return self._dma_common(q, lambda e: e.dma_start(out=out, in_=in_, **kw), reads, writes)

    def idma(self, out, out_off, in_, in_off, reads=(), writes=(), **kw):
        return self._dma_common(
            "pool",
            lambda e: e.indirect_dma_start(out=out, out_offset=out_off, in_=in_, in_offset=in_off, **kw),
            reads, writes)

    def barrier(self):
        evs = []
        for e in ("pe", "act", "dve", "pool"):
            if self.ccnt[e] > 0:
                evs.append((self.csem[e], self.ccnt[e]))
            elif e in self.cprev:
                evs.append(self.cprev[e])
        for q in self.dslots:
            for sem, cnt in self.dslots[q]:
                if cnt > 0:
                    evs.append((sem, cnt))
        for eng in self.ENG:
            waits = []
            for sem, val in evs:
                k = id(sem)
                if self.seen[eng].get(k, -1) >= val:
                    continue
                if eng == "pe" and sem is self.csem["pe"]:
                    continue
                self.seen[eng][k] = val
                waits.append((sem, val))
            if waits:
                self.streams[eng].append((waits, None, None))

    def final_wait(self, eng, toks):
        waits = self._collect(eng, toks, ())
        self.streams[eng].append((waits, None, None))

    def mm(self, out, lhsT, rhs, start=True, stop=True, reads=(), writes=(), signal=True, **kw):
        return self.op("pe", lambda e: e.matmul(out, lhsT=lhsT, rhs=rhs, start=start, stop=stop, **kw),
                       reads, writes, signal)

    def tr(self, out, in_, ident, reads=(), writes=(), signal=True):
        return self.op("pe", lambda e: e.transpose(out, in_, ident), reads, writes, signal)

    def act(self, out, in_, func, reads=(), writes=(), **kw):
        return self.op("act", lambda e: e.activation(out=out, in_=in_, func=func, **kw), reads, writes)

    def tt(self, eng, out, in0, in1, op, reads=(), writes=()):
        return self.op(eng, lambda e: e.tensor_tensor(out=out, in0=in0, in1=in1, op=op), reads, writes)

    def ts(self, eng, out, in0, s1, s2, op0, op1=None, reads=(), writes=(), accum_out=None):
        if op1 is None:
            op1 = ALU.bypass
        if accum_out is None:
            return self.op(eng, lambda e: e.tensor_scalar(out=out, in0=in0, scalar1=s1, scalar2=s2, op0=op0, op1=op1),
                           reads, writes)
        return self.op(eng, lambda e: e.tensor_scalar(out=out, in0=in0, scalar1=s1, scalar2=s2, op0=op0, op1=op1,
                                                       accum_out=accum_out), reads, writes)

    def stt(self, out, in0, scalar, in1, op0, op1, reads=(), writes=()):
        return self.op("dve", lambda e: e.scalar_tensor_tensor(out=out, in0=in0, scalar=scalar, in1=in1, op0=op0, op1=op1),
                       reads, writes)

    def cp(self, eng, out, in_, reads=(), writes=()):
        if eng == "act":
            return self.op("act", lambda e: e.copy(out=out, in_=in_), reads, writes)
        return self.op(eng, lambda e: e.tensor_copy(out=out, in_=in_), reads, writes)

    def memset(self, eng, ap, val, writes=()):
        return self.op(eng, lambda e: e.memset(ap, val), (), writes)

    def scan(self, out, d0, d1, init, op0, op1, reads=(), writes=()):
        return self.op("dve", lambda e: e.tensor_tensor_scan(out=out, data0=d0, data1=d1, initial=init, op0=op0, op1=op1),
                       reads, writes)

    def emit(self):
        nc = self.nc
        streams = self.streams
        with nc.Block() as block:
            def run(e, lst):
                for waits, fn, inc in lst:
                    for sem, val in waits:
                        e.wait_ge(sem, val)
                    if fn is None:
                        continue
                    ins = fn(e)
                    if inc is not None:
                        ins.then_inc(inc[0], inc[1])

            @block.sync
            def _(e):
                run(e, streams["sp"])

            @block.tensor
            def _(e):
                run(e, streams["pe"])

            @block.scalar
            def _(e):
                run(e, streams["act"])

            @block.vector
            def _(e):
                run(e, streams["dve"])

            @block.gpsimd
            def _(e):
                run(e, streams["pool"])


class Ring:
    check = False

    def __init__(self, bufs):
        self.bufs = bufs
        self.i = 0

    def next(self):
        b = self.bufs[self.i]
        self.i = (self.i + 1) % len(self.bufs)
        if self.check and b.k.w is not None and len(b.k.r) == 0:
            raise RuntimeError("PSUM bank handed out again before its previous contents were read")
        return b


class Builder:
    def __init__(self, cfg):
        self.cfg = cfg
        self.nc = bass.Bass("TRN2", target_bir_lowering=False)
        self.din = {}
        self.dout = {}

    def inp(self, name, shape, dt=F32):
        self.din[name] = self.nc.dram_tensor(name, list(shape), dt, kind="ExternalInput")
        return self.din[name]

    def outp(self, name, shape, dt=F32):
        self.dout[name] = self.nc.dram_tensor(name, list(shape), dt, kind="ExternalOutput")
        return self.dout[name]

    def scratch(self, name, shape, dt=F32):
        return self.nc.dram_tensor(name, list(shape), dt, kind="Internal")

    def _nm(self, name):
        self._cnt = getattr(self, "_cnt", 0) + 1
        return f"s{self._cnt}_{name}"

    def sb(self, st, name, shape, dt=F32):
        return Buf(st.enter_context(self.nc.sbuf_tensor(self._nm(name), list(shape), dt)))

    def ps(self, st, name, shape, dt=F32):
        return Buf(st.enter_context(self.nc.psum_tensor(self._nm(name), list(shape), dt)))

    def ring(self, st, name, n, shape, dt=F32, psum=False):
        f = self.ps if psum else self.sb
        return Ring([f(st, f"{name}{i}", shape, dt) for i in range(n)])

    def build(self):
        nc = self.nc
        cfg = self.cfg
        layers = cfg.get("layers", list(range(DEPTH)))
        x0 = self.inp("x0", [T, D])
        pos = self.inp("pos", [T, D])
        condT = self.inp("condT", [128, 8])
        flag = self.inp("flag", [128, 1])
        capv = self.inp("capv", [16, 16])
        ident_d = self.inp("ident", [128, 128])
        iota_d = self.inp("iota512", [128, 512])
        tokid_d = self.inp("tokid", [128, NT])
        w_ada = self.inp("w_ada", [DEPTH, D, 6 * D])
        b_ada = self.inp("b_ada", [DEPTH, 6 * D])
        norm_mix = self.inp("norm_mix", [DEPTH, D])
        norm_ffn = self.inp("norm_ffn", [DEPTH, D])
        norm_final = self.inp("norm_final", [1, D])
        moe_router = self.inp("moe_router", [DEPTH, D, NE])
        moe_w_gate = self.inp("moe_w_gate", [DEPTH, NE, D, FF])
        moe_w_up = self.inp("moe_w_up", [DEPTH, NE, D, FF])
        moe_w_down = self.inp("moe_w_down", [DEPTH, NE, FF, D])
        y_out = self.outp("y", [T, D])

        self.X = self.scratch("Xres", [T, D])
        self.Xk = [Tok() for _ in range(NT)]
        self.H2 = self.scratch("H2", [T, D], BF16)
        self.H2k = Tok()
        self.modrow = self.scratch("modrow", [1, 6 * D])
        self.HT = self.scratch("HT", [D, T], BF16)
        self.HTk = Tok()
        self.YG = self.scratch("YG", [D, T], BF16)
        self.YGk = Tok()
        self.outk = []
        self.inp("s5_lre", [2, 128, 2, 32]); self.inp("s5_lim", [2, 128, 2, 32]); self.inp("s5_ldt", [2, 128, 2, 32])
        self.inp("s5_h0r", [2, 128, 2, 32]); self.inp("s5_h0i", [2, 128, 2, 32]); self.inp("s5_dsk", [2, 128, 8])
        self.inp("s5_bre", [2, 128, 2, 8, 128]); self.inp("s5_bim", [2, 128, 2, 8, 128])
        self.inp("s5_cre", [2, 128, 2, 32, 128]); self.inp("s5_cim", [2, 128, 2, 32, 128])
        self.inp("s5_w_glu", [2, D, 2 * D]); self.inp("s5_b_glu", [2, 2 * D])
        self.inp("tau256", [128, 256])
        self.outp("o_s5re", [NSEG, 2, 2, 64, 64]); self.outp("o_s5im", [NSEG, 2, 2, 64, 64])
        self.inp("hg_w_qig", [1, D, 3 * D]); self.inp("hg_w_f", [1, 2, D, D]); self.inp("hg_w_o", [1, D, D])
        self.inp("hg_bfT", [1, 128, 2, 8]); self.inp("hg_gn", [1, 128, 1]); self.inp("hg_lbl", [128, 2, 4, 8])
        self.inp("hg_s0", [2, 8, 128, 128])
        self.inp("hg_maskF", [128, 128]); self.inp("hg_maskB", [128, 128]); self.inp("hg_rmask", [128, 512])
        self.outp("o_hg", [NSEG, 1, 2, 8, 128, 128])
        self.OF = self.scratch("OF", [8, 128, T])
        self.OFk = Tok()
        self.inp("ssd_w_in", [1, D, 5184]); self.inp("ssd_w_out", [1, 2 * D, D])
        self.inp("ssd_cwT", [1, 128, 24, 5]); self.inp("ssd_cbT", [1, 128, 24])
        self.inp("ssd_dtb", [1, 64]); self.inp("ssd_alog", [1, 64]); self.inp("ssd_d", [1, 32]); self.inp("ssd_norm", [1, 2 * D])
        self.inp("ssd_h0", [128, 2, 4, 512])
        for nm in ("TriF", "UF", "TriB", "UB", "negF", "negB", "Ones0", "Ones1"):
            self.inp("ssd_" + nm, [128, 128])
        self.outp("o_ssd", [NSEG, 1, 2, 32, 64, 128])
        self.XBC = self.scratch("XBC", [3 * D, T]); self.XBCk = Tok()
        self.XC = self.scratch("XC", [3 * D, T], BF16); self.XCk = Tok()
        self.YF = self.scratch("YF", [T, 2 * D]); self.YFk = Tok()
        self.modk = Tok()

        with ExitStack() as es:
            P = Prog(nc, es)
            self.P = P
            g = ExitStack()
            es.enter_context(g)
            self.ident = self.sb(g, "ident", [128, 128])
            self.identb = self.sb(g, "identb", [128, 128], BF16)
            self.flag = self.sb(g, "flag", [128, 1])
            self.mod = self.sb(g, "mod", [128, 6 * D])
            self.psum = self.ring(g, "psb", 8, [128, 512], F32, psum=True)
            self.psum.check = True
            self.psum8 = self.psum
            self.epsb = self.sb(g, "epsb", [128, 1])
            P.memset("dve", self.epsb[:], EPS, writes=[self.epsb])
            P.dma("sp", self.ident[:], ident_d[:, :], writes=[self.ident])
            P.dma("sp", self.flag[:], flag[:, :], writes=[self.flag])
            P.cp("dve", self.identb[:], self.ident[:], reads=[self.ident], writes=[self.identb])

            if cfg.get("init", True):
                self.phase_init(x0, pos)
            else:
                self.phase_init(x0, None)
            for i in layers:
                self.phase_mod(i, condT, w_ada, b_ada, norm_mix, norm_ffn)
                if cfg.get("mixer", True):
                    self.phase_mixer(i)
                if cfg.get("moe", True):
                    self.phase_moe(i, moe_router, moe_w_gate, moe_w_up, moe_w_down, capv, iota_d, tokid_d)
            self.phase_final(norm_final, y_out, final_norm=cfg.get("final_norm", True))
            P.emit()
        return nc

    def phase_init(self, x0, pos):
        P = self.P
        with ExitStack() as st:
            ra = self.ring(st, "ia", 3, [128, D])
            rb = self.ring(st, "ib", 3, [128, D])
            for tt in range(NT):
                a = ra.next()
                P.dma("sp", a[:], x0[tt * 128:(tt + 1) * 128, :], writes=[a])
                if pos is not None:
                    b = rb.next()
                    P.dma("sp", b[:], pos[tt * 128:(tt + 1) * 128, :], writes=[b])
                    P.tt("dve", a[:], a[:], b[:], ALU.add, reads=[a, b], writes=[a])
                P.dma("sp", self.X[tt * 128:(tt + 1) * 128, :], a[:], reads=[a], writes=[self.Xk[tt]])
            P.barrier()

    def phase_mod(self, i, condT, w_ada, b_ada, norm_mix, norm_ffn):
        P = self.P
        with ExitStack() as st:
            sc = self.sb(st, "m_sc", [128, 8])
            sg = self.sb(st, "m_sg", [128, 8])
            brow = self.sb(st, "m_brow", [1, 6 * D])
            mrow = self.sb(st, "m_mrow", [1, 6 * D])
            wr = self.ring(st, "m_w", 3, [128, 3 * D])
            nrm = self.sb(st, "m_nrm", [128, 2 * D])
            P.dma("sp", sc[:], condT[:, :], writes=[sc])
            P.dma("sp", brow[:], b_ada[i:i + 1, :], writes=[brow])
            P.act(sg[:], sc[:], AF.Sigmoid, reads=[sc], writes=[sg])
            P.tt("dve", sc[:], sc[:], sg[:], ALU.mult, reads=[sc, sg], writes=[sc])
            P.dma("sp", nrm[:, 0:D], norm_mix[i:i + 1, :].broadcast_to([128, D]), writes=[nrm])
            P.dma("sp", nrm[:, D:2 * D], norm_ffn[i:i + 1, :].broadcast_to([128, D]), writes=[nrm])
            for half in range(2):
                banks = [self.psum.next() for _ in range(6)]
                for kc in range(8):
                    w = wr.next()
                    P.dma("sp", w[:], w_ada[i, kc * 128:(kc + 1) * 128, half * 3 * D:(half + 1) * 3 * D], writes=[w])
                    for cb in range(6):
                        P.mm(banks[cb][0:1, :], lhsT=sc[:, kc:kc + 1], rhs=w[:, cb * 512:(cb + 1) * 512],
                             start=(kc == 0), stop=(kc == 7), reads=[sc, w], writes=[banks[cb]], signal=(kc == 7 or cb == 5))
                for cb in range(6):
                    c0 = half * 3 * D + cb * 512
                    P.tt("dve", mrow[0:1, c0:c0 + 512], banks[cb][0:1, :], brow[0:1, c0:c0 + 512], ALU.add,
                         reads=[banks[cb], brow], writes=[mrow])
            P.dma("sp", self.modrow[:, :], mrow[:], reads=[mrow], writes=[self.modk])
            P.dma("sp", self.mod[:], self.modrow[0:1, :].broadcast_to([128, 6 * D]), reads=[self.modk], writes=[self.mod])
            m = self.mod
            P.stt(m[:, D:2 * D], m[:, D:2 * D], 1.0, nrm[:, 0:D], ALU.add, ALU.mult, reads=[m, nrm], writes=[m])
            P.stt(m[:, 4 * D:5 * D], m[:, 4 * D:5 * D], 1.0, nrm[:, D:2 * D], ALU.add, ALU.mult, reads=[m, nrm], writes=[m])
            P.barrier()

    def modA(self, sub):
        return self.mod[:, (3 * sub + 1) * D:(3 * sub + 2) * D]

    def modB(self, sub):
        return self.mod[:, (3 * sub) * D:(3 * sub + 1) * D]

    def modG(self, sub):
        return self.mod[:, (3 * sub + 2) * D:(3 * sub + 3) * D]

    def norm_tile(self, tt, sub, xt, h, stat):
        P = self.P
        P.dma("sp", xt[:], self.X[tt * 128:(tt + 1) * 128, :], reads=[self.Xk[tt]], writes=[xt])
        P.act(h[:], xt[:], AF.Square, reads=[xt], writes=[h, stat], accum_out=stat[:, 0:1])
        P.act(stat[:, 1:2], stat[:, 0:1], AF.Ln, reads=[stat], writes=[stat], scale=1.0 / D, bias=self.epsb[:, 0:1])
        P.act(stat[:, 2:3], stat[:, 1:2], AF.Exp, reads=[stat], writes=[stat], scale=-0.5)
        P.stt(h[:], xt[:], stat[:, 2:3], self.modA(sub), ALU.mult, ALU.mult, reads=[xt, stat, self.mod], writes=[h])
        P.tt("pool", h[:], h[:], self.modB(sub), ALU.add, reads=[h, self.mod], writes=[h])

    def phase_mixer(self, i):
        self.build_hT()
        kind, j = i % 3, i // 3
        if kind == 0:
            self.phase_s5(i, j)
        elif kind == 1:
            self.phase_hgrn(i, j)
        else:
            self.phase_ssd(i, j)

    def build_hT(self):
        P = self.P
        HTv = self.HT.ap().rearrange("(c p) t -> p c t", p=128)
        with ExitStack() as st:
            rx = self.ring(st, "p_x", 2, [128, D])
            rh = self.ring(st, "p_h", 2, [128, D])
            rhb = self.ring(st, "p_hb", 2, [128, D], BF16)
            rhT = self.ring(st, "p_hT", 2, [128, 8, 512], BF16)
            rstat = self.ring(st, "p_st", 3, [128, 8])
            for tt in range(NT):
                xt, h, hb, stat = rx.next(), rh.next(), rhb.next(), rstat.next()
                if tt % 4 == 0:
                    hT = rhT.next()
                self.norm_tile(tt, 0, xt, h, stat)
                P.cp("act", hb[:], h[:], reads=[h], writes=[hb])
                pb = self.psum.next()
                for c in range(8):
                    P.tr(self.psbf(pb)[:, c * 128:(c + 1) * 128], hb[:, c * 128:(c + 1) * 128], self.identb[:],
                         reads=[hb, self.identb], writes=[pb], signal=(c == 7))
                t4 = tt % 4
                P.cp("dve", hT[:, :, t4 * 128:(t4 + 1) * 128],
                     self.psbf(pb)[:, 0:1024].rearrange("p (c t) -> p c t", t=128), reads=[pb], writes=[hT])
                if self.cfg.get("s5_dbg") and tt == 0:
                    self.dbg("d_h", h[:], [128, D], h); self.dbg("d_hb", hb[:], [128, D], hb, BF16)
                    self.dbg("d_stat", stat[:], [128, 8], stat); self.dbg("d_xt", xt[:], [128, D], xt)
                    self.dbg("d_modA", self.modA(0), [128, D], self.mod); self.dbg("d_modB", self.modB(0), [128, D], self.mod)
                if t4 == 3:
                    t0 = (tt - 3) * 128
                    P.dma("sp", HTv[:, :, t0:t0 + 512], hT[:], reads=[hT], writes=[self.HTk])
                    if self.cfg.get("s5_dbg") and tt == 3:
                        self.dbg("d_hT", hT[:], [128, 8, 512], hT, BF16)
            P.barrier()

    def phase_s5(self, i, j):
        P = self.P
        di = self.din
        TWO_PI = 2.0 * np.pi
        self._sc = {}
        with ExitStack() as st:
            so_re = self.sb(st, "so_re", [128, NSEG, 2, 32])
            so_im = self.sb(st, "so_im", [128, NSEG, 2, 32])
            with ExitStack() as s1:
                lre = self.sb(s1, "lre", [128, 2, 32]); lim = self.sb(s1, "lim", [128, 2, 32])
                ldt = self.sb(s1, "ldt", [128, 2, 32])
                h0r = self.sb(s1, "h0r", [128, 2, 32]); h0i = self.sb(s1, "h0i", [128, 2, 32])
                dsk = self.sb(s1, "dsk", [128, 8])
                rr = self.sb(s1, "rr", [128, 2, 32]); th = self.sb(s1, "th", [128, 2, 32])
                cr = self.sb(s1, "cr", [128, 2, 32]); ci = self.sb(s1, "ci", [128, 2, 32])
                tmp = [self.sb(s1, f"tmp{k}", [128, 2, 32]) for k in range(6)]
                P.dma("sp", lre[:], di["s5_lre"][j], writes=[lre])
                P.dma("sp", lim[:], di["s5_lim"][j], writes=[lim])
                P.dma("sp", ldt[:], di["s5_ldt"][j], writes=[ldt])
                P.dma("sp", h0r[:], di["s5_h0r"][j], writes=[h0r])
                P.dma("sp", h0i[:], di["s5_h0i"][j], writes=[h0i])
                P.dma("sp", dsk[:], di["s5_dsk"][j], writes=[dsk])
                dt_ = tmp[0]
                P.act(dt_[:], ldt[:], AF.Exp, reads=[ldt], writes=[dt_])
                P.tt("dve", rr[:], lre[:], dt_[:], ALU.mult, reads=[lre, dt_], writes=[rr])
                P.act(rr[:], rr[:], AF.Exp, reads=[rr], writes=[rr])
                P.tt("dve", th[:], lim[:], dt_[:], ALU.mult, reads=[lim, dt_], writes=[th])
                P.ts("dve", th[:], th[:], 1.0 / TWO_PI, None, ALU.mult, reads=[th], writes=[th])
                cs, sn = tmp[1], tmp[2]
                self.sincos_turns(s1, th, [128, 2, 32], cs, sn, "pp")
                nr, ni, den = tmp[3], tmp[4], tmp[5]
                P.tt("dve", nr[:], rr[:], cs[:], ALU.mult, reads=[rr, cs], writes=[nr])
                P.ts("dve", nr[:], nr[:], -1.0, None, ALU.add, reads=[nr], writes=[nr])
                P.tt("dve", ni[:], rr[:], sn[:], ALU.mult, reads=[rr, sn], writes=[ni])
                P.tt("dve", den[:], lre[:], lre[:], ALU.mult, reads=[lre], writes=[den])
                P.tt("dve", cs[:], lim[:], lim[:], ALU.mult, reads=[lim], writes=[cs])
                P.tt("dve", den[:], den[:], cs[:], ALU.add, reads=[den, cs], writes=[den])
                P.op("dve", lambda e: e.reciprocal(out=den[:], in_=den[:]), reads=[den], writes=[den])
                P.tt("dve", cr[:], nr[:], lre[:], ALU.mult, reads=[nr, lre], writes=[cr])
                P.tt("dve", cs[:], ni[:], lim[:], ALU.mult, reads=[ni, lim], writes=[cs])
                P.tt("dve", cr[:], cr[:], cs[:], ALU.add, reads=[cr, cs], writes=[cr])
                P.tt("dve", cr[:], cr[:], den[:], ALU.mult, reads=[cr, den], writes=[cr])
                P.tt("dve", ci[:], ni[:], lre[:], ALU.mult, reads=[ni, lre], writes=[ci])
                P.tt("dve", cs[:], nr[:], lim[:], ALU.mult, reads=[nr, lim], writes=[cs])
                P.tt("dve", ci[:], ci[:], cs[:], ALU.subtract, reads=[ci, cs], writes=[ci])
                P.tt("dve", ci[:], ci[:], den[:], ALU.mult, reads=[ci, den], writes=[ci])
                BT = self.sb(s1, "BT", [128, 2, 2, 8, 128], BF16)
                with ExitStack() as s2:
                    bre = self.sb(s2, "bre", [128, 8, 128]); bim = self.sb(s2, "bim", [128, 8, 128])
                    o1 = self.sb(s2, "o1", [128, 8, 128]); o2 = self.sb(s2, "o2", [128, 8, 128])
                    ob = self.sb(s2, "ob", [128, 8, 128], BF16)
                    for d in range(2):
                        P.dma("sp", bre[:], di["s5_bre"][j, :, d], writes=[bre])
                        P.dma("sp", bim[:], di["s5_bim"][j, :, d], writes=[bim])
                        v4 = lambda b: b[:].rearrange("p c (q x) -> p c q x", x=32)
                        bc = lambda t: t[:, d, :].rearrange("p (c q) -> p c q", q=4).unsqueeze(3).to_broadcast([128, 8, 4, 32])
                        for reim in range(2):
                            a, b_ = (bre, bim) if reim == 0 else (bim, bre)
                            P.tt("dve", v4(o1), v4(a), bc(cr), ALU.mult, reads=[a, cr], writes=[o1])
                            P.tt("dve", v4(o2), v4(b_), bc(ci), ALU.mult, reads=[b_, ci], writes=[o2])
                            P.tt("dve", ob[:], o1[:], o2[:], ALU.subtract if reim == 0 else ALU.add,
                                 reads=[o1, o2], writes=[ob])
                            for c in range(8):
                                pb = self.psum.next()
                                P.tr(self.psbf(pb)[:, 0:128], ob[:, c, :], self.identb[:], reads=[ob, self.identb], writes=[pb])
                                P.cp("act", BT[:, d, reim, c, :], self.psbf(pb)[:, 0:128], reads=[pb], writes=[BT])
                    P.barrier()
                ycs = self.ring(s1, "y_c", 1, [128, T])
                hTc_r = self.ring(s1, "hTc", 2, [128, T], BF16)
                ygc_r = self.ring(s1, "ygc", 1, [128, T], BF16)
                Cre_r = self.ring(s1, "Cre", 2, [128, 2, 4, 128]); Cim_r = self.ring(s1, "Cim", 2, [128, 2, 4, 128])
                CCs = [[self.sb(s1, f"CC{d}{q}", [128, 256]) for q in range(4)] for d in range(2)]
                SSs = [[self.sb(s1, f"SS{d}{q}", [128, 512]) for q in range(4)] for d in range(2)]
                tau = self.sb(s1, "tau", [128, 256]); ang = self.sb(s1, "ang", [128, 256])
                P.dma("sp", tau[:], di["tau256"][:, :], writes=[tau])
                rbu = self.ring(s1, "bu", 5, [128, 512]); rA = self.ring(s1, "rA", 8, [128, 512])
                rB = self.ring(s1, "rB", 8, [128, 512]); rg = self.ring(s1, "rg", 4, [128, 512])
                rh32 = self.ring(s1, "h32", 5, [128, 512])
                cars = [[self.ring(s1, f"car{d}{q}", 2, [128, 2]) for q in range(4)] for d in range(2)]
                HTv = self.HT.ap().rearrange("(c p) t -> p c t", p=128)
                swp = lambda b: b[:].rearrange("p (two x) -> p two x", two=2)[:, ::-1, :]
                v3 = lambda b: b[:].rearrange("p (two x) -> p two x", two=2)
                for c in range(8):
                    y_c, hTc, Cre, Cim = ycs.next(), hTc_r.next(), Cre_r.next(), Cim_r.next()
                    P.dma("sp", hTc[:], HTv[:, c, :], reads=[self.HTk], writes=[hTc])
                    P.dma("sp", Cre[:], di["s5_cre"][j, :, :, 4 * c:4 * c + 4, :], writes=[Cre])
                    P.dma("sp", Cim[:], di["s5_cim"][j, :, :, 4 * c:4 * c + 4, :], writes=[Cim])
                    P.ts("pool", Cim[:], Cim[:], -1.0, None, ALU.mult, reads=[Cim], writes=[Cim])
                    P.act(y_c[:], hTc[:], AF.Identity, reads=[hTc, dsk], writes=[y_c], scale=dsk[:, c:c + 1])
                    car = [[None] * 4 for _ in range(2)]
                    for d in range(2):
                        for q in range(4):
                            stx = 4 * c + q
                            CC, SS = CCs[d][q], SSs[d][q]
                            P.ts("dve", ang[:], tau[:], th[:, d, stx:stx + 1], None, ALU.mult, reads=[tau, th], writes=[ang])
                            self.sincos_turns(s1, ang, [128, 256], CC[:, :], SS[:, 0:256], f"t{c}{d}{q}", outk=[CC, SS])
                            P.ts("pool", SS[:, 256:512], SS[:, 0:256], -1.0, None, ALU.mult, reads=[SS], writes=[SS])
                            cr_ = cars[d][q].next()
                            P.cp("dve", cr_[:, 0:1], h0r[:, d, stx:stx + 1], reads=[h0r], writes=[cr_])
                            P.cp("dve", cr_[:, 1:2], h0i[:, d, stx:stx + 1], reads=[h0i], writes=[cr_])
                            car[d][q] = cr_
                    def front(kk, d):
                        sgi = kk if d == 0 else NSEG - 1 - kk
                        t0 = sgi * SEG
                        L = []
                        for q in range(4):
                            CC, SS = CCs[d][q], SSs[d][q]
                            ccb = CC[:].unsqueeze(1).to_broadcast([128, 2, 256])
                            u = hTc[32 * q:32 * q + 32, t0:t0 + SEG]
                            if d == 1:
                                u = u[:, ::-1]
                            pb = self.psum.next()
                            P.mm(pb[:, 0:256], lhsT=BT[32 * q:32 * q + 32, d, 0, c, :], rhs=u, reads=[BT, hTc],
                                 writes=[pb], signal=False, tile_position=(32 * q, 0))
                            P.mm(pb[:, 256:512], lhsT=BT[32 * q:32 * q + 32, d, 1, c, :], rhs=u, reads=[BT, hTc],
                                 writes=[pb], tile_position=(32 * q, 0))
                            bu, A_, B_ = rbu.next(), rA.next(), rB.next()
                            P.cp("act", bu[:], pb[:, :], reads=[pb], writes=[bu])
                            L.append((bu, A_, B_, CC, SS, ccb))
                        for q in range(4):
                            bu, A_, B_, CC, SS, ccb = L[q]
                            P.tt("dve", v3(A_), v3(bu), ccb, ALU.mult, reads=[bu, CC], writes=[A_])
                            P.tt("pool", v3(B_), swp(bu), v3(SS), ALU.mult, reads=[bu, SS], writes=[B_])
                        return L

                    def back(kk, d, L):
                        sgi = kk if d == 0 else NSEG - 1 - kk
                        t0 = sgi * SEG
                        G = []
                        for q in range(4):
                            bu, A_, B_, CC, SS, ccb = L[q]
                            P.tt("dve", A_[:], A_[:], B_[:], ALU.add, reads=[A_, B_], writes=[A_])
                        for q in range(4):
                            bu, A_, B_, CC, SS, ccb = L[q]
                            stx = 4 * c + q
                            rtb = rr[:, d, stx:stx + 1].to_broadcast([128, 256])
                            g_ = rg.next()
                            cr_ = car[d][q]
                            P.scan(g_[:, 0:256], rtb, A_[:, 0:256], cr_[:, 0:1], ALU.mult, ALU.add,
                                   reads=[rr, A_, cr_], writes=[g_])
                            P.scan(g_[:, 256:512], rtb, A_[:, 256:512], cr_[:, 1:2], ALU.mult, ALU.add,
                                   reads=[rr, A_, cr_], writes=[g_])
                            P.tt("pool", v3(B_), swp(g_), v3(SS), ALU.mult, reads=[g_, SS], writes=[B_])
                            G.append(g_)
                        for q in range(4):
                            bu, A_, B_, CC, SS, ccb = L[q]
                            P.tt("dve", v3(A_), v3(G[q]), ccb, ALU.mult, reads=[G[q], CC], writes=[A_])
                        Hs = []
                        for q in range(4):
                            bu, A_, B_, CC, SS, ccb = L[q]
                            h32 = rh32.next()
                            P.tt("dve", h32[:], A_[:], B_[:], ALU.subtract, reads=[A_, B_], writes=[h32])
                            Hs.append(h32)
                        for q in range(4):
                            stx = 4 * c + q
                            h32 = Hs[q]
                            cr_ = cars[d][q].next()
                            car[d][q] = cr_
                            P.cp("act", so_re[:, sgi, d, stx:stx + 1], h32[:, 255:256], reads=[h32], writes=[so_re])
                            P.cp("act", so_im[:, sgi, d, stx:stx + 1], h32[:, 511:512], reads=[h32], writes=[so_im])
                            P.ts("dve", cr_[:, 0:1], h32[:, 255:256], self.flag[:, 0:1], None, ALU.mult,
                                 reads=[h32, self.flag], writes=[cr_])
                            P.ts("dve", cr_[:, 1:2], h32[:, 511:512], self.flag[:, 0:1], None, ALU.mult,
                                 reads=[h32, self.flag], writes=[cr_])
                        for q in range(4):
                            h32 = Hs[q]
                            ysl = y_c[:, t0:t0 + SEG]
                            if d == 1:
                                ysl = ysl[:, ::-1]
                            py = self.psum.next()
                            P.mm(py[:, 0:256], lhsT=Cre[:, d, q, :], rhs=h32[:, 0:256], start=True, stop=False,
                                 reads=[Cre, h32], writes=[py], signal=False)
                            P.mm(py[:, 0:256], lhsT=Cim[:, d, q, :], rhs=h32[:, 256:512], start=False, stop=True,
                                 reads=[Cim, h32], writes=[py])
                            P.tt("dve", ysl, py[:, 0:256], ysl, ALU.add, reads=[py, y_c], writes=[y_c])

                    groups = [(kk, d) for kk in range(NSEG) for d in range(2)]
                    Lcur = front(*groups[0])
                    for gi_, (kk, d) in enumerate(groups):
                        Lnext = front(*groups[gi_ + 1]) if gi_ + 1 < len(groups) else None
                        back(kk, d, Lcur)
                        Lcur = Lnext
                    ygc = ygc_r.next()
                    P.act(ygc[:], y_c[:], AF.Gelu_apprx_tanh, reads=[y_c], writes=[ygc])
                    P.dma("sp", self.YG[c * 128:(c + 1) * 128, :], ygc[:], reads=[ygc], writes=[self.YGk])
                if self.cfg.get("state_out", True):
                    rso = self.ring(s1, "sot", 3, [32, 128])
                    for (so, name) in ((so_re, "o_s5re"), (so_im, "o_s5im")):
                        for sgi in range(NSEG):
                            for d in range(2):
                                pb = self.psum.next()
                                P.tr(pb[0:32, 0:128], so[:, sgi, d, :], self.ident[:], reads=[so, self.ident], writes=[pb])
                                sot = rso.next()
                                P.cp("act", sot[:], pb[0:32, 0:128], reads=[pb], writes=[sot])
                                k = Tok()
                                P.dma("sp", self.dout[name][sgi, j, d].rearrange("(st gl) p -> st (gl p)", gl=2), sot[:],
                                      reads=[sot], writes=[k])
                                self.outk.append(k)
                P.barrier()
            with ExitStack() as s3:
                wglu = self.sb(s3, "wglu", [128, 8, 2 * D], BF16)
                bglu = self.sb(s3, "bglu", [128, 2 * D])
                for c in range(8):
                    P.dma("pool", wglu[:, c, :], di["s5_w_glu"][j, c * 128:(c + 1) * 128, :], writes=[wglu])
                P.dma("sp", bglu[:], di["s5_b_glu"][j:j + 1, :].broadcast_to([128, 2 * D]), writes=[bglu])
                rx = self.ring(s3, "g_x", 2, [128, D])
                ra = self.ring(s3, "g_a", 2, [128, D]); rb = self.ring(s3, "g_b", 2, [128, D])
                ryg = self.ring(s3, "g_yg", 2, [128, 8, 512], BF16)
                YGv = self.YG.ap().rearrange("(c p) t -> p c t", p=128)
                for tt in range(NT):
                    xt, ta, tb = rx.next(), ra.next(), rb.next()
                    if tt % 4 == 0:
                        ygb = ryg.next()
                        P.dma("sp", ygb[:], YGv[:, :, tt * 128:tt * 128 + 512], reads=[self.YGk], writes=[ygb])
                    P.dma("sp", xt[:], self.X[tt * 128:(tt + 1) * 128, :], reads=[self.Xk[tt]], writes=[xt])
                    banks = [self.psum.next() for _ in range(4)]
                    for cb in range(4):
                        for c in range(8):
                            P.mm(banks[cb][:, :], lhsT=ygb[:, c, (tt % 4) * 128:(tt % 4 + 1) * 128], rhs=wglu[:, c, cb * 512:(cb + 1) * 512],
                                 start=(c == 0), stop=(c == 7), reads=[ygb, wglu], writes=[banks[cb]], signal=(c == 7))
                    for hf in range(2):
                        sl = slice(hf * 512, (hf + 1) * 512)
                        sl2 = slice(D + hf * 512, D + (hf + 1) * 512)
                        P.tt("dve", tb[:, sl], banks[2 + hf][:, :], bglu[:, sl2], ALU.add, reads=[banks[2 + hf], bglu], writes=[tb])
                        P.act(tb[:, sl], tb[:, sl], AF.Sigmoid, reads=[tb], writes=[tb])
                        P.tt("dve", ta[:, sl], banks[hf][:, :], bglu[:, sl], ALU.add, reads=[banks[hf], bglu], writes=[ta])
                        P.tt("pool", ta[:, sl], ta[:, sl], tb[:, sl], ALU.mult, reads=[ta, tb], writes=[ta])
                    P.tt("pool", ta[:], ta[:], self.modG(0), ALU.mult, reads=[ta, self.mod], writes=[ta])
                    P.tt("dve", xt[:], xt[:], ta[:], ALU.add, reads=[xt, ta], writes=[xt])
                    P.dma("sp", self.X[tt * 128:(tt + 1) * 128, :], xt[:], reads=[xt], writes=[self.Xk[tt]])
                P.barrier()

    def phase_hgrn(self, i, j):
        P = self.P
        di = self.din
        HTv = self.HT.ap().rearrange("(c p) t -> p c t", p=128)
        with ExitStack() as st:
            lbt = self.sb(st, "hg_lb", [128, 2, 8]); oml = self.sb(st, "hg_oml", [128, 2, 8])
            bf = self.sb(st, "hg_bf", [128, 2, 8]); gn = self.sb(st, "hg_gn", [128, 1])
            maskF = self.sb(st, "hg_mF", [128, 128]); maskB = self.sb(st, "hg_mB", [128, 128])
            rmask = self.sb(st, "hg_rm", [128, 512]); ones = self.sb(st, "hg_ones", [128, 128])
            P.dma("sp", bf[:], di["hg_bfT"][j], writes=[bf])
            P.dma("sp", gn[:], di["hg_gn"][j], writes=[gn])
            P.dma("sp", maskF[:], di["hg_maskF"][:, :], writes=[maskF])
            P.dma("sp", maskB[:], di["hg_maskB"][:, :], writes=[maskB])
            P.dma("sp", rmask[:], di["hg_rmask"][:, :], writes=[rmask])
            P.memset("pool", ones[:], 1.0, writes=[ones])
            with ExitStack() as s0:
                lg = self.sb(s0, "hg_lg", [128, 2, 4, 8]); tot = self.sb(s0, "hg_tot", [128, 2, 8])
                P.dma("sp", lg[:], di["hg_lbl"][:, :, :, :], writes=[lg])
                P.act(lg[:], lg[:], AF.Exp, reads=[lg], writes=[lg])
                P.op("dve", lambda e: e.reduce_sum(out=tot[:], in_=lg[:].rearrange("p d l c -> p d c l"), axis=AX.X),
                     reads=[lg], writes=[tot])
                P.op("dve", lambda e: e.reciprocal(out=tot[:], in_=tot[:]), reads=[tot], writes=[tot])
                if i >= 1:
                    P.op("dve", lambda e: e.reduce_sum(out=lbt[:], in_=lg[:, :, 1:i + 1, :].rearrange("p d l c -> p d c l"),
                                                       axis=AX.X), reads=[lg], writes=[lbt])
                    P.tt("dve", lbt[:], lbt[:], tot[:], ALU.mult, reads=[lbt, tot], writes=[lbt])
                else:
                    P.memset("dve", lbt[:], 0.0, writes=[lbt])
                P.ts("dve", oml[:], lbt[:], -1.0, 1.0, ALU.mult, ALU.add, reads=[lbt], writes=[oml])
                P.barrier()
            wq = self.sb(st, "hg_wq", [128, 8, D], BF16); wv = self.sb(st, "hg_wv", [128, 8, D], BF16)
            for c in range(8):
                P.dma("pool", wq[:, c, :], di["hg_w_qig"][j, c * 128:(c + 1) * 128, 0:D], writes=[wq])
                P.dma("pool", wv[:, c, :], di["hg_w_qig"][j, c * 128:(c + 1) * 128, D:2 * D], writes=[wv])
            S32 = self.sb(st, "hg_S", [128, 8, 128]); Sbf = self.sb(st, "hg_Sbf", [128, 8, 128], BF16)
            rhT = self.ring(st, "hg_hT", 2, [128, 8, 512], BF16)
            rvt = self.ring(st, "hg_vt", 2, [128, 4, 8, 128], BF16)
            r32 = self.ring(st, "hg_a", 8, [128, 512])
            r16 = self.ring(st, "hg_b", 6, [128, 512], BF16)
            rklT = self.ring(st, "hg_klT", 2, [128, 4, 128], BF16)
            rsc = self.ring(st, "hg_sc", 3, [128, 128], BF16)
            rel = self.ring(st, "hg_el", 2, [128, 16])
            rso = self.ring(st, "hg_so", 2, [128, 128])
            for d in range(2):
                with ExitStack() as s1:
                    wf = self.sb(s1, "hg_wf", [128, 8, D], BF16)
                    for c in range(8):
                        P.dma("pool", wf[:, c, :], di["hg_w_f"][j, d, c * 128:(c + 1) * 128, :], writes=[wf])
                    if d == 1:
                        wg = self.sb(s1, "hg_wg", [128, 8, D], BF16); wo = self.sb(s1, "hg_wo", [128, 8, D], BF16)
                        for c in range(8):
                            P.dma("pool", wg[:, c, :], di["hg_w_qig"][j, c * 128:(c + 1) * 128, 2 * D:3 * D], writes=[wg])
                            P.dma("pool", wo[:, c, :], di["hg_w_o"][j, c * 128:(c + 1) * 128, :], writes=[wo])
                        og = self.sb(s1, "hg_og", [128, 8, 512], BF16)
                        rx = self.ring(s1, "hg_x", 2, [128, D]); rta = self.ring(s1, "hg_ta", 2, [128, D])
                    for hd in range(8):
                        P.dma("sp", S32[:, hd, :], di["hg_s0"][d, hd], writes=[S32])
                    P.cp("act", Sbf[:], S32[:], reads=[S32], writes=[Sbf])
                    mask = maskF if d == 0 else maskB
                    tbs = range(8) if d == 0 else range(7, -1, -1)
                    for tb in tbs:
                        hT, vt = rhT.next(), rvt.next()
                        P.dma("sp", hT[:], HTv[:, :, tb * 512:(tb + 1) * 512], reads=[self.HTk], writes=[hT])
                        for g4 in range(4):
                            for hf in range(2):
                                pb = self.psum.next()
                                for c in range(8):
                                    P.mm(pb[:, :], lhsT=hT[:, c, g4 * 128:(g4 + 1) * 128], rhs=wv[:, c, hf * 512:(hf + 1) * 512],
                                         start=(c == 0), stop=(c == 7), reads=[hT, wv], writes=[pb], signal=(c == 7))
                                P.cp("act", vt[:, g4, hf * 4:(hf + 1) * 4, :].rearrange("p a b -> p (a b)"), pb[:, :],
                                     reads=[pb], writes=[vt])
                        for hd in range(8):
                            hs = slice(hd * 128, (hd + 1) * 128)
                            pq, pf = self.psum.next(), self.psum.next()
                            for c in range(8):
                                P.mm(pq[:, :], lhsT=wq[:, c, hs], rhs=hT[:, c, :], start=(c == 0), stop=(c == 7),
                                     reads=[wq, hT], writes=[pq], signal=(c == 7))
                            for c in range(8):
                                P.mm(pf[:, :], lhsT=wf[:, c, hs], rhs=hT[:, c, :], start=(c == 0), stop=(c == 7),
                                     reads=[wf, hT], writes=[pf], signal=(c == 7))
                            f_, lf, km, cum, ex = r32.next(), r32.next(), r32.next(), r32.next(), r32.next()
                            qe, ke, kl = r16.next(), r16.next(), r16.next()
                            el = rel.next()
                            P.act(f_[:], pf[:, :], AF.Sigmoid, reads=[pf, bf], writes=[f_], bias=bf[:, d, hd:hd + 1])
                            P.ts("dve", f_[:], f_[:], oml[:, d, hd:hd + 1], lbt[:, d, hd:hd + 1], ALU.mult, ALU.add,
                                 reads=[f_, oml, lbt], writes=[f_])
                            P.act(lf[:], f_[:], AF.Ln, reads=[f_], writes=[lf])
                            P.ts("pool", km[:], f_[:], -1.0, 1.0, ALU.mult, ALU.add, reads=[f_], writes=[km])
                            P.scan(cum[:], rmask[:], lf[:], 0.0, ALU.mult, ALU.add, reads=[rmask, lf], writes=[cum])
                            last = cum[:].rearrange("p (n c) -> p n c", c=32)[:, :, 31:32]
                            P.act(el[:].unsqueeze(2), last, AF.Exp, reads=[cum], writes=[el])
                            if d == 1:
                                P.tt("dve", lf[:], lf[:], cum[:], ALU.subtract, reads=[lf, cum], writes=[lf])
                                P.tt("pool", cum[:].rearrange("p (n c) -> p n c", c=32),
                                     lf[:].rearrange("p (n c) -> p n c", c=32), last.to_broadcast([128, 16, 32]), ALU.add,
                                     reads=[lf, cum], writes=[cum])
                            P.act(ex[:], cum[:], AF.Exp, reads=[cum], writes=[ex])
                            P.tt("dve", qe[:], pq[:, :], ex[:], ALU.mult, reads=[pq, ex], writes=[qe])
                            P.act(ex[:], cum[:], AF.Exp, reads=[cum], writes=[ex], scale=-1.0)
                            P.tt("pool", ke[:], km[:], ex[:], ALU.mult, reads=[km, ex], writes=[ke])
                            P.tt("pool", kl[:].rearrange("p (n c) -> p n c", c=32), ke[:].rearrange("p (n c) -> p n c", c=32),
                                 el[:].unsqueeze(2).to_broadcast([128, 16, 32]), ALU.mult, reads=[ke, el], writes=[kl])
                            klT = rklT.next()
                            pt = self.psum.next()
                            for g4 in range(4):
                                P.tr(self.psbf(pt)[:, g4 * 128:(g4 + 1) * 128], kl[:, g4 * 128:(g4 + 1) * 128], self.identb[:],
                                     reads=[kl, self.identb], writes=[pt], signal=(g4 == 3))
                            P.cp("act", klT[:].rearrange("p a b -> p (a b)"), self.psbf(pt)[:, 0:512], reads=[pt], writes=[klT])
                            if d == 1:
                                pg = self.psum.next()
                                for c in range(8):
                                    P.mm(pg[:, :], lhsT=wg[:, c, hs], rhs=hT[:, c, :], start=(c == 0), stop=(c == 7),
                                         reads=[wg, hT], writes=[pg], signal=(c == 7))
                                sgt = r32.next()
                                P.act(sgt[:], pg[:, :], AF.Silu, reads=[pg], writes=[sgt])
                                ofw = r32.next()
                                P.dma("sp", ofw[:], self.OF[hd, :, tb * 512:(tb + 1) * 512], reads=[self.OFk], writes=[ofw])
                            else:
                                ofw = r32.next()
                            g4s = range(4) if d == 0 else range(3, -1, -1)
                            for g4 in g4s:
                                gs = slice(g4 * 128, (g4 + 1) * 128)
                                psc = self.psum.next()
                                P.mm(psc[:, 0:128], lhsT=ke[:, gs], rhs=qe[:, gs], reads=[ke, qe], writes=[psc])
                                scT = rsc.next()
                                P.tt("dve", scT[:], psc[:, 0:128], mask[:], ALU.mult, reads=[psc, mask], writes=[scT])
                                po = self.psum.next()
                                P.mm(po[:, 0:128], lhsT=vt[:, g4, hd, :], rhs=scT[:], start=True, stop=False,
                                     reads=[vt, scT], writes=[po], signal=False)
                                js = range(4) if d == 0 else range(3, -1, -1)
                                for jn, jc in enumerate(js):
                                    cs = slice(g4 * 128 + jc * 32, g4 * 128 + jc * 32 + 32)
                                    ci = tb * 16 + g4 * 4 + jc
                                    P.mm(po[:, jc * 32:jc * 32 + 32], lhsT=Sbf[:, hd, :], rhs=qe[:, cs], start=False, stop=(jn == 3),
                                         reads=[Sbf, qe], writes=[po], signal=(jn == 3), skip_group_check=True)
                                    pS = self.psum.next()
                                    P.mm(pS[:, 0:128], lhsT=klT[32 * jc:32 * jc + 32, g4, :], rhs=vt[32 * jc:32 * jc + 32, g4, hd, :],
                                         reads=[klT, vt], writes=[pS], tile_position=(32 * jc, 0))
                                    P.stt(S32[:, hd, :], S32[:, hd, :], el[:, g4 * 4 + jc:g4 * 4 + jc + 1], pS[:, 0:128],
                                          ALU.mult, ALU.add, reads=[S32, el, pS], writes=[S32])
                                    seg_done = ((ci + 1) % 8 == 0) if d == 0 else (ci % 8 == 0)
                                    if seg_done:
                                        sgi = ci // 8
                                        if self.cfg.get("state_out", True):
                                            so = rso.next()
                                            P.cp("act", so[:], S32[:, hd, :], reads=[S32], writes=[so])
                                            k = Tok()
                                            P.dma("sp", self.dout["o_hg"][sgi, j, d, hd], so[:], reads=[so], writes=[k])
                                            self.outk.append(k)
                                        P.ts("pool", S32[:, hd, :], S32[:, hd, :], self.flag[:, 0:1], None, ALU.mult,
                                             reads=[S32, self.flag], writes=[S32])
                                    P.cp("act", Sbf[:, hd, :], S32[:, hd, :], reads=[S32], writes=[Sbf])
                                if d == 0:
                                    P.cp("act", ofw[:, gs], po[:, 0:128], reads=[po], writes=[ofw])
                                else:
                                    P.tt("dve", ofw[:, gs], po[:, 0:128], ofw[:, gs], ALU.add, reads=[po, ofw], writes=[ofw])
                            if d == 0:
                                P.dma("sp", self.OF[hd, :, tb * 512:(tb + 1) * 512], ofw[:], reads=[ofw], writes=[self.OFk])
                            else:
                                sq = r32.next()
                                P.act(sq[:], ofw[:], AF.Square, reads=[ofw], writes=[sq])
                                pn = self.psum.next()
                                P.mm(pn[:, :], lhsT=ones[:], rhs=sq[:], reads=[ones, sq], writes=[pn])
                                P.act(sq[:], pn[:, :], AF.Ln, reads=[pn], writes=[sq], scale=1.0 / 128, bias=self.epsb[:, 0:1])
                                P.act(sq[:], sq[:], AF.Exp, reads=[sq], writes=[sq], scale=-0.5)
                                P.tt("dve", ofw[:], ofw[:], sq[:], ALU.mult, reads=[ofw, sq], writes=[ofw])
                                P.stt(og[:, hd, :], ofw[:], gn[:, 0:1], sgt[:], ALU.mult, ALU.mult, reads=[ofw, gn, sgt], writes=[og])
                        if d == 1:
                            for g4 in range(4):
                                tt = tb * 4 + g4
                                xt, ta = rx.next(), rta.next()
                                P.dma("sp", xt[:], self.X[tt * 128:(tt + 1) * 128, :], reads=[self.Xk[tt]], writes=[xt])
                                for hf in range(2):
                                    pb = self.psum.next()
                                    for hd in range(8):
                                        P.mm(pb[:, :], lhsT=og[:, hd, g4 * 128:(g4 + 1) * 128], rhs=wo[:, hd, hf * 512:(hf + 1) * 512],
                                             start=(hd == 0), stop=(hd == 7), reads=[og, wo], writes=[pb], signal=(hd == 7))
                                    P.tt("dve", ta[:, hf * 512:(hf + 1) * 512], pb[:, :], self.modG(0)[:, hf * 512:(hf + 1) * 512],
                                         ALU.mult, reads=[pb, self.mod], writes=[ta])
                                P.tt("pool", xt[:], xt[:], ta[:], ALU.add, reads=[xt, ta], writes=[xt])
                                P.dma("sp", self.X[tt * 128:(tt + 1) * 128, :], xt[:], reads=[xt], writes=[self.Xk[tt]])
                    P.barrier()

    def phase_ssd(self, i, j):
        P = self.P
        di = self.din
        HTv = self.HT.ap().rearrange("(c p) t -> p c t", p=128)
        XI = 2 * D
        NXBC = 3 * D
        with ExitStack() as st:
            wx = self.sb(st, "sd_wx", [128, 8, NXBC], BF16)
            for c in range(8):
                P.dma("pool", wx[:, c, :], di["ssd_w_in"][j, c * 128:(c + 1) * 128, XI:XI + NXBC], writes=[wx])
            rhT = self.ring(st, "sd_hT", 2, [128, 8, 512], BF16)
            ro = self.ring(st, "sd_o", 4, [128, 512])
            for tb in range(8):
                hT = rhT.next()
                P.dma("sp", hT[:], HTv[:, :, tb * 512:(tb + 1) * 512], reads=[self.HTk], writes=[hT])
                for ct in range(24):
                    pb = self.psum.next()
                    for c in range(8):
                        P.mm(pb[:, :], lhsT=wx[:, c, ct * 128:(ct + 1) * 128], rhs=hT[:, c, :], start=(c == 0), stop=(c == 7),
                             reads=[wx, hT], writes=[pb], signal=(c == 7))
                    o = ro.next()
                    P.cp("act" if ct % 2 == 0 else "dve", o[:], pb[:, :], reads=[pb], writes=[o])
                    P.dma("sp", self.XBC[ct * 128:(ct + 1) * 128, tb * 512:(tb + 1) * 512], o[:], reads=[o], writes=[self.XBCk])
            P.barrier()
        with ExitStack() as st:
            cw = self.sb(st, "sd_cw", [128, 24, 5]); cb = self.sb(st, "sd_cb", [128, 24])
            P.dma("sp", cw[:], di["ssd_cwT"][j], writes=[cw])
            P.dma("sp", cb[:], di["ssd_cbT"][j], writes=[cb])
            rxp = self.ring(st, "sd_xp", 2, [128, NSEG, SEG + 4])
            racc = self.ring(st, "sd_acc", 2, [128, NSEG, SEG])
            rob = self.ring(st, "sd_ob", 2, [128, NSEG, SEG], BF16)
            for ct in range(24):
                xp, acc, ob = rxp.next(), racc.next(), rob.next()
                P.dma("sp", xp[:, :, 2:2 + SEG], self.XBC[ct * 128:(ct + 1) * 128, :].rearrange("p (s t) -> p s t", t=SEG),
                      reads=[self.XBCk], writes=[xp])
                P.memset("pool", xp[:, 0:1, 0:2], 0.0, writes=[xp])
                P.memset("pool", xp[:, NSEG - 1:NSEG, SEG + 2:SEG + 4], 0.0, writes=[xp])
                P.ts("pool", xp[:, 1:NSEG, 0:2], xp[:, 0:NSEG - 1, SEG:SEG + 2], self.flag[:, 0:1], None, ALU.mult,
                     reads=[xp, self.flag], writes=[xp])
                P.ts("pool", xp[:, 0:NSEG - 1, SEG + 2:SEG + 4], xp[:, 1:NSEG, 2:4], self.flag[:, 0:1], None, ALU.mult,
                     reads=[xp, self.flag], writes=[xp])
                P.ts("dve", acc[:], xp[:, :, 0:SEG], cw[:, ct, 0:1], cb[:, ct:ct + 1], ALU.mult, ALU.add,
                     reads=[xp, cw, cb], writes=[acc])
                for k in range(1, 5):
                    P.stt(acc[:], xp[:, :, k:k + SEG], cw[:, ct, k:k + 1], acc[:], ALU.mult, ALU.add,
                          reads=[xp, cw, acc], writes=[acc])
                P.act(ob[:], acc[:], AF.Silu, reads=[acc], writes=[ob])
                P.dma("sp", self.XC[ct * 128:(ct + 1) * 128, :], ob[:].rearrange("p s t -> p (s t)"), reads=[ob], writes=[self.XCk])
            P.barrier()
        with ExitStack() as st:
            XCv = self.XC.ap().rearrange("(k p) t -> p k t", p=128)
            wdt = self.sb(st, "sd_wdt", [128, 8, 64], BF16)
            for c in range(8):
                P.dma("pool", wdt[:, c, :], di["ssd_w_in"][j, c * 128:(c + 1) * 128, XI + NXBC:XI + NXBC + 64], writes=[wdt])
            dtb = self.sb(st, "sd_dtb", [128, 64]); arow = self.sb(st, "sd_a", [128, 64]); dsk = self.sb(st, "sd_dsk", [128, 32])
            P.dma("sp", dtb[:], di["ssd_dtb"][j:j + 1, :].broadcast_to([128, 64]), writes=[dtb])
            P.dma("sp", arow[:], di["ssd_alog"][j:j + 1, :].broadcast_to([128, 64]), writes=[arow])
            P.dma("sp", dsk[:], di["ssd_d"][j:j + 1, :].broadcast_to([128, 32]), writes=[dsk])
            P.act(arow[:], arow[:], AF.Exp, reads=[arow], writes=[arow])
            P.ts("dve", arow[:], arow[:], -1.0, None, ALU.mult, reads=[arow], writes=[arow])
            cm = {}
            for nm in ("TriF", "UF", "TriB", "UB", "negF", "negB", "Ones0", "Ones1"):
                cm[nm] = self.sb(st, "sd_" + nm, [128, 128])
                P.dma("sp", cm[nm][:], di["ssd_" + nm][:, :], writes=[cm[nm]])
            H32 = self.sb(st, "sd_H32", [128, 4, 512]); Hbf = self.sb(st, "sd_Hbf", [128, 4, 512], BF16)
            rxf = self.ring(st, "sd_xf", 2, [128, 24, 128], BF16)
            rhg = self.ring(st, "sd_hg", 2, [128, 8, 128], BF16)
            rxt = self.ring(st, "sd_xt", 2, [128, 2048], BF16)
            rbt = self.ring(st, "sd_bt", 2, [128, 4, 128], BF16)
            rxd = self.ring(st, "sd_xd", 1, [128, 2048], BF16); rxe = self.ring(st, "sd_xe", 1, [128, 2048], BF16)
            rsm = self.ring(st, "sd_sm", 12, [128, 64])
            rl = self.ring(st, "sd_l", 2, [128, 8, 128])
            rcbt = self.ring(st, "sd_cbt", 2, [128, 128])
            rtm = self.ring(st, "sd_tm", 2, [128, 512]); rlm = self.ring(st, "sd_lm", 2, [128, 512])
            rmt = self.ring(st, "sd_mt", 2, [128, 4, 128], BF16)
            rya = self.ring(st, "sd_ya", 1, [128, 2048])
            rso = self.ring(st, "sd_so", 2, [64, 512])
            for d in range(2):
                with ExitStack() as s1:
                    if d == 1:
                        wz = self.sb(s1, "sd_wz", [128, 8, XI], BF16); wout = self.sb(s1, "sd_wo", [128, 16, D], BF16)
                        for c in range(8):
                            P.dma("pool", wz[:, c, :], di["ssd_w_in"][j, c * 128:(c + 1) * 128, 0:XI], writes=[wz])
                        for k in range(16):
                            P.dma("pool", wout[:, k, :], di["ssd_w_out"][j, k * 128:(k + 1) * 128, :], writes=[wout])
                        ng = self.sb(s1, "sd_ng", [128, XI])
                        P.dma("sp", ng[:], di["ssd_norm"][j:j + 1, :].broadcast_to([128, XI]), writes=[ng])
                        rsz = self.ring(s1, "sd_sz", 1, [128, XI]); ryn = self.ring(s1, "sd_yn", 1, [128, XI], BF16)
                        ryT = self.ring(s1, "sd_yT", 1, [128, 16, 128], BF16)
                        rx = self.ring(s1, "sd_x", 1, [128, D]); rta = self.ring(s1, "sd_ta", 1, [128, D])
                        rst = self.ring(s1, "sd_st", 2, [128, 8])
                    P.dma("sp", H32[:], di["ssd_h0"][:, d], writes=[H32])
                    P.cp("act", Hbf[:], H32[:], reads=[H32], writes=[Hbf])
                    Tri, U, neg = (cm["TriF"], cm["UF"], cm["negF"]) if d == 0 else (cm["TriB"], cm["UB"], cm["negB"])
                    gis = range(NT) if d == 0 else range(NT - 1, -1, -1)
                    for gi in gis:
                        ts_ = slice(gi * 128, (gi + 1) * 128)
                        xf, hg, xt, bt = rxf.next(), rhg.next(), rxt.next(), rbt.next()
                        P.dma("sp", xf[:], XCv[:, :, ts_], reads=[self.XCk], writes=[xf])
                        P.dma("sp", hg[:], HTv[:, :, ts_], reads=[self.HTk], writes=[hg])
                        for hb in range(2):
                            pb = self.psum.next()
                            for k in range(8):
                                P.tr(self.psbf(pb)[:, k * 128:(k + 1) * 128], xf[:, hb * 8 + k, :], self.identb[:],
                                     reads=[xf, self.identb], writes=[pb], signal=(k == 7))
                            P.cp("act" if hb == 0 else "dve", xt[:, hb * 1024:(hb + 1) * 1024], self.psbf(pb)[:, 0:1024],
                                 reads=[pb], writes=[xt])
                        pb = self.psum.next()
                        for g in range(4):
                            P.tr(self.psbf(pb)[:, g * 128:(g + 1) * 128], xf[:, 16 + g, :], self.identb[:],
                                 reads=[xf, self.identb], writes=[pb], signal=(g == 3))
                        P.cp("act", bt[:].rearrange("p a b -> p (a b)"), self.psbf(pb)[:, 0:512], reads=[pb], writes=[bt])
                        pdt = self.psum.next()
                        for c in range(8):
                            P.mm(pdt[:, 0:32], lhsT=hg[:, c, :], rhs=wdt[:, c, d * 32:(d + 1) * 32], start=(c == 0), stop=(c == 7),
                                 reads=[hg, wdt], writes=[pdt], signal=(c == 7))
                        dt_, dA, ecum, edec = rsm.next(), rsm.next(), rsm.next(), rsm.next()
                        cdec = rsm.next()
                        P.tt("dve", dt_[:, 0:32], pdt[:, 0:32], dtb[:, d * 32:(d + 1) * 32], ALU.add, reads=[pdt, dtb], writes=[dt_])
                        P.act(dt_[:, 0:32], dt_[:, 0:32], AF.Exp, reads=[dt_], writes=[dt_])
                        P.act(dt_[:, 0:32], dt_[:, 0:32], AF.Ln, reads=[dt_], writes=[dt_], bias=1.0)
                        P.tt("dve", dA[:, 0:32], dt_[:, 0:32], arow[:, d * 32:(d + 1) * 32], ALU.mult, reads=[dt_, arow], writes=[dA])
                        pc = self.psum.next()
                        P.mm(pc[:, 0:32], lhsT=Tri[:], rhs=dA[:, 0:32], reads=[Tri, dA], writes=[pc], signal=False)
                        P.mm(pc[:, 32:64], lhsT=U[:], rhs=dA[:, 0:32], reads=[U, dA], writes=[pc], signal=False)
                        P.mm(pc[:, 64:96], lhsT=cm["Ones0"][:], rhs=dA[:, 0:32], reads=[cm["Ones0"], dA], writes=[pc], signal=False)
                        P.mm(pc[:, 96:128], lhsT=cm["Ones1"][:], rhs=dA[:, 0:32], reads=[cm["Ones1"], dA], writes=[pc])
                        P.act(ecum[:, 0:32], pc[:, 0:32], AF.Exp, reads=[pc], writes=[ecum])
                        P.act(edec[:, 0:32], pc[:, 32:64], AF.Exp, reads=[pc], writes=[edec])
                        P.act(cdec[:, 0:64], pc[:, 64:128], AF.Exp, reads=[pc], writes=[cdec])
                        xd, xe = rxd.next(), rxe.next()
                        x3 = lambda b: b[:].rearrange("p (h q) -> p h q", q=64)
                        bc32 = lambda ap: ap.unsqueeze(2).to_broadcast([128, 32, 64])
                        P.tt("pool", x3(xd), x3(xt), bc32(dt_[:, 0:32]), ALU.mult, reads=[xt, dt_], writes=[xd])
                        P.tt("dve", x3(xe), x3(xd), bc32(edec[:, 0:32]), ALU.mult, reads=[xd, edec], writes=[xe])
                        ya = rya.next()
                        if d == 1:
                            P.dma("sp", ya[:], self.YF[ts_, :], reads=[self.YFk], writes=[ya])
                        else:
                            P.tt("pool", x3(ya), x3(xt), bc32(dsk[:, 0:32]), ALU.mult, reads=[xt, dsk], writes=[ya])
                        for g in range(4):
                            hsl = slice(g * 8, g * 8 + 8)
                            lall = rl.next()
                            P.tt("pool", lall[:], U[:].unsqueeze(1).to_broadcast([128, 8, 128]),
                                 dA[:, hsl].unsqueeze(2).to_broadcast([128, 8, 128]), ALU.mult, reads=[U, dA], writes=[lall])
                            pcb = self.psum.next()
                            P.mm(pcb[:, 0:128], lhsT=xf[:, 16 + g, :], rhs=xf[:, 20 + g, :], reads=[xf], writes=[pcb])
                            cbt = rcbt.next()
                            P.cp("act", cbt[:], pcb[:, 0:128], reads=[pcb], writes=[cbt])
                            pyi = self.psum.next()
                            for hf in range(2):
                                pd = self.psum.next()
                                for h4 in range(4):
                                    P.mm(pd[:, h4 * 128:(h4 + 1) * 128], lhsT=lall[:, hf * 4 + h4, :], rhs=Tri[:],
                                         reads=[lall, Tri], writes=[pd], signal=(h4 == 3))
                                tm, lm, mt = rtm.next(), rlm.next(), rmt.next()
                                P.tt("dve", tm[:].rearrange("p (a b) -> p a b", b=128), pd[:, :].rearrange("p (a b) -> p a b", b=128),
                                     neg[:].unsqueeze(1).to_broadcast([128, 4, 128]), ALU.add, reads=[pd, neg], writes=[tm])
                                P.act(lm[:], tm[:], AF.Exp, reads=[tm], writes=[lm])
                                P.tt("pool", mt[:], lm[:].rearrange("p (a b) -> p a b", b=128),
                                     cbt[:].unsqueeze(1).to_broadcast([128, 4, 128]), ALU.mult, reads=[lm, cbt], writes=[mt])
                                for h4 in range(4):
                                    hl = hf * 4 + h4
                                    hh = g * 8 + hl
                                    P.mm(pyi[:, hl * 64:(hl + 1) * 64], lhsT=mt[:, h4, :], rhs=xd[:, hh * 64:(hh + 1) * 64],
                                         reads=[mt, xd], writes=[pyi], signal=(h4 == 3))
                            pyo = self.psum.next()
                            js = range(2) if d == 0 else range(1, -1, -1)
                            for jn, jc in enumerate(js):
                                rs = slice(64 * jc, 64 * jc + 64)
                                ci = gi * 2 + jc
                                P.mm(pyo[rs, :], lhsT=xf[:, 20 + g, rs], rhs=Hbf[:, g, :], reads=[xf, Hbf], writes=[pyo],
                                     signal=(jn == 1), tile_position=(0, 64 * jc))
                                pst = self.psum.next()
                                P.mm(pst[:, :], lhsT=bt[rs, g, :], rhs=xe[rs, g * 512:(g + 1) * 512], reads=[bt, xe], writes=[pst],
                                     tile_position=(64 * jc, 0))
                                P.tt("dve", H32[:, g, :].rearrange("p (h q) -> p h q", q=64),
                                     H32[:, g, :].rearrange("p (h q) -> p h q", q=64),
                                     cdec[:, jc * 32 + g * 8:jc * 32 + g * 8 + 8].unsqueeze(2).to_broadcast([128, 8, 64]), ALU.mult,
                                     reads=[H32, cdec], writes=[H32])
                                P.tt("dve", H32[:, g, :], H32[:, g, :], pst[:, :], ALU.add, reads=[H32, pst], writes=[H32])
                                seg_done = ((ci + 1) % 4 == 0) if d == 0 else (ci % 4 == 0)
                                if seg_done:
                                    sgi = ci // 4
                                    if self.cfg.get("state_out", True):
                                        so = rso.next()
                                        for q2 in range(2):
                                            pt = self.psum.next()
                                            for h4 in range(4):
                                                hl = q2 * 4 + h4
                                                P.tr(pt[0:64, h4 * 128:(h4 + 1) * 128], H32[:, g, hl * 64:(hl + 1) * 64], self.ident[:],
                                                     reads=[H32, self.ident], writes=[pt], signal=(h4 == 3))
                                            if q2 == 1:
                                                so = rso.next()
                                            P.cp("act", so[:], pt[0:64, :], reads=[pt], writes=[so])
                                            k = Tok()
                                            P.dma("sp", self.dout["o_ssd"][sgi, j, d, g * 8 + q2 * 4:g * 8 + q2 * 4 + 4].rearrange("h p n -> p h n"),
                                                  so[:].rearrange("p (h n) -> p h n", n=128), reads=[so], writes=[k])
                                            self.outk.append(k)
                                    P.ts("pool", H32[:, g, :], H32[:, g, :], self.flag[:, 0:1], None, ALU.mult,
                                         reads=[H32, self.flag], writes=[H32])
                                P.cp("act", Hbf[:, g, :], H32[:, g, :], reads=[H32], writes=[Hbf])
                            tm = rtm.next()
                            P.tt("dve", tm[:].rearrange("p (h q) -> p h q", q=64), pyo[:, :].rearrange("p (h q) -> p h q", q=64),
                                 ecum[:, hsl].unsqueeze(2).to_broadcast([128, 8, 64]), ALU.mult, reads=[pyo, ecum], writes=[tm])
                            P.tt("pool", ya[:, g * 512:(g + 1) * 512], ya[:, g * 512:(g + 1) * 512], tm[:], ALU.add,
                                 reads=[ya, tm], writes=[ya])
                            P.tt("dve", ya[:, g * 512:(g + 1) * 512], pyi[:, :], ya[:, g * 512:(g + 1) * 512], ALU.add,
                                 reads=[pyi, ya], writes=[ya])
                        if d == 0:
                            P.dma("sp", self.YF[ts_, :], ya[:], reads=[ya], writes=[self.YFk])
                        else:
                            sz, yn, yT, xtile, ta, stat = rsz.next(), ryn.next(), ryT.next(), rx.next(), rta.next(), rst.next()
                            P.dma("sp", xtile[:], self.X[ts_, :], reads=[self.Xk[gi]], writes=[xtile])
                            for zb in range(4):
                                pz = self.psum.next()
                                for c in range(8):
                                    P.mm(pz[:, :], lhsT=hg[:, c, :], rhs=wz[:, c, zb * 512:(zb + 1) * 512], start=(c == 0), stop=(c == 7),
                                         reads=[hg, wz], writes=[pz], signal=(c == 7))
                                P.act(sz[:, zb * 512:(zb + 1) * 512], pz[:, :], AF.Silu, reads=[pz], writes=[sz])
                            P.tt("dve", ya[:], ya[:], sz[:], ALU.mult, reads=[ya, sz], writes=[ya])
                            P.act(sz[:], ya[:], AF.Square, reads=[ya], writes=[sz, stat], accum_out=stat[:, 0:1])
                            P.act(stat[:, 1:2], stat[:, 0:1], AF.Ln, reads=[stat], writes=[stat], scale=1.0 / XI, bias=self.epsb[:, 0:1])
                            P.act(stat[:, 2:3], stat[:, 1:2], AF.Exp, reads=[stat], writes=[stat], scale=-0.5)
                            P.stt(yn[:], ya[:], stat[:, 2:3], ng[:], ALU.mult, ALU.mult, reads=[ya, stat, ng], writes=[yn])
                            for hb in range(2):
                                pb = self.psum.next()
                                for k in range(8):
                                    P.tr(self.psbf(pb)[:, k * 128:(k + 1) * 128], yn[:, (hb * 8 + k) * 128:(hb * 8 + k + 1) * 128],
                                         self.identb[:], reads=[yn, self.identb], writes=[pb], signal=(k == 7))
                                P.cp("act" if hb == 0 else "dve", yT[:, hb * 8:(hb + 1) * 8, :].rearrange("p a b -> p (a b)"),
                                     self.psbf(pb)[:, 0:1024], reads=[pb], writes=[yT])
                            for hf in range(2):
                                pb = self.psum.next()
                                for k in range(16):
                                    P.mm(pb[:, :], lhsT=yT[:, k, :], rhs=wout[:, k, hf * 512:(hf + 1) * 512], start=(k == 0), stop=(k == 15),
                                         reads=[yT, wout], writes=[pb], signal=(k == 15))
                                P.tt("dve", ta[:, hf * 512:(hf + 1) * 512], pb[:, :], self.modG(0)[:, hf * 512:(hf + 1) * 512], ALU.mult,
                                     reads=[pb, self.mod], writes=[ta])
                            P.tt("pool", xtile[:], xtile[:], ta[:], ALU.add, reads=[xtile, ta], writes=[xtile])
                            P.dma("sp", self.X[ts_, :], xtile[:], reads=[xtile], writes=[self.Xk[gi]])
                    P.barrier()

    def dbg(self, name, ap, shape, buf, dt=F32):
        o = self.outp(name, shape, dt)
        k = Tok()
        self.P.dma("sp", o.ap(), ap, reads=[buf], writes=[k])
        self.outk.append(k)

    def sincos_turns(self, st, x, shape, cs_out, sn_out, tag, outk=None):
        P = self.P
        key = ("sc", tuple(shape))
        if not hasattr(self, "_sc"):
            self._sc = {}
        if key not in self._sc:
            self._sc[key] = (self.sb(st, "sc_i", shape, I32), self.sb(st, "sc_f", shape), self.sb(st, "sc_y", shape),
                             self.sb(st, "sc_m", shape))
        ki, kf, y, m = self._sc[key]
        TWO_PI = 2.0 * np.pi
        outs = [(sn_out, 0.0), (cs_out, 0.25)]
        for (o, off) in outs:
            oap = o[:] if isinstance(o, Buf) else o
            ow = [o] if isinstance(o, Buf) else list(outk)
            if off != 0.0:
                P.ts("dve", y[:], x[:], off, None, ALU.add, reads=[x], writes=[y])
                src = y
            else:
                src = x
            P.cp("dve", ki[:], src[:], reads=[src], writes=[ki])
            P.cp("dve", kf[:], ki[:], reads=[ki], writes=[kf])
            P.tt("dve", y[:], src[:], kf[:], ALU.subtract, reads=[src, kf], writes=[y])
            P.ts("dve", y[:], y[:], 0.5, -0.5, ALU.min, ALU.max, reads=[y], writes=[y])
            P.act(oap, y[:], AF.Sin, reads=[y], writes=ow, scale=TWO_PI)

    def phase_moe(self, i, moe_router, w_gate, w_up, w_down, capv_d, iota_d, tokid_d):
        P = self.P
        nc = self.nc
        n_exp = self.cfg.get("n_exp", NE)
        with ExitStack() as st:
            aff_all = self.sb(st, "aff_all", [128, NT, NE])
            pos_tok = self.sb(st, "pos_tok", [128, NT, NE])
            gm_tok = self.sb(st, "gm_tok", [128, NT, NE])
            tokid = self.sb(st, "tokid", [128, NT])
            tg_all = self.sb(st, "tg_all", [128, NT, NE, 2])
            iota = self.sb(st, "iota", [128, 512])
            P.dma("sp", tokid[:], tokid_d[:, :], writes=[tokid])
            P.dma("sp", iota[:], iota_d[:, :], writes=[iota])
            with ExitStack() as s2:
                affT = self.sb(s2, "affT", [16, T])
                wrt = self.sb(s2, "wrt", [128, 8, NE])
                P.dma("sp", wrt[:], moe_router[i].rearrange("(c p) e -> p c e", p=128), writes=[wrt])
                with ExitStack() as s3:
                    rx = self.ring(s3, "e_x", 2, [128, D])
                    rh = self.ring(s3, "e_h", 2, [128, D])
                    rhT = self.ring(s3, "e_hT", 2, [128, 8, 128])
                    rstat = self.ring(s3, "e_st", 3, [128, 8])
                    rlg = self.ring(s3, "e_lg", 3, [128, NE])
                    for tt in range(NT):
                        xt, h, hT, stat, lg = rx.next(), rh.next(), rhT.next(), rstat.next(), rlg.next()
                        self.norm_tile(tt, 1, xt, h, stat)
                        P.dma("pool", self.H2[tt * 128:(tt + 1) * 128, :], h[:], reads=[h], writes=[self.H2k])
                        for hb in range(2):
                            pb = self.psum.next()
                            for c4 in range(4):
                                c = hb * 4 + c4
                                P.tr(pb[:, c4 * 128:(c4 + 1) * 128], h[:, c * 128:(c + 1) * 128], self.ident[:],
                                     reads=[h, self.ident], writes=[pb], signal=(c4 == 3))
                            P.cp("act" if hb == 0 else "dve", hT[:, hb * 4:(hb + 1) * 4, :].rearrange("p c t -> p (c t)"),
                                 pb[:, :], reads=[pb], writes=[hT])
                        pl = self.psum.next()
                        for c in range(8):
                            P.mm(pl[:, 0:NE], lhsT=hT[:, c, :], rhs=wrt[:, c, :], start=(c == 0), stop=(c == 7),
                                 reads=[hT, wrt], writes=[pl], signal=(c == 7))
                        P.op("dve", lambda e, o=stat[:, 3:4], a=pl[:, 0:NE]: e.reduce_max(out=o, in_=a, axis=AX.X),
                             reads=[pl], writes=[stat])
                        P.ts("dve", stat[:, 4:5], stat[:, 3:4], -1.0, None, ALU.mult, reads=[stat], writes=[stat])
                        P.act(lg[:], pl[:, 0:NE], AF.Exp, reads=[pl, stat], writes=[lg, stat], bias=stat[:, 4:5],
                              accum_out=stat[:, 5:6])
                        P.op("dve", lambda e, o=stat[:, 6:7], a=stat[:, 5:6]: e.reciprocal(out=o, in_=a),
                             reads=[stat], writes=[stat])
                        P.ts("dve", aff_all[:, tt, :], lg[:], stat[:, 6:7], None, ALU.mult, reads=[lg, stat],
                             writes=[aff_all])
                        pt = self.psum.next()
                        P.tr(pt[0:16, 0:128], aff_all[:, tt, :], self.ident[:], reads=[aff_all, self.ident], writes=[pt])
                        P.cp("act", affT[:, tt * 128:(tt + 1) * 128], pt[0:16, 0:128], reads=[pt], writes=[affT])
                    P.barrier()
                if self.cfg.get("moe_stage", "full") == "a":
                    P.barrier()
                    return
                cmpb = self.sb(s2, "cmpb", [16, T])
                incl = self.sb(s2, "incl", [16, T])
                lo = self.sb(s2, "lo", [16, NSEG])
                hi = self.sb(s2, "hi", [16, NSEG])
                mid = self.sb(s2, "mid", [16, NSEG])
                cnt = self.sb(s2, "cnt", [16, NSEG])
                tot = self.sb(s2, "tot", [16, 1])
                ge = self.sb(s2, "ge", [16, NSEG])
                capv = self.sb(s2, "capv", [16, NSEG])
                ones16 = self.sb(s2, "ones16", [16, T])
                P.dma("sp", capv[:], capv_d[:, :], writes=[capv])
                P.memset("dve", lo[:], 0.0, writes=[lo])
                P.memset("dve", hi[:], 1.0, writes=[hi])
                P.memset("pool", ones16[:], 1.0, writes=[ones16])
                a3 = affT[:].rearrange("e (s t) -> e s t", t=SEG)
                c3 = cmpb[:].rearrange("e (s t) -> e s t", t=SEG)
                fl = self.flag[0:16, 0:1]
                for it in range(self.cfg.get("bisect", 30)):
                    P.tt("dve", mid[:], lo[:], hi[:], ALU.add, reads=[lo, hi], writes=[mid])
                    P.ts("dve", mid[:], mid[:], 0.5, None, ALU.mult, reads=[mid], writes=[mid])
                    P.tt("dve", c3, a3, mid[:].unsqueeze(2).to_broadcast([16, NSEG, SEG]), ALU.is_gt,
                         reads=[affT, mid], writes=[cmpb])
                    P.op("dve", lambda e: e.reduce_sum(out=cnt[:], in_=c3, axis=AX.X), reads=[cmpb], writes=[cnt])
                    P.op("dve", lambda e: e.reduce_sum(out=tot[:], in_=cnt[:], axis=AX.X), reads=[cnt], writes=[tot])
                    P.ts("dve", ge[:], cnt[:], -1.0, tot[:, 0:1], ALU.mult, ALU.add, reads=[cnt, tot], writes=[ge])
                    P.stt(cnt[:], ge[:], fl, cnt[:], ALU.mult, ALU.add, reads=[ge, cnt, self.flag], writes=[cnt])
                    P.tt("dve", ge[:], cnt[:], capv[:], ALU.is_ge, reads=[cnt, capv], writes=[ge])
                    P.tt("dve", cnt[:], mid[:], lo[:], ALU.subtract, reads=[mid, lo], writes=[cnt])
                    P.tt("dve", cnt[:], cnt[:], ge[:], ALU.mult, reads=[cnt, ge], writes=[cnt])
                    P.tt("dve", lo[:], lo[:], cnt[:], ALU.add, reads=[lo, cnt], writes=[lo])
                    P.tt("dve", cnt[:], hi[:], mid[:], ALU.subtract, reads=[hi, mid], writes=[cnt])
                    P.tt("dve", cnt[:], cnt[:], ge[:], ALU.mult, reads=[cnt, ge], writes=[cnt])
                    P.tt("dve", hi[:], mid[:], cnt[:], ALU.add, reads=[mid, cnt], writes=[hi])
                P.tt("dve", c3, a3, lo[:].unsqueeze(2).to_broadcast([16, NSEG, SEG]), ALU.is_gt,
                     reads=[affT, lo], writes=[cmpb])
                P.scan(incl[:], ones16[:], cmpb[:], 0.0, ALU.mult, ALU.add, reads=[ones16, cmpb], writes=[incl])
                P.tt("dve", incl[:], incl[:], cmpb[:], ALU.mult, reads=[incl, cmpb], writes=[incl])
                P.ts("dve", incl[:], incl[:], -1.0, None, ALU.add, reads=[incl], writes=[incl])
                P.tt("dve", cmpb[:], cmpb[:], affT[:], ALU.mult, reads=[cmpb, affT], writes=[cmpb])
                for tt in range(NT):
                    pt = self.psum.next()
                    P.tr(pt[:, 0:16], incl[:, tt * 128:(tt + 1) * 128], self.ident[0:16, 0:16],
                         reads=[incl, self.ident], writes=[pt], signal=False)
                    P.tr(pt[:, 16:32], cmpb[:, tt * 128:(tt + 1) * 128], self.ident[0:16, 0:16],
                         reads=[cmpb, self.ident], writes=[pt])
                    P.cp("act", pos_tok[:, tt, :], pt[:, 0:16], reads=[pt], writes=[pos_tok])
                    P.cp("dve", gm_tok[:, tt, :], pt[:, 16:32], reads=[pt], writes=[gm_tok])
                P.cp("dve", tg_all[:, :, :, 0], tokid[:].unsqueeze(2).to_broadcast([128, NT, NE]), reads=[tokid], writes=[tg_all])
                P.cp("dve", tg_all[:, :, :, 1], gm_tok[:], reads=[gm_tok], writes=[tg_all])
                P.barrier()
            if self.cfg.get("moe_stage", "full") == "b":
                return
            with ExitStack() as s4:
                self.psum = Ring(self.psum8.bufs[0:7])
                self.psum.check = True
                self.pslong = self.psum8.bufs[7]
                NW = 3
                wgs = [self.sb(s4, f"wg{k}", [128, 8, 512], BF16) for k in range(NW)]
                wus = [self.sb(s4, f"wu{k}", [128, 8, 512], BF16) for k in range(NW)]
                wds = [self.sb(s4, f"wd{k}", [128, 16, D], BF16) for k in range(2)]
                roh = self.ring(s4, "oh", 3, [128, 512])
                rrow = self.ring(s4, "row", 2, [2, 512])
                ridx = self.ring(s4, "idx", 2, [128, 4], I32)
                rgt = self.ring(s4, "gt", 2, [128, 4])
                rxs = self.ring(s4, "xs", 4, [128, D], BF16)
                rxsT = self.ring(s4, "xsT", 1, [128, 8, 512], BF16)
                rhid = self.ring(s4, "hid", 1, [128, 16, 512], BF16)
                rsg = self.ring(s4, "sg", 2, [128, 512], BF16)
                rys = self.ring(s4, "ys", 2, [128, D])
                chunks = [(ex, fg) for ex in range(n_exp) for fg in range(4)]

                def issue_chunk(n):
                    ex, fg = chunks[n]
                    wg, wu = wgs[n % NW], wus[n % NW]
                    P.dma("pool", wg[:], w_gate[i, ex, :, fg * 512:(fg + 1) * 512].rearrange("(c p) f -> p c f", p=128), writes=[wg])
                    P.dma("pool", wu[:], w_up[i, ex, :, fg * 512:(fg + 1) * 512].rearrange("(c p) f -> p c f", p=128), writes=[wu])

                def issue_wd(ex):
                    wd = wds[ex % 2]
                    P.dma("pool", wd[:], w_down[i, ex].rearrange("(fc p) d -> p fc d", p=128), writes=[wd])

                class IState:
                    pass

                def I_begin(ex):
                    stt_ = IState()
                    stt_.ex = ex
                    stt_.idx, stt_.gt = ridx.next(), rgt.next()
                    stt_.pi = self.pslong
                    return stt_

                def I_part(stt_, tts):
                    ex = stt_.ex
                    for tt in tts:
                        oh = roh.next()
                        P.ts("dve", oh[:], iota[:], pos_tok[:, tt, ex:ex + 1], None, ALU.is_equal,
                             reads=[iota, pos_tok], writes=[oh])
                        P.mm(stt_.pi[0:2, :], lhsT=tg_all[:, tt, ex, :], rhs=oh[:, :], start=(tt == 0), stop=(tt == NT - 1),
                             reads=[oh, tg_all], writes=[stt_.pi], signal=True)

                def I_end(stt_):
                    row = rrow.next()
                    P.cp("act", row[:], stt_.pi[0:2, :], reads=[stt_.pi], writes=[row])
                    pj = self.psum.next()
                    for jb in range(4):
                        P.tr(pj[:, 2 * jb:2 * jb + 2], row[0:2, jb * 128:(jb + 1) * 128], self.ident[0:2, 0:2],
                             reads=[row, self.ident], writes=[pj], signal=(jb == 3))
                    pj3 = pj[:, 0:8].rearrange("p (j two) -> p j two", two=2)
                    P.cp("dve", stt_.idx[:].unsqueeze(2), pj3[:, :, 0:1], reads=[pj], writes=[stt_.idx])
                    P.cp("act", stt_.gt[:].unsqueeze(2), pj3[:, :, 1:2], reads=[pj], writes=[stt_.gt])
                    stt_.xs = []
                    for jb in range(4):
                        xs = rxs.next()
                        P.idma(xs[:], None, self.H2[:, :], bass.IndirectOffsetOnAxis(ap=stt_.idx[:, jb:jb + 1], axis=0),
                               reads=[stt_.idx, self.H2k], writes=[xs])
                        stt_.xs.append(xs)

                def G_tr(stt_):
                    xsT = rxsT.next()
                    for jb in range(4):
                        xs = stt_.xs[jb]
                        pb = self.psum.next()
                        for c in range(8):
                            P.tr(self.psbf(pb)[:, c * 128:(c + 1) * 128], xs[:, c * 128:(c + 1) * 128], self.identb[:],
                                 reads=[xs, self.identb], writes=[pb], signal=(c == 7))
                        P.cp("act" if jb % 2 == 0 else "dve", xsT[:, :, jb * 128:(jb + 1) * 128],
                             self.psbf(pb)[:, 0:1024].rearrange("p (c t) -> p c t", t=128), reads=[pb], writes=[xsT])
                    stt_.xsT = xsT

                issue_wd(0)
                issue_chunk(0)
                if len(chunks) > 1:
                    issue_chunk(1)
                cur = I_begin(0)
                I_part(cur, range(NT))
                I_end(cur)
                G_tr(cur)
                for ex in range(n_exp):
                    nxt = None
                    if ex + 1 < n_exp:
                        issue_wd(ex + 1)
                        nxt = I_begin(ex + 1)
                    xsT = cur.xsT
                    hid = rhid.next()
                    wd = wds[ex % 2]
                    for fg in range(4):
                        n = ex * 4 + fg
                        if n + 2 < len(chunks):
                            issue_chunk(n + 2)
                        wg, wu = wgs[n % NW], wus[n % NW]
                        for f4 in range(4):
                            fc = fg * 4 + f4
                            pg, pu = self.psum.next(), self.psum.next()
                            for c in range(8):
                                P.mm(pg[:, :], lhsT=wg[:, c, f4 * 128:(f4 + 1) * 128], rhs=xsT[:, c, :],
                                     start=(c == 0), stop=(c == 7), reads=[wg, xsT], writes=[pg], signal=(c == 7))
                            for c in range(8):
                                P.mm(pu[:, :], lhsT=wu[:, c, f4 * 128:(f4 + 1) * 128], rhs=xsT[:, c, :],
                                     start=(c == 0), stop=(c == 7), reads=[wu, xsT], writes=[pu], signal=(c == 7))
                            sg = rsg.next()
                            P.act(sg[:], pg[:, :], AF.Silu, reads=[pg], writes=[sg])
                            P.tt("dve", hid[:, fc, :], pu[:, :], sg[:], ALU.mult, reads=[pu, sg], writes=[hid])
                        if nxt is not None:
                            I_part(nxt, range(fg * 8, fg * 8 + 8))
                    if nxt is not None:
                        I_end(nxt)
                    for jb in range(4):
                        ys = rys.next()
                        for hf in range(2):
                            py = self.psum.next()
                            for fc in range(16):
                                P.mm(py[:, :], lhsT=hid[:, fc, jb * 128:(jb + 1) * 128], rhs=wd[:, fc, hf * 512:(hf + 1) * 512],
                                     start=(fc == 0), stop=(fc == 15), reads=[hid, wd], writes=[py], signal=(fc == 15))
                            P.stt(ys[:, hf * 512:(hf + 1) * 512], py[:, :], cur.gt[:, jb:jb + 1],
                                  self.modG(1)[:, hf * 512:(hf + 1) * 512], ALU.mult, ALU.mult,
                                  reads=[py, cur.gt, self.mod], writes=[ys])
                        P.idma(self.X[:, :], bass.IndirectOffsetOnAxis(ap=cur.idx[:, jb:jb + 1], axis=0), ys[:], None,
                               reads=[ys, cur.idx], writes=self.Xk, compute_op=ALU.add)
                    if nxt is not None:
                        G_tr(nxt)
                        cur = nxt
            self.psum = self.psum8
            P.barrier()

    def psbf(self, pb):
        return pb.t.bitcast(BF16)

    def phase_final(self, norm_final, y_out, final_norm=True):
        P = self.P
        with ExitStack() as st:
            rx = self.ring(st, "f_x", 3, [128, D])
            rh = self.ring(st, "f_h", 2, [128, D])
            rstat = self.ring(st, "f_st", 3, [128, 8])
            nf = self.sb(st, "f_nf", [128, D])
            epsb = self.sb(st, "f_eps", [128, 1])
            P.memset("dve", epsb[:], EPS, writes=[epsb])
            P.dma("sp", nf[:], norm_final[0:1, :].broadcast_to([128, D]), writes=[nf])
            outk = []
            for tt in range(NT):
                xt = rx.next()
                P.dma("sp", xt[:], self.X[tt * 128:(tt + 1) * 128, :], reads=[self.Xk[tt]], writes=[xt])
                if final_norm:
                    h, stat = rh.next(), rstat.next()
                    P.act(h[:], xt[:], AF.Square, reads=[xt], writes=[h, stat], accum_out=stat[:, 0:1])
                    P.act(stat[:, 1:2], stat[:, 0:1], AF.Ln, reads=[stat], writes=[stat], scale=1.0 / D, bias=epsb[:, 0:1])
                    P.act(stat[:, 2:3], stat[:, 1:2], AF.Exp, reads=[stat], writes=[stat], scale=-0.5)
                    P.stt(xt[:], xt[:], stat[:, 2:3], nf[:], ALU.mult, ALU.mult, reads=[xt, stat, nf], writes=[xt])
                k = Tok()
                P.dma("sp", y_out[tt * 128:(tt + 1) * 128, :], xt[:], reads=[xt], writes=[k])
                outk.append(k)
            P.final_wait("sp", outk + self.outk)


def host_consts():
    c = {}
    c["ident"] = np.eye(128, dtype=np.float32)
    c["iota512"] = np.ascontiguousarray(np.broadcast_to(np.arange(512, dtype=np.float32)[None, :], (128, 512)))
    c["tokid"] = np.ascontiguousarray((np.arange(NT)[None, :] * 128 + np.arange(128)[:, None]).astype(np.float32))
    return c


def unit_inputs(inp, unit, pos_table):
    f32 = np.float32
    d = {}
    if unit == 0:
        d["x0"] = np.ascontiguousarray(np.asarray(inp["x_prompt"], f32).reshape(T, D))
        d["pos"] = np.zeros((T, D), f32)
        cond = np.asarray(inp["c_ctx"], f32)
        d["flag"] = np.zeros((128, 1), f32)
        d["capv"] = np.full((16, 16), 32.0, f32)
    else:
        b = unit - 1
        d["x0"] = np.ascontiguousarray(np.asarray(inp["x_sample"], f32)[b])
        d["pos"] = pos_table
        cond = np.asarray(inp["c"], f32)[b]
        d["flag"] = np.ones((128, 1), f32)
        d["capv"] = np.full((16, 16), 512.0, f32)
    d["condT"] = np.ascontiguousarray(cond.reshape(8, 128).T)
    return d


def s5_host(inp, unit):
    f32 = np.float32
    out = {}

    def gp(a):
        a = np.asarray(a, f32).reshape(2, 2, 32, 2, 64)
        return np.ascontiguousarray(a.transpose(0, 3, 4, 1, 2).reshape(2, 128, 2, 32))

    out["s5_lre"] = gp(inp["s5_lam_re"])
    out["s5_lim"] = gp(inp["s5_lam_im"])
    ldt = np.asarray(inp["s5_log_dt"], f32)
    out["s5_ldt"] = gp(np.broadcast_to(ldt[..., None], (2, 2, 64, 64)))
    if unit == 0:
        out["s5_h0r"] = np.zeros((2, 128, 2, 32), f32)
        out["s5_h0i"] = np.zeros((2, 128, 2, 32), f32)
    else:
        out["s5_h0r"] = gp(np.asarray(inp["state_s5_re"], f32)[unit - 1])
        out["s5_h0i"] = gp(np.asarray(inp["state_s5_im"], f32)[unit - 1])
    out["s5_dsk"] = np.ascontiguousarray(np.asarray(inp["s5_d"], f32).reshape(2, 8, 128).transpose(0, 2, 1))
    for nm, key in (("s5_bre", "s5_b_re"), ("s5_bim", "s5_b_im")):
        b = np.asarray(inp[key], f32).reshape(2, 2, 8, 4, 2, 64, 16)
        o = np.zeros((2, 2, 64, 2, 8, 4, 2, 16), f32)
        for gl in range(2):
            o[:, gl, :, :, :, :, gl, :] = b[:, :, :, :, gl].transpose(0, 4, 1, 2, 3, 5)
        out[nm] = np.ascontiguousarray(o.reshape(2, 128, 2, 8, 128))
    for nm, key in (("s5_cre", "s5_c_re"), ("s5_cim", "s5_c_im")):
        cc = np.asarray(inp[key], f32).reshape(2, 2, 8, 4, 2, 16, 64)
        o = np.zeros((2, 2, 64, 2, 8, 4, 4, 2, 16), f32)
        for gl in range(2):
            for q in range(4):
                o[:, gl, :, :, :, q, q, gl, :] = cc[:, :, :, q, gl].transpose(0, 4, 1, 2, 3)
        out[nm] = np.ascontiguousarray(o.reshape(2, 128, 2, 32, 128))
    out["s5_w_glu"] = np.asarray(inp["s5_w_glu"], f32)
    out["s5_b_glu"] = np.asarray(inp["s5_b_glu"], f32)
    out["tau256"] = np.ascontiguousarray(np.broadcast_to(np.arange(1, 257, dtype=f32)[None, :], (128, 256)))
    return out


def mixer_host(inp, unit):
    d = {}
    d.update(s5_host(inp, unit))
    d.update(hg_host(inp, unit))
    d.update(ssd_host(inp, unit))
    return d


def hg_host(inp, unit):
    f32 = np.float32
    d = {}
    d["hg_w_qig"] = np.asarray(inp["hg_w_qig"], f32)
    d["hg_w_f"] = np.asarray(inp["hg_w_f"], f32)
    d["hg_w_o"] = np.asarray(inp["hg_w_o"], f32)
    d["hg_bfT"] = np.ascontiguousarray(np.asarray(inp["hg_b_f"], f32).reshape(1, 2, 8, 128).transpose(0, 3, 1, 2))
    d["hg_gn"] = np.ascontiguousarray(np.asarray(inp["hg_norm"], f32).reshape(1, 128, 1))
    d["hg_lbl"] = np.ascontiguousarray(np.asarray(inp["hg_lb_logits"], f32).reshape(2, 4, 8, 128).transpose(3, 0, 1, 2))
    if unit == 0:
        d["hg_s0"] = np.zeros((2, 8, 128, 128), f32)
    else:
        d["hg_s0"] = np.ascontiguousarray(np.asarray(inp["state_hgrn"], f32)[unit - 1, 0])
    p = np.arange(128)
    same = (p[:, None] // 32) == (p[None, :] // 32)
    d["hg_maskF"] = (same & ((p[None, :] % 32) >= (p[:, None] % 32))).astype(f32)
    d["hg_maskB"] = (same & ((p[None, :] % 32) <= (p[:, None] % 32))).astype(f32)
    rm = np.ones((128, 512), f32); rm[:, ::32] = 0.0
    d["hg_rmask"] = rm
    return d


def ssd_host(inp, unit):
    f32 = np.float32
    d = {}
    d["ssd_w_in"] = np.asarray(inp["ssd_w_in"], f32)
    d["ssd_w_out"] = np.asarray(inp["ssd_w_out"], f32)
    d["ssd_cwT"] = np.ascontiguousarray(np.asarray(inp["ssd_conv_w"], f32).reshape(1, 5, 24, 128).transpose(0, 3, 2, 1))
    d["ssd_cbT"] = np.ascontiguousarray(np.asarray(inp["ssd_conv_b"], f32).reshape(1, 24, 128).transpose(0, 2, 1))
    d["ssd_dtb"] = np.ascontiguousarray(np.asarray(inp["ssd_dt_bias"], f32).reshape(1, 64))
    d["ssd_alog"] = np.ascontiguousarray(np.asarray(inp["ssd_a_log"], f32).reshape(1, 64))
    d["ssd_d"] = np.asarray(inp["ssd_d"], f32).reshape(1, 32)
    d["ssd_norm"] = np.asarray(inp["ssd_norm"], f32).reshape(1, 2048)
    if unit == 0:
        d["ssd_h0"] = np.zeros((128, 2, 4, 512), f32)
    else:
        h0 = np.asarray(inp["state_ssd"], f32)[unit - 1, 0]
        d["ssd_h0"] = np.ascontiguousarray(h0.reshape(2, 4, 8, 64, 128).transpose(4, 0, 1, 2, 3).reshape(128, 2, 4, 512))
    p = np.arange(128)
    same = (p[:, None] // 64) == (p[None, :] // 64)
    r, t = p[:, None], p[None, :]
    d["ssd_TriF"] = (same & (r <= t)).astype(f32)
    d["ssd_UF"] = (same & (r > t)).astype(f32)
    d["ssd_TriB"] = (same & (r >= t)).astype(f32)
    d["ssd_UB"] = (same & (r < t)).astype(f32)
    d["ssd_negF"] = np.where(same & (t >= r), 0.0, -30000.0).astype(f32)
    d["ssd_negB"] = np.where(same & (t <= r), 0.0, -30000.0).astype(f32)
    d["ssd_Ones0"] = np.ascontiguousarray(np.broadcast_to((p[:, None] < 64), (128, 128))).astype(f32)
    d["ssd_Ones1"] = np.ascontiguousarray(np.broadcast_to((p[:, None] >= 64), (128, 128))).astype(f32)
    return d


def pos_table():
    quarter = D // 4
    omega = (1.0 / (10000.0 ** (np.arange(quarter, dtype=np.float32) / np.float32(quarter)))).astype(np.float32)
    r = np.arange(64, dtype=np.float32)[:, None] * omega
    emb_r = np.concatenate([np.sin(r), np.cos(r)], axis=-1).astype(np.float32)
    emb = np.concatenate([np.broadcast_to(emb_r[:, None], (64, 64, D // 2)),
                          np.broadcast_to(emb_r[None], (64, 64, D // 2))], axis=-1)
    return np.ascontiguousarray(emb.reshape(T, D).astype(np.float32))


_CACHE = {}
UNIT_OF_CORE = [0, 3, 1, 3, 2, 3, 3, 3]


def kernel(**inputs):
    f32 = np.float32
    if "nc" not in _CACHE:
        b = Builder(dict())
        _CACHE["nc"] = b.build()
        _CACHE["b"] = b
    nc = _CACHE["nc"]
    pt = pos_table()
    shared = host_consts()
    for k in ["w_ada", "b_ada", "norm_mix", "norm_ffn", "moe_router", "moe_w_gate", "moe_w_up", "moe_w_down"]:
        shared[k] = np.asarray(inputs[k], f32)
    shared["norm_final"] = np.asarray(inputs["norm_final"], f32).reshape(1, -1)
    units = {}
    for u in range(3):
        d = dict(shared)
        d.update(unit_inputs(inputs, u, pt))
        d.update(mixer_host(inputs, u))
        units[u] = d
    units[3] = units[0]
    in_maps = [units[u] for u in UNIT_OF_CORE]
    res = run_bass_kernel_spmd(nc, in_maps, core_ids=list(range(8)))
    R = res.results
    c0, c1, c2 = UNIT_OF_CORE.index(0), UNIT_OF_CORE.index(1), UNIT_OF_CORE.index(2)
    y_prompt = np.asarray(R[c0]["y"], f32).reshape(16, 256, D)
    y_sample = np.stack([np.asarray(R[c1]["y"], f32), np.asarray(R[c2]["y"], f32)], axis=0)
    new_s5_re = np.asarray(R[c0]["o_s5re"], f32)
    new_s5_im = np.asarray(R[c0]["o_s5im"], f32)
    new_hgrn = np.asarray(R[c0]["o_hg"], f32)
    new_ssd = np.asarray(R[c0]["o_ssd"], f32)
    return (y_prompt, y_sample, new_s5_re, new_s5_im, new_hgrn, new_ssd)
```
